# Optimizing a Trainium2 kernel written in Bass

```python
import jax, jax.numpy as jnp
from jax import lax
import numpy as np

D_MODEL = 1024
BATCH = 8
SEQ = 4096
DEPTH = 1

CONV_CH = 512
CONV_K = 3
N_HEADS = 8
N_KV_GROUPS = 2
HEADS_PER_GROUP = N_HEADS // N_KV_GROUPS
HEAD_DIM = 64
NSA_WIDTH = N_HEADS * HEAD_DIM
KV_WIDTH = N_KV_GROUPS * HEAD_DIM
ROPE_DIM = HEAD_DIM // 4
ROPE_THETA = 500000.0
CMP_BLOCK = 32
CMP_STRIDE = 16
CMP_HIDDEN = 256
SEL_BLOCK = 64
N_SELECT = 16
WINDOW = 512
Q_BLOCK = 64
N_EXPERTS = 32
TOP_K = 4
D_FF = 1024
SWIGLU_LIMIT = 7.0
SWIGLU_ALPHA = 1.702
MOE_BLOCK = 128
DN_ALPHA = (2 * DEPTH) ** 0.25
DN_BETA = (8 * DEPTH) ** -0.25
LN_EPS = 1e-5
NEG_INF = -1e30
FORCE_SCORE = 1e4
IN_COLS = 3 * CONV_CH + NSA_WIDTH + 6 * KV_WIDTH + 3 * N_HEADS + 2 * D_MODEL

kernel_name = 'hybrid_shortconv_nsa_moe_deepnorm'


def layer_norm(x, g, b):
    xf = x.astype(jnp.float32)
    mu = jnp.mean(xf, axis=-1, keepdims=True)
    var = jnp.mean(jnp.square(xf - mu), axis=-1, keepdims=True)
    y = (xf - mu) * lax.rsqrt(var + LN_EPS)
    return (y * g.astype(jnp.float32) + b.astype(jnp.float32)).astype(x.dtype)


def partial_rope(t, positions):
    half = ROPE_DIM // 2
    inv_freq = ROPE_THETA ** (-jnp.arange(half, dtype=jnp.float32) * (2.0 / ROPE_DIM))
    ang = positions.astype(jnp.float32)[..., None] * inv_freq
    cos = jnp.cos(ang)[:, :, None, :].astype(t.dtype)
    sin = jnp.sin(ang)[:, :, None, :].astype(t.dtype)
    t1 = t[..., :half]
    t2 = t[..., half:ROPE_DIM]
    return jnp.concatenate([t1 * cos - t2 * sin, t2 * cos + t1 * sin, t[..., ROPE_DIM:]], axis=-1)


def compress_blocks(k, pos_emb, w1, w2):
    T = k.shape[2]
    n_cmp = (T - CMP_BLOCK) // CMP_STRIDE + 1
    idx = np.arange(n_cmp)[:, None] * CMP_STRIDE + np.arange(CMP_BLOCK)[None, :]
    blocks = k[:, :, idx] + pos_emb
    flat = blocks.reshape(blocks.shape[0], blocks.shape[1], n_cmp, CMP_BLOCK * HEAD_DIM)
    return jax.nn.gelu(flat @ w1) @ w2


def nsa_attention(q, kc_raw, vc_raw, ks, vs, kw, vw, gate_logits,
                  cmp_pos_k, cmp_w1_k, cmp_w2_k, cmp_pos_v, cmp_w1_v, cmp_w2_v):
    B, G, R, T, DH = q.shape
    f32 = jnp.float32
    t_idx = np.arange(T)

    kc = compress_blocks(kc_raw, cmp_pos_k, cmp_w1_k, cmp_w2_k)
    vc = compress_blocks(vc_raw, cmp_pos_v, cmp_w1_v, cmp_w2_v)
    n_cmp = kc.shape[2]
    c_start = np.arange(n_cmp) * CMP_STRIDE
    mask_c = (c_start + CMP_BLOCK - 1)[None, :] <= t_idx[:, None]
    s_c = jnp.einsum('bgrtd,bgcd->bgrtc', q, kc).astype(f32)
    p_c = jnp.where(mask_c, jax.nn.softmax(jnp.where(mask_c, s_c, NEG_INF), axis=-1), 0.0)
    o_c = jnp.einsum('bgrtc,bgcd->bgrtd', p_c.astype(vc.dtype), vc)

    n_sel = T // SEL_BLOCK
    j_start = np.arange(n_sel) * SEL_BLOCK
    overlap = ((c_start[:, None] < j_start[None, :] + SEL_BLOCK)
               & (c_start[:, None] + CMP_BLOCK > j_start[None, :])).astype(np.float32)
    imp = jnp.einsum('bgrtc,cj->bgtj', p_c, overlap)
    cur = t_idx // SEL_BLOCK
    jj = np.arange(n_sel)
    valid = jj[None, :] <= cur[:, None]
    forced = (jj[None, :] == 0) | (jj[None, :] == cur[:, None]) | (jj[None, :] == cur[:, None] - 1)
    sel_score = jnp.where(valid, jnp.where(forced, FORCE_SCORE, imp), NEG_INF)
    k_eff = min(N_SELECT, n_sel)
    _, sel_idx = lax.top_k(sel_score, k_eff)

    ks_blk = ks.reshape(B, G, n_sel, SEL_BLOCK, DH)
    vs_blk = vs.reshape(B, G, n_sel, SEL_BLOCK, DH)
    kw_pad = jnp.pad(kw, ((0, 0), (0, 0), (WINDOW, 0), (0, 0)))
    vw_pad = jnp.pad(vw, ((0, 0), (0, 0), (WINDOW, 0), (0, 0)))
    b_i = jnp.arange(B)[:, None, None, None]
    g_i = jnp.arange(G)[None, :, None, None]
    in_blk = jnp.arange(SEL_BLOCK)
    win_off = jnp.arange(WINDOW + Q_BLOCK)

    def query_block(i):
        q0 = i * Q_BLOCK
        qb = lax.dynamic_slice_in_dim(q, q0, Q_BLOCK, axis=3)
        tq = q0 + jnp.arange(Q_BLOCK)
        idx = lax.dynamic_slice_in_dim(sel_idx, q0, Q_BLOCK, axis=2)
        k_g = ks_blk[b_i, g_i, idx]
        v_g = vs_blk[b_i, g_i, idx]
        s = jnp.einsum('bgrqd,bgqnld->bgrqnl', qb, k_g).astype(f32)
        kpos = idx[..., None] * SEL_BLOCK + in_blk
        m = (kpos <= tq[:, None, None])[:, :, None]
        s = jnp.where(m, s, NEG_INF).reshape(B, G, R, Q_BLOCK, k_eff * SEL_BLOCK)
        p = jax.nn.softmax(s, axis=-1).reshape(B, G, R, Q_BLOCK, k_eff, SEL_BLOCK)
        o_s = jnp.einsum('bgrqnl,bgqnld->bgrqd', p.astype(v_g.dtype), v_g)
        k_w = lax.dynamic_slice_in_dim(kw_pad, q0, WINDOW + Q_BLOCK, axis=2)
        v_w = lax.dynamic_slice_in_dim(vw_pad, q0, WINDOW + Q_BLOCK, axis=2)
        kp = q0 - WINDOW + win_off
        diff = tq[:, None] - kp[None, :]
        mw = (diff >= 0) & (diff < WINDOW) & (kp[None, :] >= 0)
        s_w = jnp.einsum('bgrqd,bgkd->bgrqk', qb, k_w).astype(f32)
        p_w = jax.nn.softmax(jnp.where(mw, s_w, NEG_INF), axis=-1)
        o_w = jnp.einsum('bgrqk,bgkd->bgrqd', p_w.astype(v_w.dtype), v_w)
        return o_s, o_w

    o_s, o_w = lax.map(query_block, jnp.arange(T // Q_BLOCK))
    o_s = jnp.moveaxis(o_s, 0, 3).reshape(B, G, R, T, DH)
    o_w = jnp.moveaxis(o_w, 0, 3).reshape(B, G, R, T, DH)

    gates = jax.nn.sigmoid(gate_logits.astype(f32)).reshape(B, T, 3, G, R)
    gates = jnp.transpose(gates, (2, 0, 3, 4, 1))[..., None].astype(q.dtype)
    o = gates[0] * o_c + gates[1] * o_s + gates[2] * o_w
    return jnp.transpose(o, (0, 3, 1, 2, 4)).reshape(B, T, G * R * DH)


def moe_ffn(x, w_router, b_router, w_gate_up, b_gate_up, w_down, b_down):
    B, T, D = x.shape
    xf = x.reshape(B * T, D)
    N = B * T
    logits = (xf @ w_router + b_router).astype(jnp.float32)
    top_val, top_idx = lax.top_k(logits, TOP_K)
    gate = jax.nn.softmax(top_val, axis=-1)
    M = N * TOP_K
    e_flat = top_idx.reshape(M)
    tok_flat = jnp.repeat(jnp.arange(N, dtype=jnp.int32), TOP_K)
    g_flat = gate.reshape(M).astype(x.dtype)
    order = jnp.argsort(e_flat)
    e_sorted = e_flat[order]
    tok_sorted = tok_flat[order]
    g_sorted = g_flat[order]
    counts = jnp.zeros((N_EXPERTS,), jnp.int32).at[e_flat].add(1)
    start = jnp.cumsum(counts) - counts
    padded = (counts + MOE_BLOCK - 1) // MOE_BLOCK * MOE_BLOCK
    pend = jnp.cumsum(padded)
    pstart = pend - padded
    dest = pstart[e_sorted] + (jnp.arange(M, dtype=jnp.int32) - start[e_sorted])
    n_blocks = -(-M // MOE_BLOCK) + N_EXPERTS
    P = n_blocks * MOE_BLOCK
    row_tok = jnp.full((P,), N, jnp.int32).at[dest].set(tok_sorted)
    row_gate = jnp.zeros((P,), x.dtype).at[dest].set(g_sorted)
    block_expert = jnp.minimum(
        jnp.searchsorted(pend, jnp.arange(n_blocks, dtype=jnp.int32) * MOE_BLOCK, side='right'),
        N_EXPERTS - 1)
    x_pad = jnp.concatenate([xf, jnp.zeros((1, D), xf.dtype)], axis=0)

    def expert_block(args):
        toks, e = args
        xb = x_pad[toks]
        h = xb @ w_gate_up[e] + b_gate_up[e]
        x_glu = jnp.minimum(h[:, ::2], SWIGLU_LIMIT)
        x_lin = jnp.clip(h[:, 1::2], -SWIGLU_LIMIT, SWIGLU_LIMIT)
        act = x_glu * jax.nn.sigmoid(SWIGLU_ALPHA * x_glu) * (x_lin + 1.0)
        return act @ w_down[e] + b_down[e]

    y = lax.map(expert_block, (row_tok.reshape(n_blocks, MOE_BLOCK), block_expert))
    y = y.reshape(P, D) * row_gate[:, None]
    out = jnp.zeros((N + 1, D), y.dtype).at[row_tok].add(y)[:N]
    return out.reshape(B, T, D)


def hybrid_layer(x, positions, w_in, conv_w, cmp_pos_k, cmp_w1_k, cmp_w2_k, cmp_pos_v, cmp_w1_v, cmp_w2_v,
                 w_up_conv, w_up_nsa, w_o, ln1_g, ln1_b, w_router, b_router, w_gate_up, b_gate_up,
                 w_down, b_down, ln2_g, ln2_b):
    B, T, _ = x.shape
    G, R = N_KV_GROUPS, HEADS_PER_GROUP
    proj = x @ w_in
    sizes = [CONV_CH] * 3 + [NSA_WIDTH] + [KV_WIDTH] * 6 + [3 * N_HEADS, D_MODEL, D_MODEL]
    cuts = [int(c) for c in np.cumsum(sizes)[:-1]]
    (xv, b_gate, c_gate, q, kc, vc, ks, vs, kw, vw, nsa_gate, mg_a, mg_b) = jnp.split(proj, cuts, axis=-1)

    u = c_gate * xv
    conv = lax.conv_general_dilated(u, conv_w, window_strides=(1,), padding=[(CONV_K - 1, 0)],
                                    dimension_numbers=('NWC', 'WIO', 'NWC'), feature_group_count=CONV_CH)
    y_a = (b_gate * conv) @ w_up_conv

    qh = partial_rope(q.reshape(B, T, N_HEADS, HEAD_DIM), positions) * (HEAD_DIM ** -0.5)
    qh = qh.reshape(B, T, G, R, HEAD_DIM).transpose(0, 2, 3, 1, 4)

    def kv_heads(t, rotate):
        t = t.reshape(B, T, G, HEAD_DIM)
        if rotate:
            t = partial_rope(t, positions)
        return t.transpose(0, 2, 1, 3)

    o_nsa = nsa_attention(qh, kv_heads(kc, True), kv_heads(vc, False), kv_heads(ks, True), kv_heads(vs, False),
                          kv_heads(kw, True), kv_heads(vw, False), nsa_gate,
                          cmp_pos_k, cmp_w1_k, cmp_w2_k, cmp_pos_v, cmp_w1_v, cmp_w2_v)
    y_b = o_nsa @ w_up_nsa

    merged = jax.nn.sigmoid(mg_a) * y_a + jax.nn.sigmoid(mg_b) * y_b
    x1 = layer_norm(DN_ALPHA * x + merged @ w_o, ln1_g, ln1_b)
    x2 = layer_norm(DN_ALPHA * x1 + moe_ffn(x1, w_router, b_router, w_gate_up, b_gate_up, w_down, b_down),
                    ln2_g, ln2_b)
    return x2


def setup_inputs(seed: int = 0) -> dict:
    key = jax.random.key(seed)
    ks = jax.random.split(key, 24)
    f32 = jnp.float32

    def nrm(k, shape, scale):
        return jax.random.normal(k, (DEPTH,) + shape, f32) * scale

    x = jax.random.normal(ks[0], (BATCH, SEQ, D_MODEL), f32)
    offset = jax.random.randint(ks[1], (BATCH, 1), 0, 1024, dtype=jnp.int32)
    positions = offset + jnp.arange(SEQ, dtype=jnp.int32)[None, :]
    flat_cmp = CMP_BLOCK * HEAD_DIM
    return {
        'x': x,
        'positions': positions,
        'w_in': nrm(ks[2], (D_MODEL, IN_COLS), D_MODEL ** -0.5),
        'conv_w': nrm(ks[3], (CONV_K, 1, CONV_CH), CONV_K ** -0.5),
        'cmp_pos_k': nrm(ks[4], (CMP_BLOCK, HEAD_DIM), 0.1),
        'cmp_w1_k': nrm(ks[5], (flat_cmp, CMP_HIDDEN), flat_cmp ** -0.5),
        'cmp_w2_k': nrm(ks[6], (CMP_HIDDEN, HEAD_DIM), CMP_HIDDEN ** -0.5),
        'cmp_pos_v': nrm(ks[7], (CMP_BLOCK, HEAD_DIM), 0.1),
        'cmp_w1_v': nrm(ks[8], (flat_cmp, CMP_HIDDEN), flat_cmp ** -0.5),
        'cmp_w2_v': nrm(ks[9], (CMP_HIDDEN, HEAD_DIM), CMP_HIDDEN ** -0.5),
        'w_up_conv': nrm(ks[10], (CONV_CH, D_MODEL), CONV_CH ** -0.5),
        'w_up_nsa': nrm(ks[11], (NSA_WIDTH, D_MODEL), NSA_WIDTH ** -0.5),
        'w_o': nrm(ks[12], (D_MODEL, D_MODEL), D_MODEL ** -0.5 * DN_BETA),
        'ln1_g': 1.0 + nrm(ks[13], (D_MODEL,), 0.02),
        'ln1_b': nrm(ks[14], (D_MODEL,), 0.02),
        'w_router': nrm(ks[15], (D_MODEL, N_EXPERTS), D_MODEL ** -0.5),
        'b_router': nrm(ks[16], (N_EXPERTS,), 0.01),
        'w_gate_up': nrm(ks[17], (N_EXPERTS, D_MODEL, 2 * D_FF), D_MODEL ** -0.5),
        'b_gate_up': nrm(ks[18], (N_EXPERTS, 2 * D_FF), 0.02),
        'w_down': nrm(ks[19], (N_EXPERTS, D_FF, D_MODEL), D_FF ** -0.5 * DN_BETA),
        'b_down': nrm(ks[20], (N_EXPERTS, D_MODEL), 0.02),
        'ln2_g': 1.0 + nrm(ks[21], (D_MODEL,), 0.02),
        'ln2_b': nrm(ks[22], (D_MODEL,), 0.02),
    }


def reference(x, positions, w_in, conv_w, cmp_pos_k, cmp_w1_k, cmp_w2_k, cmp_pos_v, cmp_w1_v, cmp_w2_v,
              w_up_conv, w_up_nsa, w_o, ln1_g, ln1_b, w_router, b_router, w_gate_up, b_gate_up,
              w_down, b_down, ln2_g, ln2_b):
    h = x
    for l in range(DEPTH):
        h = hybrid_layer(h, positions, w_in[l], conv_w[l], cmp_pos_k[l], cmp_w1_k[l], cmp_w2_k[l],
                         cmp_pos_v[l], cmp_w1_v[l], cmp_w2_v[l], w_up_conv[l], w_up_nsa[l], w_o[l],
                         ln1_g[l], ln1_b[l], w_router[l], b_router[l], w_gate_up[l], b_gate_up[l],
                         w_down[l], b_down[l], ln2_g[l], ln2_b[l])
    return h
```

```python
import os as _os
import numpy as np
from contextlib import ExitStack
import concourse.bass as bass
import concourse.mybir as mybir
from concourse.bass_utils import run_bass_kernel_spmd

F32 = mybir.dt.float32
BF16 = mybir.dt.bfloat16
I32 = mybir.dt.int32
AF = mybir.ActivationFunctionType
ALU = mybir.AluOpType
AX = mybir.AxisListType

D_MODEL = 1024
CONV_CH = 512
N_HEADS = 8
HEAD_DIM = 64
N_EXPERTS = 32
D_FF = 1024
ROPE_THETA = 500000.0
DN_ALPHA = 2.0 ** 0.25
LN_EPS = 1e-5
IN_COLS = 4888
NEGBIG = -30000.0


class Buf:
    __slots__ = ("name", "w", "r")

    def __init__(self, name):
        self.name = name
        self.w = {}
        self.r = {}


class Sched:
    def __init__(self, nc, stack, n_dma_slots=6):
        self.nc = nc
        self.names = ["pe", "dve", "act", "pool", "sp"]
        self.sem = {}
        self.cnt = {}
        self.seen = {}
        self.prog = {}
        for e in self.names:
            self.sem[e] = stack.enter_context(nc.semaphore("sem_" + e))
            self.cnt[e] = 0
            self.seen[e] = {}
            self.prog[e] = []
        self.dq = {}
        self.dqi = {}
        for q in ("sp", "pool", "act"):
            self.dq[q] = [
                {"sem": stack.enter_context(nc.semaphore("dsem_%s%d" % (q, i))), "total": 0, "key": "d%s%d" % (q, i)}
                for i in range(n_dma_slots)
            ]
            self.dqi[q] = 0

    def _waits(self, e, reads, writes, extra=(), awrites=()):
        waits = {}
        seen = self.seen[e]

        def need(key, sem, val):
            if e == "pe" and key == "pe":
                return
            if seen.get(key, 0) >= val:
                return
            if key not in waits or waits[key][1] < val:
                waits[key] = (sem, val)

        for b in reads:
            for key, (sem, val) in b.w.items():
                need(key, sem, val)
        for b in writes:
            for key, (sem, val) in b.w.items():
                need(key, sem, val)
            for key, (sem, val) in b.r.items():
                need(key, sem, val)
        for b in awrites:
            for key, (sem, val) in b.r.items():
                need(key, sem, val)
        for key, sem, val in extra:
            need(key, sem, val)
        for key, (sem, val) in waits.items():
            seen[key] = val
        return list(waits.values())

    def _record(self, key, tok, reads, writes, awrites=()):
        for b in reads:
            b.r[key] = tok
        for b in writes:
            b.w = {key: tok}
            b.r = {}
        for b in awrites:
            b.w[key] = tok

    def op(self, e, fn, reads=(), writes=(), inc=True, awrites=()):
        wl = self._waits(e, reads, writes, (), awrites)
        semE = self.sem[e]
        if inc:
            self.cnt[e] += 1
            tok = (semE, self.cnt[e])
        else:
            tok = (semE, self.cnt[e] + 1)

        def emit(h, wl=wl, fn=fn, inc=inc, semE=semE):
            for sem, val in wl:
                h.wait_ge(sem, val)
            ins = fn(h)
            if inc:
                ins.then_inc(semE, 1)

        self.prog[e].append(emit)
        self._record(e, tok, reads, writes, awrites)

    def dma(self, q, fn, reads=(), writes=(), awrites=()):
        slots = self.dq[q]
        slot = slots[self.dqi[q] % len(slots)]
        self.dqi[q] += 1
        extra = []
        if slot["total"] > 0:
            extra.append((slot["key"], slot["sem"], slot["total"]))
        wl = self._waits(q, reads, writes, extra, awrites)
        slot["total"] += 16
        tok = (slot["sem"], slot["total"])
        sem = slot["sem"]

        def emit(h, wl=wl, fn=fn, sem=sem):
            for s, val in wl:
                h.wait_ge(s, val)
            fn(h).then_inc(sem, 16)

        self.prog[q].append(emit)
        self._record(slot["key"], tok, reads, writes, awrites)

    def drain_dma(self):
        for q in ("sp", "pool", "act"):
            for slot in self.dq[q]:
                if slot["total"] > 0 and self.seen[q].get(slot["key"], 0) < slot["total"]:
                    self.seen[q][slot["key"]] = slot["total"]

                    def emit(h, sem=slot["sem"], val=slot["total"]):
                        h.wait_ge(sem, val)

                    self.prog[q].append(emit)

    def flush(self):
        nc = self.nc
        prog = self.prog
        with nc.Block() as block:

            @block.tensor
            def _(h):
                for f in prog["pe"]:
                    f(h)

            @block.vector
            def _(h):
                for f in prog["dve"]:
                    f(h)

            @block.scalar
            def _(h):
                for f in prog["act"]:
                    f(h)

            @block.gpsimd
            def _(h):
                for f in prog["pool"]:
                    f(h)

            @block.sync
            def _(h):
                for f in prog["sp"]:
                    f(h)

        for e in self.names:
            self.prog[e] = []


def dims(T):
    d = dict(T=T, NT=T // 128, NCH=T // 512, NCMP=T // 16 - 1, NSEL=T // 64, CAP=T // 8 + 128)
    d["NCT"] = (d["NCMP"] + 127) // 128
    d["NCAPT"] = d["CAP"] // 128
    d["WW"] = 4 * d["NT"] - 1
    return d


def make_consts(T):
    import ml_dtypes
    d = dims(T)
    NT, NCT, NCMP, NSEL, CAP, WW = d["NT"], d["NCT"], d["NCMP"], d["NSEL"], d["CAP"], d["WW"]
    p = np.arange(128)
    ident = np.eye(128, dtype=np.float32)
    D1 = (p[None, :] - 16 * p[:, None]).astype(np.float32)
    D2 = (p[None, :] - p[:, None]).astype(np.float32)
    r = p % 64
    invf = np.where(r < 16, ROPE_THETA ** (-(r % 8).astype(np.float32) * (2.0 / 16.0)), 0.0).astype(np.float32)
    sgn = np.where(r < 8, -1.0, 1.0).astype(np.float32)
    m = np.arange(WW)
    dd = m[None, :] - 2 * (NT - 1) - (p[:, None] >= 64)
    Wf = np.where(dd == 0, 2e4, np.where(dd == -1, 1e4, 0.0)).astype(np.float32)
    Wv = np.where(dd > 0, -1e30, 0.0).astype(np.float32)
    eoff = np.broadcast_to((np.arange(32) * CAP).astype(np.float32)[None, :], (128, 32))
    cf = np.concatenate([ident, D1, D2, invf[:, None], sgn[:, None], Wf, Wv, eoff], axis=1).astype(np.float32)
    E = np.zeros((128, NT, 128), np.float32)
    for kt in range(NT):
        for k in range(128):
            E[2 * kt + k // 64, kt, k] = 1.0
            E[64 + 2 * kt + k // 64, kt, k] = 1.0
    ov = np.zeros((128, NCT, 64), np.float32)
    for ct in range(NCT):
        for pp in range(128):
            c = ct * 128 + pp
            if c < NCMP:
                for j in range(min(NSEL, 64)):
                    if 16 * c < 64 * j + 64 and 16 * c + 32 > 64 * j:
                        ov[pp, ct, j] = 1.0
    U = (p[:, None] < p[None, :]).astype(np.float32)
    ones = np.ones((128, 128), np.float32)
    cb = np.concatenate([ident, E.reshape(128, -1), ov.reshape(128, -1), U, ones], axis=1).astype(ml_dtypes.bfloat16)
    return np.ascontiguousarray(cf), np.ascontiguousarray(cb)


def _swap_cols(w64):
    idx = np.arange(64)
    idx[:8] = np.arange(8, 16)
    idx[8:16] = np.arange(0, 8)
    return w64[:, idx]


def prep_shared(inp):
    w_in = inp["w_in"][0]
    c = np.cumsum([0, 512, 512, 512, 512, 128, 128, 128, 128, 128, 128, 24, 1024, 1024])
    xv, bg, cg, q = (w_in[:, c[i]:c[i + 1]] for i in range(4))
    kc, vc, ks, vs, kw, vw = (w_in[:, c[i]:c[i + 1]] for i in range(4, 10))
    ng, mga, mgb = (w_in[:, c[i]:c[i + 1]] for i in range(10, 13))
    chunks = []
    for cc in range(4):
        s = slice(cc * 128, (cc + 1) * 128)
        chunks += [xv[:, s], cg[:, s], bg[:, s]]
    for j in range(4):
        h0 = q[:, j * 64:(j + 1) * 64]
        h1 = q[:, (4 + j) * 64:(5 + j) * 64]
        chunks += [np.concatenate([h0, h1], 1), np.concatenate([_swap_cols(h0), _swap_cols(h1)], 1)]
    for k in (kc, ks, kw):
        chunks += [k, np.concatenate([_swap_cols(k[:, :64]), _swap_cols(k[:, 64:])], 1)]
    chunks += [vc]
    sh = {}
    sh["w_fm"] = np.ascontiguousarray(np.concatenate(chunks, 1))
    sh["w_tm"] = np.ascontiguousarray(np.concatenate([vs, vw, ng], 1))
    sh["w_mg"] = np.ascontiguousarray(np.concatenate([mga, mgb], 1))
    cw = inp["conv_w"][0][:, 0, :]
    sh["convw"] = np.ascontiguousarray(cw.reshape(3, 4, 128).transpose(2, 1, 0).reshape(128, 12))
    for n in ("k", "v"):
        pt = inp["cmp_pos_" + n][0].T
        sh["posT_" + n] = np.ascontiguousarray(np.concatenate([pt, pt], 0))
        sh["w1" + n] = np.ascontiguousarray(inp["cmp_w1_" + n][0])
    w2k = inp["cmp_w2_k"][0]
    sh["w2k"] = np.ascontiguousarray(np.concatenate([w2k, w2k], 1))
    sh["w2v"] = np.ascontiguousarray(inp["cmp_w2_v"][0])
    sh["w_upc"] = np.ascontiguousarray(inp["w_up_conv"][0])
    sh["w_upn"] = np.ascontiguousarray(inp["w_up_nsa"][0])
    sh["w_o"] = np.ascontiguousarray(inp["w_o"][0])
    lnp = np.stack([inp["ln1_g"][0], inp["ln1_b"][0], inp["ln2_g"][0], inp["ln2_b"][0]], 0)
    sh["lnp"] = np.ascontiguousarray(np.broadcast_to(lnp[None], (128, 4, 1024)))
    sh["w_r"] = np.ascontiguousarray(inp["w_router"][0])
    sh["b_r"] = np.ascontiguousarray(np.broadcast_to(inp["b_router"][0][None], (128, 32)))
    wgu = inp["w_gate_up"][0]
    sh["w_glu"] = np.ascontiguousarray(wgu[:, :, 0::2])
    sh["w_lin"] = np.ascontiguousarray(wgu[:, :, 1::2])
    sh["w_dn"] = np.ascontiguousarray(inp["w_down"][0])
    bgu = inp["b_gate_up"][0].reshape(32, 8, 128, 2)
    sh["bgu"] = np.ascontiguousarray(bgu.transpose(2, 0, 3, 1).reshape(128, 32 * 2 * 8))
    sh["b_dn"] = np.ascontiguousarray(inp["b_down"][0])
    return sh


INPUT_SPECS = None


def input_specs(T):
    d = dims(T)
    cf, cb = make_consts(T) if False else (None, None)
    ncf = 128 * 3 + 2 + 2 * d["WW"] + 32
    ncb = 128 + d["NT"] * 128 + d["NCT"] * 64 + 128 + 128
    return [
        ("xT", [1024, T], F32), ("xtok", [T, 1024], F32), ("pos", [1, T], I32),
        ("w_fm", [1024, 27 * 128], F32), ("w_tm", [1024, 280], F32), ("w_mg", [1024, 2048], F32),
        ("convw", [128, 12], F32), ("posT_k", [128, 32], F32), ("posT_v", [128, 32], F32),
        ("w1k", [2048, 256], F32), ("w1v", [2048, 256], F32), ("w2k", [256, 128], F32), ("w2v", [256, 64], F32),
        ("w_upc", [512, 1024], F32), ("w_upn", [512, 1024], F32), ("w_o", [1024, 1024], F32),
        ("lnp", [128, 4, 1024], F32), ("w_r", [1024, 32], F32), ("b_r", [128, 32], F32),
        ("w_glu", [32, 1024, 1024], F32), ("w_lin", [32, 1024, 1024], F32), ("w_dn", [32, 1024, 1024], F32),
        ("bgu", [128, 512], F32), ("b_dn", [32, 1024], F32),
        ("cf", [128, ncf], F32), ("cb", [128, ncb], BF16),
    ]


class _Stop(Exception):
    pass


def build(T, debug=False, upto=9):
    try:
        return _build(T, debug, upto)
    except _Stop as e:
        return e.args[0]


def _build(T, debug, upto):
    d = dims(T)
    NT, NCH, NCMP, NCT, NSEL, CAP, NCAPT, WW = (d[k] for k in ("NT", "NCH", "NCMP", "NCT", "NSEL", "CAP", "NCAPT", "WW"))
    TH = min(T, 1024)
    NH = T // TH
    nc = bass.Bass("TRN2", target_bir_lowering=False)
    I = {}
    for name, shape, dt in input_specs(T):
        I[name] = nc.dram_tensor(name, shape, dt, kind="ExternalInput").ap()
    out = nc.dram_tensor("out", [T, 1024], F32, kind="ExternalOutput").ap()
    x1f_d = nc.dram_tensor("x1f_scr", [T, 1024], F32, kind="Internal").ap()
    xg_d = nc.dram_tensor("xg_scr", [32 * CAP + 128, 1024], BF16, kind="Internal").ap()
    on_d = nc.dram_tensor("on_scr", [128, 4, T], BF16, kind="Internal").ap()
    y_d = nc.dram_tensor("y_scr", [32 * CAP + 128, 1024], F32, kind="Internal").ap()
    dbg = {}
    if debug:
        dbg["x1"] = nc.dram_tensor("dbg_x1", [T, 1024], F32, kind="ExternalOutput").ap()
        dbg["onsa"] = nc.dram_tensor("dbg_onsa", [T, 512], F32, kind="ExternalOutput").ap()
        dbg["lg"] = nc.dram_tensor("dbg_lg", [T, 32], F32, kind="ExternalOutput").ap()
        dbg["nsel"] = nc.dram_tensor("dbg_nsel", [T, 2, NSEL], F32, kind="ExternalOutput").ap()

    bufs = {}

    def B(*key):
        if key not in bufs:
            bufs[key] = Buf(str(key))
        return bufs[key]

    with ExitStack() as top:
        S = Sched(nc, top)

        def TT(e, out, in0, in1, op, reads, writes, **kw):
            S.op(e, lambda h: h.tensor_tensor(out=out, in0=in0, in1=in1, op=op), reads, writes, **kw)

        def TS(e, out, in0, s1, s2, op0, op1, reads, writes, **kw):
            if op1 is None:
                S.op(e, lambda h: h.tensor_scalar(out=out, in0=in0, scalar1=s1, scalar2=None, op0=op0), reads, writes, **kw)
            else:
                S.op(e, lambda h: h.tensor_scalar(out=out, in0=in0, scalar1=s1, scalar2=s2, op0=op0, op1=op1), reads, writes, **kw)

        def STT(e, out, in0, sc, in1, op0, op1, reads, writes, **kw):
            S.op(e, lambda h: h.scalar_tensor_tensor(out=out, in0=in0, scalar=sc, in1=in1, op0=op0, op1=op1), reads, writes, **kw)

        def ACT(out, in_, func, reads, writes, scale=1.0, bias=None, **kw):
            if bias is None:
                S.op("act", lambda h: h.activation(out=out, in_=in_, func=func, scale=scale), reads, writes, **kw)
            else:
                S.op("act", lambda h: h.activation(out=out, in_=in_, func=func, scale=scale, bias=bias), reads, writes, **kw)

        def CP(e, out, in_, reads, writes, **kw):
            S.op(e, lambda h: h.tensor_copy(out=out, in_=in_), reads, writes, **kw)

        def MS(e, ap, val, reads, writes, **kw):
            S.op(e, lambda h: h.memset(ap, val), reads, writes, **kw)

        def RCP(out, in_, reads, writes):
            S.op("dve", lambda h: h.reciprocal(out=out, in_=in_), reads, writes)

        def DMA(q, out, in_, reads=(), writes=(), awrites=()):
            S.dma(q, lambda h: h.dma_start(out=out, in_=in_), reads, writes, awrites)

        def MM(bank_ap, pairs, reads, writes, first=True, last=True):
            n = len(pairs)

            def fn(h):
                ins = None
                for k, (l, r) in enumerate(pairs):
                    ins = h.matmul(bank_ap, lhsT=l, rhs=r, start=(first and k == 0), stop=(last and k == n - 1))
                return ins
            S.op("pe", fn, reads, writes)

        def sb(stack, name, shape, dt):
            return stack.enter_context(nc.sbuf_tensor("s_" + name, shape, dt))

        PS = [top.enter_context(nc.psum_tensor("ps%d" % i, [128, 512], F32)) for i in range(8)]
        PB = [B("ps", i) for i in range(8)]

        NCF = I["cf"].shape[1]
        NCB = I["cb"].shape[1]
        cf = sb(top, "cf", [128, NCF], F32)
        cb = sb(top, "cb", [128, NCB], BF16)
        ident_f = cf[:, 0:128]
        D1 = cf[:, 128:256]
        D2 = cf[:, 256:384]
        invf = cf[:, 384:385]
        sgn = cf[:, 385:386]
        Wf = cf[:, 386:386 + WW]
        Wv = cf[:, 386 + WW:386 + 2 * WW]
        eoff = cf[:, 386 + 2 * WW:386 + 2 * WW + 32]
        ident_b = cb[:, 0:128]
        o_ = 128
        Ecb = cb[:, o_:o_ + NT * 128].rearrange("p (a b) -> p a b", a=NT)
        o_ += NT * 128
        ovc = cb[:, o_:o_ + NCT * 64].rearrange("p (a b) -> p a b", a=NCT)
        o_ += NCT * 64
        Ust = cb[:, o_:o_ + 128]
        o_ += 128
        ones_b = cb[:, o_:o_ + 128]
        CF, CB = B("cf"), B("cb")
        DMA("sp", cf[:], I["cf"], writes=[CF])
        DMA("sp", cb[:], I["cb"], writes=[CB])

        epsc = sb(top, "epsc", [128, 1], F32)
        MS("pool", epsc[:], LN_EPS, [], [B("epsc")])
        offs = sb(top, "offs", [128, NT, 4], I32)
        gk = sb(top, "gk", [128, NT, 4], F32)
        gd = sb(top, "gd", [128, NT, 32], F32)
        mid = ExitStack()
        u2T = sb(mid, "u2T", [128, 4, T], BF16)

        def v4(ap, a=4):
            return ap.rearrange("p (a b) -> p a b", a=a)

        with ExitStack() as att:
            qT = sb(att, "qT", [128, 4, T], BF16)
            kT = [sb(att, "kT%d" % i, [128, T], BF16) for i in range(3)]
            vcT = sb(att, "vcT", [128, T], BF16)
            Vext = [sb(att, "Vext%d" % i, [128, NT, 2, 65], BF16) for i in range(2)]
            gates = sb(att, "gates", [128, NT, 24], F32)
            kcT = sb(att, "kcT", [128, NCT * 128], BF16)
            Vc = sb(att, "Vc", [128, NCT, 2, 65], BF16)
            MS("pool", Vext[0][:, :, :, 64:65], 1.0, [], [B("Vones", 0)])
            MS("pool", Vext[1][:, :, :, 64:65], 1.0, [], [B("Vones", 1)])
            MS("pool", Vc[:, :, :, 64:65], 1.0, [], [B("Vcones")])
            MS("pool", kcT[:], 0.0, [], [B("kcT")])

            with ExitStack() as p1:
                xTb = sb(p1, "xTb", [128, 8, TH], BF16)
                wbuf = [sb(p1, "wbuf%d" % i, [128, 8, 512], BF16) for i in range(2)]
                wtm = sb(p1, "wtm", [128, 8, 280], BF16)
                cosT = sb(p1, "cosT", [128, TH], F32)
                sinT = sb(p1, "sinT", [128, TH], F32)
                posi = sb(p1, "posi", [128, 512], I32)
                ang = sb(p1, "ang", [128, 512], F32)
                angi = sb(p1, "angi", [128, 512], I32)
                angf = sb(p1, "angf", [128, 512], F32)
                ut = [sb(p1, "ut%d" % i, [128, 514], F32) for i in range(2)]
                csb = [sb(p1, "csb%d" % i, [128, 512], F32) for i in range(2)]
                c1 = [sb(p1, "c1_%d" % i, [128, 512], F32) for i in range(2)]
                hal = sb(p1, "hal", [128, 4, 2], F32)
                cw = sb(p1, "cw", [128, 12], F32)
                rt1 = [sb(p1, "rt1_%d" % i, [128, 512], F32) for i in range(2)]
                rt2 = [sb(p1, "rt2_%d" % i, [128, 512], F32) for i in range(2)]
                DMA("sp", cw[:], I["convw"], writes=[B("cw")])
                MS("pool", hal[:], 0.0, [], [B("hal", cc) for cc in range(4)])
                DMA("pool", wtm[:], I["w_tm"].rearrange("(k p) n -> p k n", p=128), writes=[B("wtm")])

                groups = [[0, 1, 2], [3, 4, 5], [6, 7, 8], [9, 10, 11], [12, 13, 14, 15], [16, 17, 18, 19], [20, 21, 22, 23], [24, 25, 26]]
                gl = 0
                bank_rr = [0]

                def take_banks(n):
                    r = [(bank_rr[0] + i) % 6 for i in range(n)]
                    bank_rr[0] = (bank_rr[0] + n) % 6
                    return r

                ui = 0
                for hf in range(NH):
                    h0 = hf * TH
                    DMA("pool", xTb[:, :, :], I["xT"][:, h0:h0 + TH].rearrange("(k p) t -> p k t", p=128), writes=[B("xTb")])
                    for rc_ in range(TH // 512):
                        rs = slice(rc_ * 512, (rc_ + 1) * 512)
                        DMA("sp", posi[:], I["pos"][:, h0 + rc_ * 512:h0 + (rc_ + 1) * 512].to_broadcast([128, 512]), writes=[B("posi")])
                        CP("dve", ang[:], posi[:], [B("posi")], [B("ang")])
                        TS("dve", ang[:], ang[:], invf, float(1.0 / (2 * np.pi)), ALU.mult, ALU.mult, [B("ang"), CF], [B("ang")])
                        for which, tab in ((0, sinT), (1, cosT)):
                            if which == 1:
                                TS("dve", ang[:], ang[:], 0.25, None, ALU.add, None, [B("ang")], [B("ang")])
                            CP("dve", angi[:], ang[:], [B("ang")], [B("angi")])
                            CP("dve", angf[:], angi[:], [B("angi")], [B("angf")])
                            TT("dve", angf[:], ang[:], angf[:], ALU.subtract, [B("ang"), B("angf")], [B("angf")])
                            STT("dve", angf[:], angf[:], 0.5, angf[:], ALU.is_gt, ALU.subtract, [B("angf")], [B("angf")])
                            ACT(tab[:, rs], angf[:], AF.Sin, [B("angf")], [B("tab", which)], scale=float(-2 * np.pi))
                    TS("dve", sinT[:], sinT[:], sgn, None, ALU.mult, None, [B("tab", 0), CF], [B("tab", 0)])
                    if upto == 0.1:
                        S.drain_dma()
                        S.flush()
                        raise _Stop(nc)

                    for grp in groups:
                        wb = wbuf[gl % 2]
                        wB = B("wbuf", gl % 2)
                        gl += 1
                        ncols = len(grp) * 128
                        c0 = grp[0] * 128
                        DMA("pool", wb[:, :, 0:ncols], I["w_fm"][:, c0:c0 + ncols].rearrange("(k p) n -> p k n", p=128), writes=[wB])
                        for tcl in range(TH // 512):
                            t0 = h0 + tcl * 512
                            tl = tcl * 512
                            tc = t0 // 512
                            banks = take_banks(3) if len(grp) == 3 else take_banks(4)
                            for ci, ch in enumerate(grp):
                                bk = banks[ci]
                                MM(PS[bk][:, :], [(wb[:, k, ci * 128:(ci + 1) * 128], xTb[:, k, tl:tl + 512]) for k in range(8)], [wB, B("xTb")], [PB[bk]])
                            if grp[0] < 12:
                                cc = grp[0] // 3
                                bx, bc, bb = banks
                                u, uB = ut[ui % 2], B("ut", ui % 2)
                                cs, csB = csb[ui % 2], B("csb", ui % 2)
                                cc1, c1B = c1[ui % 2], B("c1", ui % 2)
                                ui += 1
                                ACT(cs[:], PS[bc][:, :], AF.Copy, [PB[bc]], [csB])
                                CP("pool", u[:, 0:2], hal[:, cc, :], [B("hal", cc)], [uB])
                                TT("dve", u[:, 2:514], PS[bx][:, :], cs[:], ALU.mult, [PB[bx], csB, uB], [uB])
                                CP("pool", hal[:, cc, :], u[:, 512:514], [uB], [B("hal", cc)])
                                TS("dve", cc1[:], u[:, 2:514], cw[:, cc * 3 + 2:cc * 3 + 3], None, ALU.mult, None, [uB, B("cw")], [c1B])
                                STT("dve", cc1[:], u[:, 1:513], cw[:, cc * 3 + 1:cc * 3 + 2], cc1[:], ALU.mult, ALU.add, [uB, c1B], [c1B])
                                STT("dve", cc1[:], u[:, 0:512], cw[:, cc * 3:cc * 3 + 1], cc1[:], ALU.mult, ALU.add, [uB, c1B], [c1B])
                                TT("dve", u2T[:, cc, t0:t0 + 512], PS[bb][:, :], cc1[:], ALU.mult, [PB[bb], c1B], [B("u2T", cc, tc)])
                            else:
                                ci = 0
                                while ci < len(grp):
                                    ch = grp[ci]
                                    if ch == 26:
                                        bk = banks[ci]
                                        ACT(vcT[:, t0:t0 + 512], PS[bk][:, :], AF.Copy, [PB[bk]], [B("vcT", tc)])
                                        ci += 1
                                        continue
                                    bq, bs = banks[ci], banks[ci + 1]
                                    if ch < 20:
                                        j = (ch - 12) // 2
                                        dten, dB = qT[:, j, :], B("qT", j, tc)
                                    else:
                                        br = (ch - 20) // 2
                                        dten, dB = kT[br][:, :], B("kT", br, tc)
                                    r1, r2, rB = rt1[ui % 2], rt2[ui % 2], B("rt", ui % 2)
                                    ui += 1
                                    TT("dve", r1[:], PS[bq][:, :], cosT[:, tl:tl + 512], ALU.mult, [PB[bq], B("tab", 1)], [rB])
                                    TT("dve", r2[:], PS[bs][:, :], sinT[:, tl:tl + 512], ALU.mult, [PB[bs], B("tab", 0), rB], [rB])
                                    TT("pool", dten[:, t0:t0 + 512], r1[:], r2[:], ALU.add, [rB], [dB])
                                    ci += 2
                        if upto == 0.2 and grp[0] == 0:
                            S.drain_dma()
                            S.flush()
                            raise _Stop(nc)
                        if upto == 0.25 and grp[0] == 12:
                            S.drain_dma()
                            S.flush()
                            raise _Stop(nc)
                        if upto == 0.3 and grp[0] == 24:
                            S.drain_dma()
                            S.flush()
                            raise _Stop(nc)
                    for il in range(TH // 128):
                        i = h0 // 128 + il
                        bk = 6 + (i % 2)
                        MM(PS[bk][:, 0:280], [(xTb[:, k, il * 128:(il + 1) * 128], wtm[:, k, :]) for k in range(8)], [B("wtm"), B("xTb")], [PB[bk]])
                        for vi in range(2):
                            ACT(Vext[vi][:, i, :, 0:64], v4(PS[bk][:, vi * 128:(vi + 1) * 128], 2), AF.Copy, [PB[bk], B("Vones", vi)], [B("Vext", vi, i)])
                        ACT(gates[:, i, :], PS[bk][:, 256:280], AF.Sigmoid, [PB[bk]], [B("gates", i)])
                S.flush()

            if upto == 1:
                S.drain_dma()
                S.flush()
                raise _Stop(nc)
            with ExitStack() as p2:
                w1 = {n: sb(p2, "w1" + n, [128, 32, 256], BF16) for n in "kv"}
                posT = {n: sb(p2, "posT" + n, [128, 32], BF16) for n in "kv"}
                w2k = sb(p2, "w2k", [128, 2, 128], BF16)
                w2v = sb(p2, "w2v", [128, 2, 64], BF16)
                cst = sb(p2, "cst", [128, 4], F32)
                hsb = sb(p2, "hsb", [128, 256], F32)
                h2 = sb(p2, "h2", [128, 256], F32)
                gT = {(n, g): sb(p2, "gT%s%d" % (n, g), [128, 2, 256], BF16) for n in "kv" for g in range(2)}
                for n in "kv":
                    for half in range(2):
                        DMA("pool", w1[n][half * 64:(half + 1) * 64, :, :], I["w1" + n].rearrange("(l d) h -> d l h", d=64), awrites=[B("w1", n)])
                    DMA("pool", posT[n][:], I["posT_" + n], writes=[B("posT", n)])
                DMA("pool", w2k[:], I["w2k"].rearrange("(c p) n -> p c n", p=128), writes=[B("w2k")])
                DMA("pool", w2v[:], I["w2v"].rearrange("(c p) n -> p c n", p=128), writes=[B("w2v")])
                bi = 0
                for ni, n in enumerate("kv"):
                    for hc in range(2):
                        bk = bi % 6
                        bi += 1
                        MM(PS[bk][:, 0:1], [(w1[n][0:64, l, hc * 128:(hc + 1) * 128], posT[n][0:64, l:l + 1]) for l in range(32)], [B("w1", n), B("posT", n)], [PB[bk]])
                        ACT(cst[:, ni * 2 + hc:ni * 2 + hc + 1], PS[bk][:, 0:1], AF.Copy, [PB[bk]], [B("cst", ni, hc)])
                for ni, n in enumerate("kv"):
                    src = kT[0] if n == "k" else vcT
                    srcB = [B("kT", 0, tc) for tc in range(NCH)] if n == "k" else [B("vcT", tc) for tc in range(NCH)]
                    for g in range(2):
                        for hc in range(2):
                            bk = bi % 6
                            bi += 1
                            MM(PS[bk][:, 0:NCMP], [(w1[n][64 * g:64 * g + 64, l, hc * 128:(hc + 1) * 128], src[64 * g:64 * g + 64, l:l + 16 * (NCMP - 1) + 1:16]) for l in range(32)],
                               [B("w1", n)] + srcB, [PB[bk]])
                            hs, hh2 = hsb[:, 0:NCMP], h2[:, 0:NCMP]
                            ACT(hs, PS[bk][:, 0:NCMP], AF.Identity, [PB[bk], B("cst", ni, hc)], [B("hsb")], bias=cst[:, ni * 2 + hc:ni * 2 + hc + 1])
                            TT("dve", hh2, hs, hs, ALU.mult, [B("hsb")], [B("h2")])
                            TS("dve", hh2, hh2, 0.044715, 1.0, ALU.mult, ALU.add, [B("h2")], [B("h2")])
                            TT("dve", hh2, hh2, hs, ALU.mult, [B("h2"), B("hsb")], [B("h2")])
                            ACT(hh2, hh2, AF.Sigmoid, [B("h2")], [B("h2")], scale=1.5957691216057308)
                            TT("dve", gT[(n, g)][:, hc, 0:NCMP], hh2, hs, ALU.mult, [B("h2"), B("hsb")], [B("gT", n, g, hc)])
                for g in range(2):
                    bk = bi % 6
                    bi += 1
                    MM(PS[bk][:, 0:NCMP], [(w2k[:, hc, :], gT[("k", g)][:, hc, 0:NCMP]) for hc in range(2)], [B("w2k"), B("gT", "k", g, 0), B("gT", "k", g, 1)], [PB[bk]])
                    ACT(kcT[64 * g:64 * g + 64, 0:NCMP], PS[bk][64 * g:64 * g + 64, 0:NCMP], AF.Copy, [PB[bk], B("kcT")], [], awrites=[B("kcT")])
                for g in range(2):
                    for ct in range(NCT):
                        cn = min(128, NCMP - ct * 128)
                        bk = bi % 6
                        bi += 1
                        MM(PS[bk][0:cn, 0:64], [(gT[("v", g)][:, hc, ct * 128:ct * 128 + cn], w2v[:, hc, :]) for hc in range(2)], [B("w2v"), B("gT", "v", g, 0), B("gT", "v", g, 1)], [PB[bk]])
                        ACT(Vc[0:cn, ct, g, 0:64], PS[bk][0:cn, 0:64], AF.Copy, [PB[bk], B("Vcones")], [], awrites=[B("Vc")])
                S.flush()

            if upto == 2:
                S.drain_dma()
                S.flush()
                raise _Stop(nc)
            with ExitStack() as p3:
                NPR = NT + 5 + 2
                Pring = sb(p3, "Pring", [128, NPR, 512], BF16)
                nselT = sb(p3, "nselT", [128, 4, 128], BF16)
                onst = [sb(p3, "onst%d" % i, [128, 4, 128], BF16) for i in range(2)]
                nsel = sb(p3, "nsel", [128, 128], BF16)
                MS("pool", nsel[:], 0.0, [], [B("nsel")])
                otok = sb(p3, "otok", [128, 512], BF16)
                acc = sb(p3, "acc", [128, 4, 64], F32)
                tmp3 = sb(p3, "tmp3", [128, 4, 64], F32)
                rc = sb(p3, "rc", [128, 3, 4], F32)
                impsb = sb(p3, "impsb", [128, 64], F32)
                scr2 = sb(p3, "scr2", [128, 64], F32)
                mx = sb(p3, "mx", [128, 16], F32)
                dbgt = sb(p3, "dbgt", [128, 512], F32) if debug else None
                D1b = D1.unsqueeze(1).to_broadcast([128, 4, 128])
                D2b = D2.unsqueeze(1).to_broadcast([128, 4, 128])
                pti = [0]
                sbank = [0]
                BOC, BIMP, BOS, BOW, BTR = 3, 4, 5, 6, 7
                trb = PS[BTR][:, :].bitcast(BF16)

                def score_tile(i, g, M, lhs_ap, lhsB, mask=None, extra=None, extraB=(), slot=0):
                    bk = sbank[0] % 3
                    sbank[0] += 1
                    P = Pring[:, slot, :]
                    PBf = B("Pt", slot)
                    pairs = [(lhs_ap, qT[64 * g:64 * g + 64, :, i * 128:(i + 1) * 128])]
                    if extra is not None:
                        pairs.append(extra)
                    MM(v4(PS[bk][0:M, :]), pairs, list(lhsB) + [B("qT", j, i // 4) for j in range(4)] + list(extraB), [PB[bk]])
                    ACT(P[0:M, :], PS[bk][0:M, :], AF.Exp, [PB[bk]], [PBf], scale=0.125)
                    if mask is not None:
                        Db, cmp_op, thr = mask
                        STT("dve", v4(P[0:M, :]), Db[0:M], float(thr), v4(P[0:M, :]), cmp_op, ALU.mult, [PBf, CF], [PBf])
                    return P, PBf

                def pv(tiles, bank, ncol=65):
                    n = len(tiles)

                    def fn(h):
                        ins = None
                        for hh in range(4):
                            for k, (P, PBf, M, rhs_ap, rhsB) in enumerate(tiles):
                                ins = h.matmul(PS[bank][:, hh * ncol:(hh + 1) * ncol], lhsT=P[0:M, hh * 128:(hh + 1) * 128], rhs=rhs_ap, start=(k == 0), stop=(k == n - 1))
                        return ins
                    rd = []
                    for (P, PBf, M, rhs_ap, rhsB) in tiles:
                        rd.append(PBf)
                        rd += list(rhsB)
                    S.op("pe", fn, rd, [PB[bank]])

                accs = [acc, sb(p3, "acc_b", [128, 4, 64], F32)]
                nselTs = [nselT, sb(p3, "nselT_b", [128, 4, 128], BF16)]
                tmp3b = sb(p3, "tmp3b", [128, 4, 64], F32)
                NI = min(NT, int(_os.environ.get('K_MAXI', '9999')))
                units = [(i, g) for i in range(NI) for g in range(2)]

                def stageA(i, g, u):
                    q0 = i * 128
                    gs = slice(64 * g, 64 * g + 64)
                    accu, accB = accs[u % 2], B("acc", u % 2)
                    nsT, nsTB = nselTs[u % 2], B("nselT", u % 2)
                    cts = []
                    for ct in range(NCT):
                        cn = min(128, NCMP - ct * 128)
                        M = min(cn, 8 * i + 7 - 128 * ct)
                        if M > 0:
                            cts.append((ct, M))
                    tl_c, tl_i = [], []
                    for idx, (ct, M) in enumerate(cts):
                        full = 16 * (ct * 128 + M - 1) + 31 <= q0
                        mask = None if full else (D1b, ALU.is_ge, 31 + 16 * 128 * ct - q0)
                        P, PBf = score_tile(i, g, M, kcT[gs, ct * 128:ct * 128 + M], [B("kcT")], mask, slot=NT + 5 + idx)
                        tl_c.append((P, PBf, M, Vc[0:M, ct, g, :], [B("Vc"), B("Vcones")]))
                        tl_i.append((P, PBf, M, ovc[0:M, ct, 0:NSEL], [CB]))
                    pv(tl_c, BOC)
                    pv(tl_i, BIMP, ncol=NSEL)
                    kts = list(range(max(0, i - 4), i + 1))
                    tl_w = []
                    for wi, kt in enumerate(kts):
                        mask = None
                        if kt == i:
                            mask = (D2b, ALU.is_ge, 0.0)
                        elif kt == i - 4:
                            mask = (D2b, ALU.is_le, -1.0)
                        P, PBf = score_tile(i, g, 128, kT[2][gs, kt * 128:(kt + 1) * 128], [B("kT", 2, kt // 4)], mask, slot=NT + wi)
                        tl_w.append((P, PBf, 128, Vext[1][:, kt, g, :], [B("Vext", 1, kt), B("Vones", 1)]))
                    pv(tl_w, BOW)
                    ocv = v4(PS[BOC][:, 0:260])
                    imp = impsb[:, 0:NSEL]
                    TS("dve", rc[:, 0, :], ocv[:, :, 64], 1e-30, None, ALU.max, None, [PB[BOC]], [B("rc", 0)])
                    RCP(rc[:, 0, :], rc[:, 0, :], [B("rc", 0)], [B("rc", 0)])
                    TS("dve", imp, PS[BIMP][:, 0:NSEL], rc[:, 0, 0:1], None, ALU.mult, None, [PB[BIMP], B("rc", 0)], [B("imp")])
                    for hh in range(1, 4):
                        STT("dve", imp, PS[BIMP][:, hh * NSEL:(hh + 1) * NSEL], rc[:, 0, hh:hh + 1], imp, ALU.mult, ALU.add, [PB[BIMP], B("rc", 0), B("imp")], [B("imp")])
                    w0 = 2 * (NT - 1) - 2 * i
                    TT("dve", imp, imp, Wf[:, w0:w0 + NSEL], ALU.max, [B("imp"), CF], [B("imp")])
                    TT("dve", imp, imp, Wv[:, w0:w0 + NSEL], ALU.add, [B("imp"), CF], [B("imp")])
                    MS("dve", impsb[:, 0:1], 3e4, [B("imp")], [B("imp")])
                    S.op("dve", lambda h: h.max(out=mx[:, 0:8], in_=imp), [B("imp")], [B("mx")])
                    S.op("dve", lambda h: h.match_replace(out=scr2[:, 0:NSEL], in_to_replace=mx[:, 0:8], in_values=imp, imm_value=-3e38), [B("imp"), B("mx")], [B("scr2")])
                    S.op("dve", lambda h: h.max(out=mx[:, 8:16], in_=scr2[:, 0:NSEL]), [B("scr2"), B("mx")], [B("mx")])
                    TS("dve", nsel[:, 0:NSEL], imp, mx[:, 15:16], NEGBIG, ALU.is_lt, ALU.mult, [B("imp"), B("mx")], [B("nsel")])
                    TS("dve", nsel[:, 64:64 + NSEL], imp, mx[:, 15:16], NEGBIG, ALU.is_lt, ALU.mult, [B("imp"), B("mx"), B("nsel")], [], awrites=[B("nsel")])
                    if debug:
                        CP("dve", dbgt[:, 0:NSEL], nsel[:, 0:NSEL], [B("nsel")], [B("dbgt")])
                        DMA("sp", dbg["nsel"][i * 128:(i + 1) * 128, g, :], dbgt[:, 0:NSEL], reads=[B("dbgt")])
                    S.op("pe", lambda h: h.transpose(out=trb[:, 0:128], in_=nsel[:, :], identity=ident_b), [B("nsel"), CB], [PB[BTR]])
                    ACT(nsT[:, :, :], trb[:, 0:128].unsqueeze(1).to_broadcast([128, 4, 128]), AF.Copy, [PB[BTR]], [nsTB])
                    TT("dve", rc[:, 0, :], rc[:, 0, :], gates[:, i, g * 4:g * 4 + 4], ALU.mult, [B("rc", 0), B("gates", i)], [B("rc", 0)])
                    TT("dve", accu[:], ocv[:, :, 0:64], rc[:, 0, :].unsqueeze(2).to_broadcast([128, 4, 64]), ALU.mult, [PB[BOC], B("rc", 0)], [accB])
                    owv = v4(PS[BOW][:, 0:260])
                    TS("dve", rc[:, 2, :], owv[:, :, 64], 1e-30, None, ALU.max, None, [PB[BOW]], [B("rc", 2)])
                    RCP(rc[:, 2, :], rc[:, 2, :], [B("rc", 2)], [B("rc", 2)])
                    TT("dve", rc[:, 2, :], rc[:, 2, :], gates[:, i, 16 + g * 4:16 + g * 4 + 4], ALU.mult, [B("rc", 2), B("gates", i)], [B("rc", 2)])
                    TT("dve", tmp3[:], owv[:, :, 0:64], rc[:, 2, :].unsqueeze(2).to_broadcast([128, 4, 64]), ALU.mult, [PB[BOW], B("rc", 2)], [B("tmp3")])
                    TT("dve", accu[:], accu[:], tmp3[:], ALU.add, [accB, B("tmp3")], [accB])

                def stageB(i, g, u):
                    gs = slice(64 * g, 64 * g + 64)
                    accu, accB = accs[u % 2], B("acc", u % 2)
                    nsT, nsTB = nselTs[u % 2], B("nselT", u % 2)
                    tl_s = []
                    for kt in range(i + 1):
                        mask = (D2b, ALU.is_ge, 0.0) if kt == i else None
                        P, PBf = score_tile(i, g, 128, kT[1][gs, kt * 128:(kt + 1) * 128], [B("kT", 1, kt // 4)], mask,
                                            extra=(Ecb[64 * g:64 * g + NSEL, kt, :], nsT[64 * g:64 * g + NSEL, :, :]), extraB=[nsTB, CB], slot=kt)
                        tl_s.append((P, PBf, 128, Vext[0][:, kt, g, :], [B("Vext", 0, kt), B("Vones", 0)]))
                    pv(tl_s, BOS)
                    osv = v4(PS[BOS][:, 0:260])
                    TS("dve", rc[:, 1, :], osv[:, :, 64], 1e-30, None, ALU.max, None, [PB[BOS]], [B("rc", 1)])
                    RCP(rc[:, 1, :], rc[:, 1, :], [B("rc", 1)], [B("rc", 1)])
                    TT("dve", rc[:, 1, :], rc[:, 1, :], gates[:, i, 8 + g * 4:8 + g * 4 + 4], ALU.mult, [B("rc", 1), B("gates", i)], [B("rc", 1)])
                    TT("dve", tmp3b[:], osv[:, :, 0:64], rc[:, 1, :].unsqueeze(2).to_broadcast([128, 4, 64]), ALU.mult, [PB[BOS], B("rc", 1)], [B("tmp3b")])
                    TT("dve", v4(otok[:, g * 256:(g + 1) * 256]), accu[:], tmp3b[:], ALU.add, [accB, B("tmp3b")], [B("otok", g)])
                    if g == 1:
                        def trs(h):
                            ins = None
                            for c4 in range(4):
                                ins = h.transpose(out=trb[:, c4 * 128:(c4 + 1) * 128], in_=otok[:, c4 * 128:(c4 + 1) * 128], identity=ident_b)
                            return ins
                        S.op("pe", trs, [B("otok", 0), B("otok", 1), CB], [PB[BTR]])
                        ACT(onst[i % 2][:], v4(trb[:, 0:512]), AF.Copy, [PB[BTR]], [B("onst", i % 2)])
                        DMA("sp", on_d[:, :, i * 128:(i + 1) * 128], onst[i % 2][:], reads=[B("onst", i % 2)], awrites=[B("on_d")])
                        if debug:
                            CP("dve", dbgt[:], otok[:], [B("otok", 0), B("otok", 1)], [B("dbgt")])
                            DMA("sp", dbg["onsa"][i * 128:(i + 1) * 128, :], dbgt[:], reads=[B("dbgt")])

                for u in range(len(units) + 1):
                    if u < len(units):
                        stageA(units[u][0], units[u][1], u)
                    if u >= 1:
                        stageB(units[u - 1][0], units[u - 1][1], u - 1)
                S.flush()

        if upto == 3:
            S.drain_dma()
            S.flush()
            raise _Stop(nc)
        XG, YD, X1F = B("xg_d"), B("y_d"), B("x1f_d")

        def layer_norm(pre, preB, gi, dst, dstB, st6, mv, sfx, lnp, ge="pool"):
            for hf in range(2):
                S.op("dve", lambda h, hf=hf: h.bn_stats(out=st6[:, hf, :], in_=pre[:, hf * 512:(hf + 1) * 512]), [preB], [B("st6" + sfx, hf)])
            S.op("dve", lambda h: h.bn_aggr(out=mv[:, 0:2], in_=st6[:, :, :].rearrange("p a b -> p (a b)")), [B("st6" + sfx, 0), B("st6" + sfx, 1)], [B("mv" + sfx)])
            ACT(mv[:, 2:3], mv[:, 1:2], AF.Sqrt, [B("mv" + sfx)], [B("mv" + sfx)], bias=epsc[:, 0:1])
            RCP(mv[:, 2:3], mv[:, 2:3], [B("mv" + sfx)], [B("mv" + sfx)])
            STT("dve", mv[:, 3:4], mv[:, 0:1], -1.0, mv[:, 2:3], ALU.mult, ALU.mult, [B("mv" + sfx)], [B("mv" + sfx)])
            ACT(pre[:], pre[:], AF.Identity, [preB, B("mv" + sfx)], [preB], scale=mv[:, 2:3], bias=mv[:, 3:4])
            TT(ge, pre[:], pre[:], lnp[:, gi, :], ALU.mult, [preB, B("lnp")], [preB])
            TT(ge, dst[:], pre[:], lnp[:, gi + 1, :], ALU.add, [preB, B("lnp")], [dstB])

        with ExitStack() as p4:
            wmg = sb(p4, "wmg", [128, 8, 2048], BF16)
            wupc = sb(p4, "wupc", [128, 4, 1024], BF16)
            wupn = sb(p4, "wupn", [128, 4, 1024], BF16)
            wo = sb(p4, "wo", [128, 8, 1024], BF16)
            wr = sb(p4, "wr", [128, 8, 32], F32)
            br_ = sb(p4, "br_", [128, 32], F32)
            xTc = [sb(p4, "xTc%d" % i, [128, 8, 512], BF16) for i in range(2)]
            oncs = [sb(p4, "onc%d" % i, [128, 4, 512], BF16) for i in range(2)]
            lnp = sb(p4, "lnp", [128, 2, 1024], F32)
            DMA("sp", lnp[:], I["lnp"][:, 0:2, :], writes=[B("lnp")])
            xt = [sb(p4, "xt%d" % i, [128, 1024], F32) for i in range(2)]
            sga = sb(p4, "sga", [128, 512], F32)
            sgb = sb(p4, "sgb", [128, 512], F32)
            m1 = sb(p4, "m1", [128, 512], F32)
            m2 = sb(p4, "m2", [128, 512], F32)
            mrgT = [sb(p4, "mrgT%d" % i, [128, 8, 512], BF16) for i in range(2)]
            pre = sb(p4, "pre", [128, 1024], F32)
            x1 = [sb(p4, "x1_%d" % i, [128, 1024], F32) for i in range(2)]
            x1b = [sb(p4, "x1b%d" % i, [128, 1024], BF16) for i in range(2)]
            x1T = [sb(p4, "x1T%d" % i, [128, 8, 128], F32) for i in range(2)]
            st6 = sb(p4, "st6", [128, 2, 6], F32)
            mv = sb(p4, "mv", [128, 4], F32)
            lg = sb(p4, "lg", [128, 32], F32)
            mx4 = sb(p4, "mx4", [128, 8], F32)
            nm = sb(p4, "nm", [128, 1], F32)
            msk = sb(p4, "msk", [128, 32], F32)
            mskb = sb(p4, "mskb", [128, 32], BF16)
            ex = sb(p4, "ex", [128, 32], F32)
            ssum = sb(p4, "ssum", [128, 1], F32)
            carry = sb(p4, "carry", [128, 32], F32)
            posf = sb(p4, "posf", [128, 32], F32)
            offv = sb(p4, "offv", [128, 32], F32)
            ovf = sb(p4, "ovf", [128, 32], F32)
            oh = sb(p4, "oh", [128, 32], F32)
            t32 = sb(p4, "t32", [128, 32], F32)
            offk = sb(p4, "offk", [128, 4], F32)
            DMA("pool", wmg[:], I["w_mg"].rearrange("(k p) n -> p k n", p=128), writes=[B("wmg")])
            DMA("pool", wupc[:], I["w_upc"].rearrange("(k p) n -> p k n", p=128), writes=[B("wupc")])
            DMA("pool", wupn[:], I["w_upn"].rearrange("(k p) n -> p k n", p=128), writes=[B("wupn")])
            DMA("pool", wo[:], I["w_o"].rearrange("(k p) n -> p k n", p=128), writes=[B("wo")])
            DMA("sp", wr[:], I["w_r"].rearrange("(k p) n -> p k n", p=128), writes=[B("wr")])
            DMA("sp", br_[:], I["b_r"], writes=[B("br")])
            MS("pool", carry[:], 0.0, [], [B("carry")])
            bi = 0
            bi_ = [0]

            def load_chunk(tc):
                t0 = tc * 512
                DMA("pool", xTc[tc % 2][:], I["xT"][:, t0:t0 + 512].rearrange("(k p) t -> p k t", p=128), writes=[B("xTc", tc % 2)])
                DMA("sp", oncs[tc % 2][:], on_d[:, :, t0:t0 + 512], reads=[B("on_d")], writes=[B("onc", tc % 2)])

            def merge_step(tc, oc):
                t0 = tc * 512
                xc, xcB = xTc[tc % 2], B("xTc", tc % 2)
                onc, oncB = oncs[tc % 2], B("onc", tc % 2)
                cs_ = slice(oc * 128, (oc + 1) * 128)
                b0, b1, b2, b3 = [(bi_[0] + j) % 4 for j in range(4)]
                MM(PS[b0][:, :], [(wmg[:, k, oc * 128:(oc + 1) * 128], xc[:, k, :]) for k in range(8)], [B("wmg"), xcB], [PB[b0]])
                ACT(sga[:], PS[b0][:, :], AF.Sigmoid, [PB[b0]], [B("sga")])
                MM(PS[b1][:, :], [(wmg[:, k, 1024 + oc * 128:1024 + (oc + 1) * 128], xc[:, k, :]) for k in range(8)], [B("wmg"), xcB], [PB[b1]])
                ACT(sgb[:], PS[b1][:, :], AF.Sigmoid, [PB[b1]], [B("sgb")])
                MM(PS[b2][:, :], [(wupc[:, k, cs_], u2T[:, k, t0:t0 + 512]) for k in range(4)], [B("wupc")] + [B("u2T", k, tc) for k in range(4)], [PB[b2]])
                TT("dve", m1[:], PS[b2][:, :], sga[:], ALU.mult, [PB[b2], B("sga")], [B("m1")])
                MM(PS[b3][:, :], [(wupn[:, k, cs_], onc[:, k, :]) for k in range(4)], [B("wupn"), oncB], [PB[b3]])
                TT("dve", m2[:], PS[b3][:, :], sgb[:], ALU.mult, [PB[b3], B("sgb")], [B("m2")])
                TT("pool", mrgT[tc % 2][:, oc, :], m1[:], m2[:], ALU.add, [B("m1"), B("m2")], [B("mrgT", tc % 2, oc)])


            def H1(i):
                tc, tt = i // 4, i % 4
                xti, xtB = xt[i % 2], B("xt", i % 2)
                x1i, x1B = x1[i % 2], B("x1", i % 2)
                x1bi, x1bB = x1b[i % 2], B("x1b", i % 2)
                DMA("sp", xti[:], I["xtok"][i * 128:(i + 1) * 128, :], writes=[xtB])
                for hf in range(2):
                    bz = 4 + hf
                    MM(PS[bz][:, :], [(mrgT[tc % 2][:, k, tt * 128:(tt + 1) * 128], wo[:, k, hf * 512:(hf + 1) * 512]) for k in range(8)], [B("wo")] + [B("mrgT", tc % 2, k) for k in range(8)], [PB[bz]])
                    STT("dve", pre[:, hf * 512:(hf + 1) * 512], xti[:, hf * 512:(hf + 1) * 512], DN_ALPHA, PS[bz][:, :], ALU.mult, ALU.add, [xtB, PB[bz]], [B("pre")] if hf == 0 else [], awrites=[] if hf == 0 else [B("pre")])
                layer_norm(pre, B("pre"), 0, x1i, x1B, st6, mv, "a", lnp)
                DMA("sp", x1f_d[i * 128:(i + 1) * 128, :], x1i[:], reads=[x1B], awrites=[X1F])
                if debug:
                    DMA("sp", dbg["x1"][i * 128:(i + 1) * 128, :], x1i[:], reads=[x1B])
                ACT(x1bi[:], x1i[:], AF.Copy, [x1B], [x1bB])
                for half in range(2):
                    bt = 6 + half

                    def trf(h, half=half, bt=bt, x1i=x1i):
                        ins = None
                        for k4 in range(4):
                            k = half * 4 + k4
                            ins = h.transpose(out=PS[bt][:, k4 * 128:(k4 + 1) * 128], in_=x1i[:, k * 128:(k + 1) * 128], identity=ident_f)
                        return ins
                    S.op("pe", trf, [x1B, CF], [PB[bt]])
                    if half == 0:
                        ACT(x1T[i % 2][:, 0:4, :], v4(PS[bt][:, :]), AF.Copy, [PB[bt]], [B("x1T", i % 2, 0)])
                    else:
                        ACT(x1T[i % 2][:, 4:8, :], v4(PS[bt][:, :]), AF.Copy, [PB[bt]], [B("x1T", i % 2, 1)])


            def H2(i):
                x1bi, x1bB = x1b[i % 2], B("x1b", i % 2)
                bl = bi_[0] % 4
                MM(PS[bl][:, 0:32], [(x1T[i % 2][:, k, :], wr[:, k, :]) for k in range(8)], [B("x1T", i % 2, 0), B("x1T", i % 2, 1), B("wr")], [PB[bl]])
                TT("dve", lg[:], PS[bl][:, 0:32], br_[:], ALU.add, [PB[bl], B("br")], [B("lg")])
                if debug:
                    DMA("sp", dbg["lg"][i * 128:(i + 1) * 128, :], lg[:], reads=[B("lg")])
                S.op("dve", lambda h: h.max(out=mx4[:], in_=lg[:]), [B("lg")], [B("mx4")])
                TS("dve", msk[:], lg[:], mx4[:, 3:4], None, ALU.is_ge, None, [B("lg"), B("mx4")], [B("msk")])
                TS("dve", nm[:], mx4[:, 0:1], -1.0, None, ALU.mult, None, [B("mx4")], [B("nm")])
                ACT(ex[:], lg[:], AF.Exp, [B("lg"), B("nm")], [B("ex")], bias=nm[:, 0:1])
                TT("dve", ex[:], ex[:], msk[:], ALU.mult, [B("ex"), B("msk")], [B("ex")])
                S.op("dve", lambda h: h.reduce_sum(out=ssum[:], in_=ex[:], axis=AX.X), [B("ex")], [B("ssum")])
                RCP(ssum[:], ssum[:], [B("ssum")], [B("ssum")])
                TS("dve", gd[:, i, :], ex[:], ssum[:, 0:1], None, ALU.mult, None, [B("ex"), B("ssum")], [B("gd", i)])
                CP("dve", mskb[:], msk[:], [B("msk")], [B("mskb")])
                bp = (bi_[0] + 1) % 4
                bc_ = (bi_[0] + 2) % 4
                MM(PS[bp][:, 0:32], [(Ust, mskb[:])], [CB, B("mskb")], [PB[bp]])
                MM(PS[bc_][:, 0:32], [(ones_b, mskb[:])], [CB, B("mskb")], [PB[bc_]])
                TT("dve", posf[:], PS[bp][:, 0:32], carry[:], ALU.add, [PB[bp], B("carry")], [B("posf")])
                TT("dve", carry[:], PS[bc_][:, 0:32], carry[:], ALU.add, [PB[bc_], B("carry")], [B("carry")])
                TS("dve", ovf[:], posf[:], float(CAP), 1e6, ALU.is_ge, ALU.mult, [B("posf")], [B("ovf")])
                TT("dve", offv[:], posf[:], eoff, ALU.add, [B("posf"), CF], [B("offv")])
                TT("dve", offv[:], offv[:], ovf[:], ALU.add, [B("offv"), B("ovf")], [B("offv")])
                TS("dve", offv[:], offv[:], float(32 * CAP), None, ALU.min, None, [B("offv")], [B("offv")])
                for k in range(4):
                    TS("dve", oh[:], lg[:], mx4[:, k:k + 1], None, ALU.is_equal, None, [B("lg"), B("mx4")], [B("oh")])
                    TT("dve", t32[:], oh[:], offv[:], ALU.mult, [B("oh"), B("offv")], [B("t32")])
                    S.op("dve", lambda h, k=k: h.reduce_sum(out=offk[:, k:k + 1], in_=t32[:], axis=AX.X), [B("t32")], [B("offk", k)])
                    TT("dve", t32[:], oh[:], gd[:, i, :], ALU.mult, [B("oh"), B("gd", i), B("t32")], [B("t32")])
                    S.op("dve", lambda h, k=k, i=i: h.reduce_sum(out=gk[:, i, k:k + 1], in_=t32[:], axis=AX.X), [B("t32")], [B("gk", i, k)])
                CP("dve", offs[:, i, :], offk[:], [B("offk", k) for k in range(4)], [B("offs", i)])
                for k in range(4):
                    S.dma("pool", lambda h, i=i, k=k, x1bi=x1bi: h.indirect_dma_start(
                        out=xg_d, out_offset=bass.IndirectOffsetOnAxis(ap=offs[:, i, k:k + 1], axis=0), in_=x1bi[:], in_offset=None,
                        ), reads=[x1bB, B("offs", i)], awrites=[XG])
                bi_[0] += 3


            load_chunk(0)
            for oc in range(8):
                merge_step(0, oc)
            pend = None
            for tc in range(NCH):
                if tc + 1 < NCH:
                    load_chunk(tc + 1)
                for tt in range(4):
                    i = tc * 4 + tt
                    H1(i)
                    if tc + 1 < NCH:
                        merge_step(tc + 1, 2 * tt)
                        merge_step(tc + 1, 2 * tt + 1)
                    if pend is not None:
                        H2(pend)
                    pend = i
            H2(pend)
            S.flush()

        if upto == 4:
            S.drain_dma()
            S.flush()
            raise _Stop(nc)
        mid.close()
        with ExitStack() as p5:
            wg = [sb(p5, "wg%d" % i, [128, 8, 1024], BF16) for i in range(2)]
            wl = [sb(p5, "wl%d" % i, [128, 8, 1024], BF16) for i in range(2)]
            wd = [sb(p5, "wd%d" % i, [128, 8, 1024], BF16) for i in range(2)]
            bgu = sb(p5, "bgu", [128, 32, 2, 8], F32)
            xgt = [sb(p5, "xgt%d" % i, [128, NCAPT, 1024], BF16) for i in range(2)]
            xgT = sb(p5, "xgT", [128, 8, CAP], BF16)
            actT = sb(p5, "actT", [128, 8, CAP], BF16)
            NCK = (CAP + 511) // 512
            CW = CAP // NCK
            NBF = 4
            g1 = [sb(p5, "g1_%d" % i, [128, CW], F32) for i in range(NBF)]
            sg = [sb(p5, "sg_%d" % i, [128, CW], F32) for i in range(NBF)]
            l1 = [sb(p5, "l1_%d" % i, [128, CW], F32) for i in range(NBF)]
            ysb = [sb(p5, "ysb%d" % i, [128, 1024], F32) for i in range(2)]
            DMA("sp", bgu[:], I["bgu"].rearrange("p (e a f) -> p e a f", e=32, a=2), writes=[B("bgu")])
            MS("pool", ysb[0][0:1, :], 0.0, [], [B("ysb", 0)])
            DMA("sp", y_d[32 * CAP:32 * CAP + 1, :], ysb[0][0:1, :], reads=[B("ysb", 0)], awrites=[YD])
            nch = [(c * CW, CW) for c in range(NCK)]
            ei = 0
            yi = 0
            bi = 0
            def load_expert(e):
                eb = e % 2
                DMA("sp", xgt[eb][:], xg_d[e * CAP:(e + 1) * CAP, :].rearrange("(s p) d -> p s d", p=128), reads=[XG], writes=[B("xgt", eb)])
                DMA("pool", wg[eb][:], I["w_glu"][e].rearrange("(k p) n -> p k n", p=128), writes=[B("wg", eb)])
                DMA("pool", wl[eb][:], I["w_lin"][e].rearrange("(k p) n -> p k n", p=128), writes=[B("wl", eb)])
                DMA("pool", wd[eb][:], I["w_dn"][e].rearrange("(k p) n -> p k n", p=128), writes=[B("wd", eb)])

            load_expert(0)
            for e in range(N_EXPERTS):
                eb = e % 2
                if e + 1 < N_EXPERTS:
                    load_expert(e + 1)
                trbb = None
                for s in range(NCAPT):
                    bt = 6 + (s % 2)
                    trbb = PS[bt][:, :].bitcast(BF16)

                    def trx(h, s=s, trbb=trbb, eb=eb):
                        ins = None
                        for k in range(8):
                            ins = h.transpose(out=trbb[:, k * 128:(k + 1) * 128], in_=xgt[eb][:, s, k * 128:(k + 1) * 128], identity=ident_b)
                        return ins
                    S.op("pe", trx, [B("xgt", eb), CB], [PB[bt]])
                    if s % 2 == 0:
                        ACT(xgT[:, :, s * 128:(s + 1) * 128], v4(trbb[:, 0:1024], 8), AF.Copy, [PB[bt]], [B("xgT", s)])
                    else:
                        CP("dve", xgT[:, :, s * 128:(s + 1) * 128], v4(trbb[:, 0:1024], 8), [PB[bt]], [B("xgT", s)])
                xgB = [B("xgT", s) for s in range(NCAPT)]
                for f in range(8):
                    fs = slice(f * 128, (f + 1) * 128)
                    for (n0, nn) in nch:
                        bg_, bl_ = bi % 4, (bi + 1) % 4
                        bi += 2
                        gg, ggB = g1[ei % NBF], B("g1", ei % NBF)
                        ss, ssB = sg[ei % NBF], B("sg", ei % NBF)
                        ll, llB = l1[ei % NBF], B("l1", ei % NBF)
                        ei += 1
                        MM(PS[bg_][:, 0:nn], [(wg[eb][:, k, fs], xgT[:, k, n0:n0 + nn]) for k in range(8)], [B("wg", eb)] + xgB, [PB[bg_]])
                        MM(PS[bl_][:, 0:nn], [(wl[eb][:, k, fs], xgT[:, k, n0:n0 + nn]) for k in range(8)], [B("wl", eb)] + xgB, [PB[bl_]])
                        TS("dve", gg[:, 0:nn], PS[bg_][:, 0:nn], bgu[:, e, 0, f:f + 1], 7.0, ALU.add, ALU.min, [PB[bg_], B("bgu")], [ggB])
                        ACT(ss[:, 0:nn], gg[:, 0:nn], AF.Sigmoid, [ggB], [ssB], scale=1.702)
                        TS("dve", ll[:, 0:nn], PS[bl_][:, 0:nn], bgu[:, e, 1, f:f + 1], 7.0, ALU.add, ALU.min, [PB[bl_], B("bgu")], [llB])
                        TS("dve", ll[:, 0:nn], ll[:, 0:nn], -7.0, 1.0, ALU.max, ALU.add, [llB], [llB])
                        TT("pool", gg[:, 0:nn], gg[:, 0:nn], ss[:, 0:nn], ALU.mult, [ggB, ssB], [ggB])
                        TT("pool", actT[:, f, n0:n0 + nn], gg[:, 0:nn], ll[:, 0:nn], ALU.mult, [ggB, llB], [B("actT", f)] if n0 == 0 else [], awrites=[] if n0 == 0 else [B("actT", f)])
                aB = [B("actT", f) for f in range(8)]
                for s in range(NCAPT):
                    yy, yB = ysb[yi % 2], B("ysb", yi % 2)
                    yi += 1
                    for hf in range(2):
                        by = 4 + hf
                        MM(PS[by][:, :], [(actT[:, f, s * 128:(s + 1) * 128], wd[eb][:, f, hf * 512:(hf + 1) * 512]) for f in range(8)], [B("wd", eb)] + aB, [PB[by]])
                        ACT(yy[:, hf * 512:(hf + 1) * 512], PS[by][:, :], AF.Copy, [PB[by]], [yB] if hf == 0 else [], awrites=[] if hf == 0 else [yB])
                    r0 = e * CAP + s * 128
                    DMA("sp", y_d[r0:r0 + 128, :], yy[:], reads=[yB], awrites=[YD])
            S.flush()

        if upto == 5:
            S.drain_dma()
            S.flush()
            raise _Stop(nc)
        with ExitStack() as p6:
            yk = [sb(p6, "yk%d" % i, [128, 4, 1024], F32) for i in range(3)]
            x1r = [sb(p6, "x1r%d" % i, [128, 1024], F32) for i in range(2)]
            accf = sb(p6, "accf", [128, 1024], F32)
            outt = [sb(p6, "outt%d" % i, [128, 1024], F32) for i in range(2)]
            gdT = sb(p6, "gdT", [32, 128], F32)
            bdn = sb(p6, "bdn", [32, 1024], F32)
            st6b = sb(p6, "st6b", [128, 2, 6], F32)
            mvb = sb(p6, "mvb", [128, 4], F32)
            DMA("sp", bdn[:], I["b_dn"], writes=[B("bdn")])
            lnp = sb(p6, "lnp6", [128, 4, 1024], F32)
            DMA("sp", lnp[:], I["lnp"], writes=[B("lnp")])
            for i in range(NT):
                yki, ykB = yk[i % 3], B("yk", i % 3)
                x1i, x1B = x1r[i % 2], B("x1r", i % 2)
                oi, oB = outt[i % 2], B("outt", i % 2)
                MS("pool", yki[:, 0, 0:1], 0.0, [], [ykB])
                for k in range(4):
                    S.dma("pool", lambda h, i=i, k=k, yki=yki: h.indirect_dma_start(
                        out=yki[:, k, :], out_offset=None, in_=y_d, in_offset=bass.IndirectOffsetOnAxis(ap=offs[:, i, k:k + 1], axis=0),
                        ), reads=[YD, B("offs", i)], awrites=[ykB])
                DMA("sp", x1i[:], x1f_d[i * 128:(i + 1) * 128, :], reads=[X1F], writes=[x1B])
                ACT(accf[:], x1i[:], AF.Copy, [x1B], [B("accf")], scale=DN_ALPHA)
                for k in range(4):
                    STT("dve", accf[:], yki[:, k, :], gk[:, i, k:k + 1], accf[:], ALU.mult, ALU.add, [ykB, B("gk", i, k), B("accf")], [B("accf")])
                S.op("pe", lambda h, i=i: h.transpose(out=PS[6][0:32, 0:128], in_=gd[:, i, :], identity=ident_f), [B("gd", i), CF], [PB[6]])
                ACT(gdT[:], PS[6][0:32, 0:128], AF.Copy, [PB[6]], [B("gdT")])
                for hf in range(2):
                    bb_ = 4 + hf
                    MM(PS[bb_][:, :], [(gdT[:], bdn[:, hf * 512:(hf + 1) * 512])], [B("gdT"), B("bdn")], [PB[bb_]])
                    TT("dve", accf[:, hf * 512:(hf + 1) * 512], accf[:, hf * 512:(hf + 1) * 512], PS[bb_][:, :], ALU.add, [B("accf"), PB[bb_]], [B("accf")])
                layer_norm(accf, B("accf"), 2, oi, oB, st6b, mvb, "b", lnp, ge="dve")
                DMA("sp", out[i * 128:(i + 1) * 128, :], oi[:], reads=[oB])
            S.drain_dma()
            S.flush()
    return nc


_CACHE = {}


def core_inputs(inp, sh, b, T, consts):
    m = dict(sh)
    m["xT"] = np.ascontiguousarray(inp["x"][b, :T].T)
    m["xtok"] = np.ascontiguousarray(inp["x"][b, :T])
    m["pos"] = np.ascontiguousarray(inp["positions"][b, :T].reshape(1, T).astype(np.int32))
    m["cf"], m["cb"] = consts
    return m


def kernel(**inputs):
    inp = {k: np.asarray(v) for k, v in inputs.items()}
    Bn, T = inp["x"].shape[0], inp["x"].shape[1]
    sh = prep_shared(inp)
    consts = make_consts(T)
    nc = build(T)
    in_maps = [core_inputs(inp, sh, b, T, consts) for b in range(Bn)]
    res = run_bass_kernel_spmd(nc, in_maps, core_ids=list(range(Bn)))
    out = np.stack([np.asarray(r["out"]) for r in res.results], 0).astype(np.float32)
    return out
```

```python
import os as _os
import numpy as np
from contextlib import ExitStack
import concourse.bass as bass
import concourse.mybir as mybir
from concourse.bass_utils import run_bass_kernel_spmd

F32 = mybir.dt.float32
BF16 = mybir.dt.bfloat16
I32 = mybir.dt.int32
AF = mybir.ActivationFunctionType
ALU = mybir.AluOpType
AX = mybir.AxisListType

D_MODEL = 1024
CONV_CH = 512
N_HEADS = 8
HEAD_DIM = 64
N_EXPERTS = 32
D_FF = 1024
ROPE_THETA = 500000.0
DN_ALPHA = 2.0 ** 0.25
LN_EPS = 1e-5
IN_COLS = 4888
NEGBIG = -30000.0


class Buf:
    __slots__ = ("name", "w", "r")

    def __init__(self, name):
        self.name = name
        self.w = {}
        self.r = {}


class Sched:
    def __init__(self, nc, stack, n_dma_slots=6):
        self.nc = nc
        self.names = ["pe", "dve", "act", "pool", "sp"]
        self.sem = {}
        self.cnt = {}
        self.seen = {}
        self.prog = {}
        for e in self.names:
            self.sem[e] = stack.enter_context(nc.semaphore("sem_" + e))
            self.cnt[e] = 0
            self.seen[e] = {}
            self.prog[e] = []
        self.dq = {}
        self.dqi = {}
        for q in ("sp", "pool", "act"):
            self.dq[q] = [
                {"sem": stack.enter_context(nc.semaphore("dsem_%s%d" % (q, i))), "total": 0, "key": "d%s%d" % (q, i)}
                for i in range(n_dma_slots)
            ]
            self.dqi[q] = 0

    def _waits(self, e, reads, writes, extra=(), awrites=()):
        waits = {}
        seen = self.seen[e]

        def need(key, sem, val):
            if e == "pe" and key == "pe":
                return
            if seen.get(key, 0) >= val:
                return
            if key not in waits or waits[key][1] < val:
                waits[key] = (sem, val)

        for b in reads:
            for key, (sem, val) in b.w.items():
                need(key, sem, val)
        for b in writes:
            for key, (sem, val) in b.w.items():
                need(key, sem, val)
            for key, (sem, val) in b.r.items():
                need(key, sem, val)
        for b in awrites:
            for key, (sem, val) in b.r.items():
                need(key, sem, val)
        for key, sem, val in extra:
            need(key, sem, val)
        for key, (sem, val) in waits.items():
            seen[key] = val
        return list(waits.values())

    def _record(self, key, tok, reads, writes, awrites=()):
        for b in reads:
            b.r[key] = tok
        for b in writes:
            b.w = {key: tok}
            b.r = {}
        for b in awrites:
            b.w[key] = tok

    def op(self, e, fn, reads=(), writes=(), inc=True, awrites=()):
        wl = self._waits(e, reads, writes, (), awrites)
        semE = self.sem[e]
        if inc:
            self.cnt[e] += 1
            tok = (semE, self.cnt[e])
        else:
            tok = (semE, self.cnt[e] + 1)

        def emit(h, wl=wl, fn=fn, inc=inc, semE=semE):
            for sem, val in wl:
                h.wait_ge(sem, val)
            ins = fn(h)
            if inc:
                ins.then_inc(semE, 1)

        self.prog[e].append(emit)
        self._record(e, tok, reads, writes, awrites)

    def dma(self, q, fn, reads=(), writes=(), awrites=()):
        slots = self.dq[q]
        slot = slots[self.dqi[q] % len(slots)]
        self.dqi[q] += 1
        extra = []
        if slot["total"] > 0:
            extra.append((slot["key"], slot["sem"], slot["total"]))
        wl = self._waits(q, reads, writes, extra, awrites)
        slot["total"] += 16
        tok = (slot["sem"], slot["total"])
        sem = slot["sem"]

        def emit(h, wl=wl, fn=fn, sem=sem):
            for s, val in wl:
                h.wait_ge(s, val)
            fn(h).then_inc(sem, 16)

        self.prog[q].append(emit)
        self._record(slot["key"], tok, reads, writes, awrites)

    def drain_dma(self):
        for q in ("sp", "pool", "act"):
            for slot in self.dq[q]:
                if slot["total"] > 0 and self.seen[q].get(slot["key"], 0) < slot["total"]:
                    self.seen[q][slot["key"]] = slot["total"]

                    def emit(h, sem=slot["sem"], val=slot["total"]):
                        h.wait_ge(sem, val)

                    self.prog[q].append(emit)

    def flush(self):
        nc = self.nc
        prog = self.prog
        with nc.Block() as block:

            @block.tensor
            def _(h):
                for f in prog["pe"]:
                    f(h)

            @block.vector
            def _(h):
                for f in prog["dve"]:
                    f(h)

            @block.scalar
            def _(h):
                for f in prog["act"]:
                    f(h)

            @block.gpsimd
            def _(h):
                for f in prog["pool"]:
                    f(h)

            @block.sync
            def _(h):
                for f in prog["sp"]:
                    f(h)

        for e in self.names:
            self.prog[e] = []


def dims(T):
    d = dict(T=T, NT=T // 128, NCH=T // 512, NCMP=T // 16 - 1, NSEL=T // 64, CAP=T // 8 + 128)
    d["NCT"] = (d["NCMP"] + 127) // 128
    d["NCAPT"] = d["CAP"] // 128
    d["WW"] = 4 * d["NT"] - 1
    return d


def make_consts(T):
    import ml_dtypes
    d = dims(T)
    NT, NCT, NCMP, NSEL, CAP, WW = d["NT"], d["NCT"], d["NCMP"], d["NSEL"], d["CAP"], d["WW"]
    p = np.arange(128)
    ident = np.eye(128, dtype=np.float32)
    D1 = (p[None, :] - 16 * p[:, None]).astype(np.float32)
    D2 = (p[None, :] - p[:, None]).astype(np.float32)
    r = p % 64
    invf = np.where(r < 16, ROPE_THETA ** (-(r % 8).astype(np.float32) * (2.0 / 16.0)), 0.0).astype(np.float32)
    sgn = np.where(r < 8, -1.0, 1.0).astype(np.float32)
    m = np.arange(WW)
    dd = m[None, :] - 2 * (NT - 1) - (p[:, None] >= 64)
    Wf = np.where(dd == 0, 2e4, np.where(dd == -1, 1e4, 0.0)).astype(np.float32)
    Wv = np.where(dd > 0, -1e30, 0.0).astype(np.float32)
    eoff = np.broadcast_to((np.arange(32) * CAP).astype(np.float32)[None, :], (128, 32))
    cf = np.concatenate([ident, D1, D2, invf[:, None], sgn[:, None], Wf, Wv, eoff], axis=1).astype(np.float32)
    E = np.zeros((128, NT, 128), np.float32)
    for kt in range(NT):
        for k in range(128):
            E[2 * kt + k // 64, kt, k] = 1.0
            E[64 + 2 * kt + k // 64, kt, k] = 1.0
    ov = np.zeros((128, NCT, 64), np.float32)
    for ct in range(NCT):
        for pp in range(128):
            c = ct * 128 + pp
            if c < NCMP:
                for j in range(min(NSEL, 64)):
                    if 16 * c < 64 * j + 64 and 16 * c + 32 > 64 * j:
                        ov[pp, ct, j] = 1.0
    U = (p[:, None] < p[None, :]).astype(np.float32)
    ones = np.ones((128, 128), np.float32)
    cb = np.concatenate([ident, E.reshape(128, -1), ov.reshape(128, -1), U, ones], axis=1).astype(ml_dtypes.bfloat16)
    return np.ascontiguousarray(cf), np.ascontiguousarray(cb)


def _swap_cols(w64):
    idx = np.arange(64)
    idx[:8] = np.arange(8, 16)
    idx[8:16] = np.arange(0, 8)
    return w64[:, idx]


def prep_shared(inp):
    w_in = inp["w_in"][0]
    c = np.cumsum([0, 512, 512, 512, 512, 128, 128, 128, 128, 128, 128, 24, 1024, 1024])
    xv, bg, cg, q = (w_in[:, c[i]:c[i + 1]] for i in range(4))
    kc, vc, ks, vs, kw, vw = (w_in[:, c[i]:c[i + 1]] for i in range(4, 10))
    ng, mga, mgb = (w_in[:, c[i]:c[i + 1]] for i in range(10, 13))
    chunks = []
    for cc in range(4):
        s = slice(cc * 128, (cc + 1) * 128)
        chunks += [xv[:, s], cg[:, s], bg[:, s]]
    for j in range(4):
        h0 = q[:, j * 64:(j + 1) * 64]
        h1 = q[:, (4 + j) * 64:(5 + j) * 64]
        chunks += [np.concatenate([h0, h1], 1), np.concatenate([_swap_cols(h0), _swap_cols(h1)], 1)]
    for k in (kc, ks, kw):
        chunks += [k, np.concatenate([_swap_cols(k[:, :64]), _swap_cols(k[:, 64:])], 1)]
    chunks += [vc]
    sh = {}
    sh["w_fm"] = np.ascontiguousarray(np.concatenate(chunks, 1))
    sh["w_tm"] = np.ascontiguousarray(np.concatenate([vs, vw, ng], 1))
    sh["w_mg"] = np.ascontiguousarray(np.concatenate([mga, mgb], 1))
    cw = inp["conv_w"][0][:, 0, :]
    sh["convw"] = np.ascontiguousarray(cw.reshape(3, 4, 128).transpose(2, 1, 0).reshape(128, 12))
    for n in ("k", "v"):
        pt = inp["cmp_pos_" + n][0].T
        sh["posT_" + n] = np.ascontiguousarray(np.concatenate([pt, pt], 0))
        sh["w1" + n] = np.ascontiguousarray(inp["cmp_w1_" + n][0])
    w2k = inp["cmp_w2_k"][0]
    sh["w2k"] = np.ascontiguousarray(np.concatenate([w2k, w2k], 1))
    sh["w2v"] = np.ascontiguousarray(inp["cmp_w2_v"][0])
    sh["w_upc"] = np.ascontiguousarray(inp["w_up_conv"][0])
    sh["w_upn"] = np.ascontiguousarray(inp["w_up_nsa"][0])
    sh["w_o"] = np.ascontiguousarray(inp["w_o"][0])
    lnp = np.stack([inp["ln1_g"][0], inp["ln1_b"][0], inp["ln2_g"][0], inp["ln2_b"][0]], 0)
    sh["lnp"] = np.ascontiguousarray(np.broadcast_to(lnp[None], (128, 4, 1024)))
    sh["w_r"] = np.ascontiguousarray(inp["w_router"][0])
    sh["b_r"] = np.ascontiguousarray(np.broadcast_to(inp["b_router"][0][None], (128, 32)))
    wgu = inp["w_gate_up"][0]
    sh["w_glu"] = np.ascontiguousarray(wgu[:, :, 0::2])
    sh["w_lin"] = np.ascontiguousarray(wgu[:, :, 1::2])
    sh["w_dn"] = np.ascontiguousarray(inp["w_down"][0])
    bgu = inp["b_gate_up"][0].reshape(32, 8, 128, 2)
    sh["bgu"] = np.ascontiguousarray(bgu.transpose(2, 0, 3, 1).reshape(128, 32 * 2 * 8))
    sh["b_dn"] = np.ascontiguousarray(inp["b_down"][0])
    return sh


INPUT_SPECS = None


def input_specs(T):
    d = dims(T)
    cf, cb = make_consts(T) if False else (None, None)
    ncf = 128 * 3 + 2 + 2 * d["WW"] + 32
    ncb = 128 + d["NT"] * 128 + d["NCT"] * 64 + 128 + 128
    return [
        ("xT", [1024, T], F32), ("xtok", [T, 1024], F32), ("pos", [1, T], I32),
        ("w_fm", [1024, 27 * 128], F32), ("w_tm", [1024, 280], F32), ("w_mg", [1024, 2048], F32),
        ("convw", [128, 12], F32), ("posT_k", [128, 32], F32), ("posT_v", [128, 32], F32),
        ("w1k", [2048, 256], F32), ("w1v", [2048, 256], F32), ("w2k", [256, 128], F32), ("w2v", [256, 64], F32),
        ("w_upc", [512, 1024], F32), ("w_upn", [512, 1024], F32), ("w_o", [1024, 1024], F32),
        ("lnp", [128, 4, 1024], F32), ("w_r", [1024, 32], F32), ("b_r", [128, 32], F32),
        ("w_glu", [32, 1024, 1024], F32), ("w_lin", [32, 1024, 1024], F32), ("w_dn", [32, 1024, 1024], F32),
        ("bgu", [128, 512], F32), ("b_dn", [32, 1024], F32),
        ("cf", [128, ncf], F32), ("cb", [128, ncb], BF16),
    ]


class _Stop(Exception):
    pass


def build(T, debug=False, upto=9):
    try:
        return _build(T, debug, upto)
    except _Stop as e:
        return e.args[0]


def _build(T, debug, upto):
    d = dims(T)
    NT, NCH, NCMP, NCT, NSEL, CAP, NCAPT, WW = (d[k] for k in ("NT", "NCH", "NCMP", "NCT", "NSEL", "CAP", "NCAPT", "WW"))
    TH = min(T, 1024)
    NH = T // TH
    nc = bass.Bass("TRN2", target_bir_lowering=False)
    I = {}
    for name, shape, dt in input_specs(T):
        I[name] = nc.dram_tensor(name, shape, dt, kind="ExternalInput").ap()
    out = nc.dram_tensor("out", [T, 1024], F32, kind="ExternalOutput").ap()
    x1f_d = nc.dram_tensor("x1f_scr", [T, 1024], F32, kind="Internal").ap()
    xg_d = nc.dram_tensor("xg_scr", [32 * CAP + 128, 1024], BF16, kind="Internal").ap()
    on_d = nc.dram_tensor("on_scr", [128, 4, T], BF16, kind="Internal").ap()
    y_d = nc.dram_tensor("y_scr", [32 * CAP + 128, 1024], F32, kind="Internal").ap()
    dbg = {}
    if debug:
        dbg["x1"] = nc.dram_tensor("dbg_x1", [T, 1024], F32, kind="ExternalOutput").ap()
        dbg["onsa"] = nc.dram_tensor("dbg_onsa", [T, 512], F32, kind="ExternalOutput").ap()
        dbg["lg"] = nc.dram_tensor("dbg_lg", [T, 32], F32, kind="ExternalOutput").ap()
        dbg["nsel"] = nc.dram_tensor("dbg_nsel", [T, 2, NSEL], F32, kind="ExternalOutput").ap()

    bufs = {}

    def B(*key):
        if key not in bufs:
            bufs[key] = Buf(str(key))
        return bufs[key]

    with ExitStack() as top:
        S = Sched(nc, top)

        def TT(e, out, in0, in1, op, reads, writes, **kw):
            S.op(e, lambda h: h.tensor_tensor(out=out, in0=in0, in1=in1, op=op), reads, writes, **kw)

        def TS(e, out, in0, s1, s2, op0, op1, reads, writes, **kw):
            if op1 is None:
                S.op(e, lambda h: h.tensor_scalar(out=out, in0=in0, scalar1=s1, scalar2=None, op0=op0), reads, writes, **kw)
            else:
                S.op(e, lambda h: h.tensor_scalar(out=out, in0=in0, scalar1=s1, scalar2=s2, op0=op0, op1=op1), reads, writes, **kw)

        def STT(e, out, in0, sc, in1, op0, op1, reads, writes, **kw):
            S.op(e, lambda h: h.scalar_tensor_tensor(out=out, in0=in0, scalar=sc, in1=in1, op0=op0, op1=op1), reads, writes, **kw)

        def ACT(out, in_, func, reads, writes, scale=1.0, bias=None, **kw):
            if bias is None:
                S.op("act", lambda h: h.activation(out=out, in_=in_, func=func, scale=scale), reads, writes, **kw)
            else:
                S.op("act", lambda h: h.activation(out=out, in_=in_, func=func, scale=scale, bias=bias), reads, writes, **kw)

        def CP(e, out, in_, reads, writes, **kw):
            S.op(e, lambda h: h.tensor_copy(out=out, in_=in_), reads, writes, **kw)

        def MS(e, ap, val, reads, writes, **kw):
            S.op(e, lambda h: h.memset(ap, val), reads, writes, **kw)

        def RCP(out, in_, reads, writes):
            S.op("dve", lambda h: h.reciprocal(out=out, in_=in_), reads, writes)

        def DMA(q, out, in_, reads=(), writes=(), awrites=()):
            S.dma(q, lambda h: h.dma_start(out=out, in_=in_), reads, writes, awrites)

        def MM(bank_ap, pairs, reads, writes, first=True, last=True):
            n = len(pairs)

            def fn(h):
                ins = None
                for k, (l, r) in enumerate(pairs):
                    ins = h.matmul(bank_ap, lhsT=l, rhs=r, start=(first and k == 0), stop=(last and k == n - 1))
                return ins
            S.op("pe", fn, reads, writes)

        def sb(stack, name, shape, dt):
            return stack.enter_context(nc.sbuf_tensor("s_" + name, shape, dt))

        PS = [top.enter_context(nc.psum_tensor("ps%d" % i, [128, 512], F32)) for i in range(8)]
        PB = [B("ps", i) for i in range(8)]

        NCF = I["cf"].shape[1]
        NCB = I["cb"].shape[1]
        cf = sb(top, "cf", [128, NCF], F32)
        cb = sb(top, "cb", [128, NCB], BF16)
        ident_f = cf[:, 0:128]
        D1 = cf[:, 128:256]
        D2 = cf[:, 256:384]
        invf = cf[:, 384:385]
        sgn = cf[:, 385:386]
        Wf = cf[:, 386:386 + WW]
        Wv = cf[:, 386 + WW:386 + 2 * WW]
        eoff = cf[:, 386 + 2 * WW:386 + 2 * WW + 32]
        ident_b = cb[:, 0:128]
        o_ = 128
        Ecb = cb[:, o_:o_ + NT * 128].rearrange("p (a b) -> p a b", a=NT)
        o_ += NT * 128
        ovc = cb[:, o_:o_ + NCT * 64].rearrange("p (a b) -> p a b", a=NCT)
        o_ += NCT * 64
        Ust = cb[:, o_:o_ + 128]
        o_ += 128
        ones_b = cb[:, o_:o_ + 128]
        CF, CB = B("cf"), B("cb")
        DMA("sp", cf[:], I["cf"], writes=[CF])
        DMA("sp", cb[:], I["cb"], writes=[CB])

        epsc = sb(top, "epsc", [128, 1], F32)
        MS("pool", epsc[:], LN_EPS, [], [B("epsc")])
        offs = sb(top, "offs", [128, NT, 4], I32)
        gk = sb(top, "gk", [128, NT, 4], F32)
        gd = sb(top, "gd", [128, NT, 32], F32)
        mid = ExitStack()
        u2T = sb(mid, "u2T", [128, 4, T], BF16)

        def v4(ap, a=4):
            return ap.rearrange("p (a b) -> p a b", a=a)

        with ExitStack() as att:
            qT = sb(att, "qT", [128, 4, T], BF16)
            kT = [sb(att, "kT0", [128, T], BF16), None, sb(att, "kT2", [128, T], BF16)]
            kA = [sb(att, "kA%d" % i, [128, T], BF16) for i in range(2)]
            DMA("sp", kA[0][64:128, :], I["cb"][64:128, 128:128 + T], awrites=[B("kAE", 0)])
            DMA("sp", kA[1][0:64, :], I["cb"][0:64, 128:128 + T], awrites=[B("kAE", 1)])
            vcT = sb(att, "vcT", [128, T], BF16)
            Vext = [sb(att, "Vext%d" % i, [128, NT, 2, 65], BF16) for i in range(2)]
            gates = sb(att, "gates", [128, NT, 24], F32)
            kcT = sb(att, "kcT", [128, NCT * 128], BF16)
            Vc = sb(att, "Vc", [128, NCT, 2, 65], BF16)
            MS("pool", Vext[0][:, :, :, 64:65], 1.0, [], [B("Vones", 0)])
            MS("pool", Vext[1][:, :, :, 64:65], 1.0, [], [B("Vones", 1)])
            MS("pool", Vc[:, :, :, 64:65], 1.0, [], [B("Vcones")])
            MS("pool", kcT[:], 0.0, [], [B("kcT")])

            with ExitStack() as p1:
                xTb = sb(p1, "xTb", [128, 8, TH], BF16)
                wbuf = [sb(p1, "wbuf%d" % i, [128, 8, 512], BF16) for i in range(2)]
                wtm = sb(p1, "wtm", [128, 8, 280], BF16)
                cosT = sb(p1, "cosT", [128, TH], F32)
                sinT = sb(p1, "sinT", [128, TH], F32)
                posi = sb(p1, "posi", [128, 512], I32)
                ang = sb(p1, "ang", [128, 512], F32)
                angi = sb(p1, "angi", [128, 512], I32)
                angf = sb(p1, "angf", [128, 512], F32)
                ut = [sb(p1, "ut%d" % i, [128, 514], F32) for i in range(2)]
                csb = [sb(p1, "csb%d" % i, [128, 512], F32) for i in range(2)]
                c1 = [sb(p1, "c1_0", [128, 512], F32)] * 2
                hal = sb(p1, "hal", [128, 4, 2], F32)
                cw = sb(p1, "cw", [128, 12], F32)
                rt1 = [sb(p1, "rt1_0", [128, 512], F32)] * 2
                rt2 = [sb(p1, "rt2_0", [128, 512], F32)] * 2
                DMA("sp", cw[:], I["convw"], writes=[B("cw")])
                MS("pool", hal[:], 0.0, [], [B("hal", cc) for cc in range(4)])
                DMA("pool", wtm[:], I["w_tm"].rearrange("(k p) n -> p k n", p=128), writes=[B("wtm")])

                groups = [[0, 1, 2], [3, 4, 5], [6, 7, 8], [9, 10, 11], [12, 13, 14, 15], [16, 17, 18, 19], [20, 21, 22, 23], [24, 25, 26]]
                gl = 0
                bank_rr = [0]

                def take_banks(n):
                    r = [(bank_rr[0] + i) % 6 for i in range(n)]
                    bank_rr[0] = (bank_rr[0] + n) % 6
                    return r

                ui = 0
                for hf in range(NH):
                    h0 = hf * TH
                    DMA("pool", xTb[:, :, :], I["xT"][:, h0:h0 + TH].rearrange("(k p) t -> p k t", p=128), writes=[B("xTb")])
                    for rc_ in range(TH // 512):
                        rs = slice(rc_ * 512, (rc_ + 1) * 512)
                        DMA("sp", posi[:], I["pos"][:, h0 + rc_ * 512:h0 + (rc_ + 1) * 512].to_broadcast([128, 512]), writes=[B("posi")])
                        CP("dve", ang[:], posi[:], [B("posi")], [B("ang")])
                        TS("dve", ang[:], ang[:], invf, float(1.0 / (2 * np.pi)), ALU.mult, ALU.mult, [B("ang"), CF], [B("ang")])
                        for which, tab in ((0, sinT), (1, cosT)):
                            if which == 1:
                                TS("dve", ang[:], ang[:], 0.25, None, ALU.add, None, [B("ang")], [B("ang")])
                            CP("dve", angi[:], ang[:], [B("ang")], [B("angi")])
                            CP("dve", angf[:], angi[:], [B("angi")], [B("angf")])
                            TT("dve", angf[:], ang[:], angf[:], ALU.subtract, [B("ang"), B("angf")], [B("angf")])
                            STT("dve", angf[:], angf[:], 0.5, angf[:], ALU.is_gt, ALU.subtract, [B("angf")], [B("angf")])
                            ACT(tab[:, rs], angf[:], AF.Sin, [B("angf")], [B("tab", which)], scale=float(-2 * np.pi))
                    TS("dve", sinT[:], sinT[:], sgn, None, ALU.mult, None, [B("tab", 0), CF], [B("tab", 0)])
                    if upto == 0.1:
                        S.drain_dma()
                        S.flush()
                        raise _Stop(nc)

                    for grp in groups:
                        wb = wbuf[gl % 2]
                        wB = B("wbuf", gl % 2)
                        gl += 1
                        ncols = len(grp) * 128
                        c0 = grp[0] * 128
                        DMA("pool", wb[:, :, 0:ncols], I["w_fm"][:, c0:c0 + ncols].rearrange("(k p) n -> p k n", p=128), writes=[wB])
                        for tcl in range(TH // 512):
                            t0 = h0 + tcl * 512
                            tl = tcl * 512
                            tc = t0 // 512
                            banks = take_banks(3) if len(grp) == 3 else take_banks(4)
                            for ci, ch in enumerate(grp):
                                bk = banks[ci]
                                MM(PS[bk][:, :], [(wb[:, k, ci * 128:(ci + 1) * 128], xTb[:, k, tl:tl + 512]) for k in range(8)], [wB, B("xTb")], [PB[bk]])
                            if grp[0] < 12:
                                cc = grp[0] // 3
                                bx, bc, bb = banks
                                u, uB = ut[ui % 2], B("ut", ui % 2)
                                cs, csB = csb[ui % 2], B("csb", ui % 2)
                                cc1, c1B = c1[0], B("c1", 0)
                                ui += 1
                                ACT(cs[:], PS[bc][:, :], AF.Copy, [PB[bc]], [csB])
                                CP("pool", u[:, 0:2], hal[:, cc, :], [B("hal", cc)], [uB])
                                TT("dve", u[:, 2:514], PS[bx][:, :], cs[:], ALU.mult, [PB[bx], csB, uB], [uB])
                                CP("pool", hal[:, cc, :], u[:, 512:514], [uB], [B("hal", cc)])
                                TS("dve", cc1[:], u[:, 2:514], cw[:, cc * 3 + 2:cc * 3 + 3], None, ALU.mult, None, [uB, B("cw")], [c1B])
                                STT("dve", cc1[:], u[:, 1:513], cw[:, cc * 3 + 1:cc * 3 + 2], cc1[:], ALU.mult, ALU.add, [uB, c1B], [c1B])
                                STT("dve", cc1[:], u[:, 0:512], cw[:, cc * 3:cc * 3 + 1], cc1[:], ALU.mult, ALU.add, [uB, c1B], [c1B])
                                TT("dve", u2T[:, cc, t0:t0 + 512], PS[bb][:, :], cc1[:], ALU.mult, [PB[bb], c1B], [B("u2T", cc, tc)])
                            else:
                                ci = 0
                                while ci < len(grp):
                                    ch = grp[ci]
                                    if ch == 26:
                                        bk = banks[ci]
                                        ACT(vcT[:, t0:t0 + 512], PS[bk][:, :], AF.Copy, [PB[bk]], [B("vcT", tc)])
                                        ci += 1
                                        continue
                                    bq, bs = banks[ci], banks[ci + 1]
                                    if ch < 20:
                                        j = (ch - 12) // 2
                                        dten, dB = qT[:, j, :], B("qT", j, tc)
                                    else:
                                        br = (ch - 20) // 2
                                        dten, dB = (kT[br][:, :] if br != 1 else None), B("kT", br, tc)
                                    r1, r2, rB = rt1[0], rt2[0], B("rt", 0)
                                    ui += 1
                                    TT("dve", r1[:], PS[bq][:, :], cosT[:, tl:tl + 512], ALU.mult, [PB[bq], B("tab", 1)], [rB])
                                    TT("dve", r2[:], PS[bs][:, :], sinT[:, tl:tl + 512], ALU.mult, [PB[bs], B("tab", 0), rB], [rB])
                                    if dten is None:
                                        TT("pool", kA[0][0:64, t0:t0 + 512], r1[0:64, :], r2[0:64, :], ALU.add, [rB], [dB])
                                        TT("pool", kA[1][64:128, t0:t0 + 512], r1[64:128, :], r2[64:128, :], ALU.add, [rB, dB], [], awrites=[dB])
                                    else:
                                        TT("pool", dten[:, t0:t0 + 512], r1[:], r2[:], ALU.add, [rB], [dB])
                                    ci += 2
                        if upto == 0.2 and grp[0] == 0:
                            S.drain_dma()
                            S.flush()
                            raise _Stop(nc)
                        if upto == 0.25 and grp[0] == 12:
                            S.drain_dma()
                            S.flush()
                            raise _Stop(nc)
                        if upto == 0.3 and grp[0] == 24:
                            S.drain_dma()
                            S.flush()
                            raise _Stop(nc)
                    for il in range(TH // 128):
                        i = h0 // 128 + il
                        bk = 6 + (i % 2)
                        MM(PS[bk][:, 0:280], [(xTb[:, k, il * 128:(il + 1) * 128], wtm[:, k, :]) for k in range(8)], [B("wtm"), B("xTb")], [PB[bk]])
                        for vi in range(2):
                            ACT(Vext[vi][:, i, :, 0:64], v4(PS[bk][:, vi * 128:(vi + 1) * 128], 2), AF.Copy, [PB[bk], B("Vones", vi)], [B("Vext", vi, i)])
                        ACT(gates[:, i, :], PS[bk][:, 256:280], AF.Sigmoid, [PB[bk]], [B("gates", i)])
                S.flush()

            if upto == 1:
                S.drain_dma()
                S.flush()
                raise _Stop(nc)
            with ExitStack() as p2:
                w1 = {n: sb(p2, "w1" + n, [128, 32, 256], BF16) for n in "kv"}
                posT = {n: sb(p2, "posT" + n, [128, 32], BF16) for n in "kv"}
                w2k = sb(p2, "w2k", [128, 2, 128], BF16)
                w2v = sb(p2, "w2v", [128, 2, 64], BF16)
                cst = sb(p2, "cst", [128, 4], F32)
                hsb = sb(p2, "hsb", [128, 256], F32)
                h2 = sb(p2, "h2", [128, 256], F32)
                gT = {(n, g): sb(p2, "gT%s%d" % (n, g), [128, 2, 256], BF16) for n in "kv" for g in range(2)}
                for n in "kv":
                    for half in range(2):
                        DMA("pool", w1[n][half * 64:(half + 1) * 64, :, :], I["w1" + n].rearrange("(l d) h -> d l h", d=64), awrites=[B("w1", n)])
                    DMA("pool", posT[n][:], I["posT_" + n], writes=[B("posT", n)])
                DMA("pool", w2k[:], I["w2k"].rearrange("(c p) n -> p c n", p=128), writes=[B("w2k")])
                DMA("pool", w2v[:], I["w2v"].rearrange("(c p) n -> p c n", p=128), writes=[B("w2v")])
                bi = 0
                for ni, n in enumerate("kv"):
                    for hc in range(2):
                        bk = bi % 6
                        bi += 1
                        MM(PS[bk][:, 0:1], [(w1[n][0:64, l, hc * 128:(hc + 1) * 128], posT[n][0:64, l:l + 1]) for l in range(32)], [B("w1", n), B("posT", n)], [PB[bk]])
                        ACT(cst[:, ni * 2 + hc:ni * 2 + hc + 1], PS[bk][:, 0:1], AF.Copy, [PB[bk]], [B("cst", ni, hc)])
                for ni, n in enumerate("kv"):
                    src = kT[0] if n == "k" else vcT
                    srcB = [B("kT", 0, tc) for tc in range(NCH)] if n == "k" else [B("vcT", tc) for tc in range(NCH)]
                    for g in range(2):
                        for hc in range(2):
                            bk = bi % 6
                            bi += 1
                            MM(PS[bk][:, 0:NCMP], [(w1[n][64 * g:64 * g + 64, l, hc * 128:(hc + 1) * 128], src[64 * g:64 * g + 64, l:l + 16 * (NCMP - 1) + 1:16]) for l in range(32)],
                               [B("w1", n)] + srcB, [PB[bk]])
                            hs, hh2 = hsb[:, 0:NCMP], h2[:, 0:NCMP]
                            ACT(hs, PS[bk][:, 0:NCMP], AF.Identity, [PB[bk], B("cst", ni, hc)], [B("hsb")], bias=cst[:, ni * 2 + hc:ni * 2 + hc + 1])
                            TT("dve", hh2, hs, hs, ALU.mult, [B("hsb")], [B("h2")])
                            TS("dve", hh2, hh2, 0.044715, 1.0, ALU.mult, ALU.add, [B("h2")], [B("h2")])
                            TT("dve", hh2, hh2, hs, ALU.mult, [B("h2"), B("hsb")], [B("h2")])
                            ACT(hh2, hh2, AF.Sigmoid, [B("h2")], [B("h2")], scale=1.5957691216057308)
                            TT("dve", gT[(n, g)][:, hc, 0:NCMP], hh2, hs, ALU.mult, [B("h2"), B("hsb")], [B("gT", n, g, hc)])
                for g in range(2):
                    bk = bi % 6
                    bi += 1
                    MM(PS[bk][:, 0:NCMP], [(w2k[:, hc, :], gT[("k", g)][:, hc, 0:NCMP]) for hc in range(2)], [B("w2k"), B("gT", "k", g, 0), B("gT", "k", g, 1)], [PB[bk]])
                    ACT(kcT[64 * g:64 * g + 64, 0:NCMP], PS[bk][64 * g:64 * g + 64, 0:NCMP], AF.Copy, [PB[bk], B("kcT")], [], awrites=[B("kcT")])
                for g in range(2):
                    for ct in range(NCT):
                        cn = min(128, NCMP - ct * 128)
                        bk = bi % 6
                        bi += 1
                        MM(PS[bk][0:cn, 0:64], [(gT[("v", g)][:, hc, ct * 128:ct * 128 + cn], w2v[:, hc, :]) for hc in range(2)], [B("w2v"), B("gT", "v", g, 0), B("gT", "v", g, 1)], [PB[bk]])
                        ACT(Vc[0:cn, ct, g, 0:64], PS[bk][0:cn, 0:64], AF.Copy, [PB[bk], B("Vcones")], [], awrites=[B("Vc")])
                S.flush()

            if upto == 2:
                S.drain_dma()
                S.flush()
                raise _Stop(nc)
            with ExitStack() as p3:
                NPR = NT + 5 + 2
                Pring = sb(p3, "Pring", [128, NPR, 512], BF16)
                nselT = sb(p3, "nselT", [128, 4, 128], BF16)
                onst = [sb(p3, "onst%d" % i, [128, 4, 128], BF16) for i in range(2)]
                nsel = sb(p3, "nsel", [128, 128], BF16)
                MS("pool", nsel[:], 0.0, [], [B("nsel")])
                otok = sb(p3, "otok", [128, 512], BF16)
                acc = sb(p3, "acc", [128, 4, 64], F32)
                tmp3 = sb(p3, "tmp3", [128, 4, 64], F32)
                rc = sb(p3, "rc", [128, 3, 4], F32)
                impsb = sb(p3, "impsb", [128, 64], F32)
                scr2 = sb(p3, "scr2", [128, 64], F32)
                mx = sb(p3, "mx", [128, 16], F32)
                dbgt = sb(p3, "dbgt", [128, 512], F32) if debug else None
                D1b = D1.unsqueeze(1).to_broadcast([128, 4, 128])
                D2b = D2.unsqueeze(1).to_broadcast([128, 4, 128])
                pti = [0]
                sbank = [0]
                BOC, BIMP, BOS, BOW, BTR = 3, 4, 5, 6, 7
                trb = PS[BTR][:, :].bitcast(BF16)

                def score_tile(i, g, M, lhs_ap, lhsB, mask=None, extra=None, extraB=(), slot=0, rhs=None):
                    bk = sbank[0] % 3
                    sbank[0] += 1
                    P = Pring[:, slot, :]
                    PBf = B("Pt", slot)
                    pairs = [(lhs_ap, qT[64 * g:64 * g + 64, :, i * 128:(i + 1) * 128] if rhs is None else rhs)]
                    if extra is not None:
                        pairs.append(extra)
                    MM(v4(PS[bk][0:M, :]), pairs, list(lhsB) + [B("qT", j, i // 4) for j in range(4)] + list(extraB), [PB[bk]])
                    ACT(P[0:M, :], PS[bk][0:M, :], AF.Exp, [PB[bk]], [PBf], scale=0.125)
                    if mask is not None:
                        Db, cmp_op, thr = mask
                        STT("dve", v4(P[0:M, :]), Db[0:M], float(thr), v4(P[0:M, :]), cmp_op, ALU.mult, [PBf, CF], [PBf])
                    return P, PBf

                def pv(tiles, bank, ncol=65):
                    n = len(tiles)

                    def fn(h):
                        ins = None
                        for hh in range(4):
                            for k, (P, PBf, M, rhs_ap, rhsB) in enumerate(tiles):
                                ins = h.matmul(PS[bank][:, hh * ncol:(hh + 1) * ncol], lhsT=P[0:M, hh * 128:(hh + 1) * 128], rhs=rhs_ap, start=(k == 0), stop=(k == n - 1))
                        return ins
                    rd = []
                    for (P, PBf, M, rhs_ap, rhsB) in tiles:
                        rd.append(PBf)
                        rd += list(rhsB)
                    S.op("pe", fn, rd, [PB[bank]])

                accs = [acc, sb(p3, "acc_b", [128, 4, 64], F32)]
                nselTs = [nselT, sb(p3, "nselT_b", [128, 4, 128], BF16)]
                tmp3b = sb(p3, "tmp3b", [128, 4, 64], F32)
                NI = min(NT, int(_os.environ.get('K_MAXI', '9999')))
                units = [(i, g) for i in range(NI) for g in range(2)]

                def stageA(i, g, u):
                    q0 = i * 128
                    gs = slice(64 * g, 64 * g + 64)
                    accu, accB = accs[u % 2], B("acc", u % 2)
                    nsT, nsTB = nselTs[u % 2], B("nselT", u % 2)
                    cts = []
                    for ct in range(NCT):
                        cn = min(128, NCMP - ct * 128)
                        M = min(cn, 8 * i + 7 - 128 * ct)
                        if M > 0:
                            cts.append((ct, M))
                    tl_c, tl_i = [], []
                    for idx, (ct, M) in enumerate(cts):
                        full = 16 * (ct * 128 + M - 1) + 31 <= q0
                        mask = None if full else (D1b, ALU.is_ge, 31 + 16 * 128 * ct - q0)
                        P, PBf = score_tile(i, g, M, kcT[gs, ct * 128:ct * 128 + M], [B("kcT")], mask, slot=NT + 5 + idx)
                        tl_c.append((P, PBf, M, Vc[0:M, ct, g, :], [B("Vc"), B("Vcones")]))
                        tl_i.append((P, PBf, M, ovc[0:M, ct, 0:NSEL], [CB]))
                    pv(tl_c, BOC)
                    pv(tl_i, BIMP, ncol=NSEL)
                    kts = list(range(max(0, i - 4), i + 1))
                    tl_w = []
                    for wi, kt in enumerate(kts):
                        mask = None
                        if kt == i:
                            mask = (D2b, ALU.is_ge, 0.0)
                        elif kt == i - 4:
                            mask = (D2b, ALU.is_le, -1.0)
                        P, PBf = score_tile(i, g, 128, kT[2][gs, kt * 128:(kt + 1) * 128], [B("kT", 2, kt // 4)], mask, slot=NT + wi)
                        tl_w.append((P, PBf, 128, Vext[1][:, kt, g, :], [B("Vext", 1, kt), B("Vones", 1)]))
                    pv(tl_w, BOW)
                    ocv = v4(PS[BOC][:, 0:260])
                    imp = impsb[:, 0:NSEL]
                    TS("dve", rc[:, 0, :], ocv[:, :, 64], 1e-30, None, ALU.max, None, [PB[BOC]], [B("rc", 0)])
                    RCP(rc[:, 0, :], rc[:, 0, :], [B("rc", 0)], [B("rc", 0)])
                    TS("dve", imp, PS[BIMP][:, 0:NSEL], rc[:, 0, 0:1], None, ALU.mult, None, [PB[BIMP], B("rc", 0)], [B("imp")])
                    for hh in range(1, 4):
                        STT("dve", imp, PS[BIMP][:, hh * NSEL:(hh + 1) * NSEL], rc[:, 0, hh:hh + 1], imp, ALU.mult, ALU.add, [PB[BIMP], B("rc", 0), B("imp")], [B("imp")])
                    w0 = 2 * (NT - 1) - 2 * i
                    TT("dve", imp, imp, Wf[:, w0:w0 + NSEL], ALU.max, [B("imp"), CF], [B("imp")])
                    TT("dve", imp, imp, Wv[:, w0:w0 + NSEL], ALU.add, [B("imp"), CF], [B("imp")])
                    MS("dve", impsb[:, 0:1], 3e4, [B("imp")], [B("imp")])
                    S.op("dve", lambda h: h.max(out=mx[:, 0:8], in_=imp), [B("imp")], [B("mx")])
                    S.op("dve", lambda h: h.match_replace(out=scr2[:, 0:NSEL], in_to_replace=mx[:, 0:8], in_values=imp, imm_value=-3e38), [B("imp"), B("mx")], [B("scr2")])
                    S.op("dve", lambda h: h.max(out=mx[:, 8:16], in_=scr2[:, 0:NSEL]), [B("scr2"), B("mx")], [B("mx")])
                    TS("dve", nsel[:, 0:NSEL], imp, mx[:, 15:16], NEGBIG, ALU.is_lt, ALU.mult, [B("imp"), B("mx")], [B("nsel")])
                    TS("dve", nsel[:, 64:64 + NSEL], imp, mx[:, 15:16], NEGBIG, ALU.is_lt, ALU.mult, [B("imp"), B("mx"), B("nsel")], [], awrites=[B("nsel")])
                    if debug:
                        CP("dve", dbgt[:, 0:NSEL], nsel[:, 0:NSEL], [B("nsel")], [B("dbgt")])
                        DMA("sp", dbg["nsel"][i * 128:(i + 1) * 128, g, :], dbgt[:, 0:NSEL], reads=[B("dbgt")])
                    S.op("pe", lambda h: h.transpose(out=trb[:, 0:128], in_=nsel[:, :], identity=ident_b), [B("nsel"), CB], [PB[BTR]])
                    og = 64 * (1 - g)
                    ACT(nsT[og:og + 64, :, :], trb[og:og + 64, 0:128].unsqueeze(1).to_broadcast([64, 4, 128]), AF.Copy, [PB[BTR]], [nsTB])
                    CP("pool", nsT[gs, :, :], qT[gs, :, i * 128:(i + 1) * 128], [B("qT", j, i // 4) for j in range(4)] + [nsTB], [], awrites=[nsTB])
                    TT("dve", rc[:, 0, :], rc[:, 0, :], gates[:, i, g * 4:g * 4 + 4], ALU.mult, [B("rc", 0), B("gates", i)], [B("rc", 0)])
                    TT("dve", accu[:], ocv[:, :, 0:64], rc[:, 0, :].unsqueeze(2).to_broadcast([128, 4, 64]), ALU.mult, [PB[BOC], B("rc", 0)], [accB])
                    owv = v4(PS[BOW][:, 0:260])
                    TS("dve", rc[:, 2, :], owv[:, :, 64], 1e-30, None, ALU.max, None, [PB[BOW]], [B("rc", 2)])
                    RCP(rc[:, 2, :], rc[:, 2, :], [B("rc", 2)], [B("rc", 2)])
                    TT("dve", rc[:, 2, :], rc[:, 2, :], gates[:, i, 16 + g * 4:16 + g * 4 + 4], ALU.mult, [B("rc", 2), B("gates", i)], [B("rc", 2)])
                    TT("dve", tmp3[:], owv[:, :, 0:64], rc[:, 2, :].unsqueeze(2).to_broadcast([128, 4, 64]), ALU.mult, [PB[BOW], B("rc", 2)], [B("tmp3")])
                    TT("dve", accu[:], accu[:], tmp3[:], ALU.add, [accB, B("tmp3")], [accB])

                def stageB(i, g, u):
                    gs = slice(64 * g, 64 * g + 64)
                    accu, accB = accs[u % 2], B("acc", u % 2)
                    nsT, nsTB = nselTs[u % 2], B("nselT", u % 2)
                    tl_s = []
                    for kt in range(i + 1):
                        mask = (D2b, ALU.is_ge, 0.0) if kt == i else None
                        P, PBf = score_tile(i, g, 128, kA[g][:, kt * 128:(kt + 1) * 128], [B("kT", 1, kt // 4), B("kAE", g)], mask,
                                            extraB=[nsTB], slot=kt, rhs=nsT[:, :, :])
                        tl_s.append((P, PBf, 128, Vext[0][:, kt, g, :], [B("Vext", 0, kt), B("Vones", 0)]))
                    pv(tl_s, BOS)
                    osv = v4(PS[BOS][:, 0:260])
                    TS("dve", rc[:, 1, :], osv[:, :, 64], 1e-30, None, ALU.max, None, [PB[BOS]], [B("rc", 1)])
                    RCP(rc[:, 1, :], rc[:, 1, :], [B("rc", 1)], [B("rc", 1)])
                    TT("dve", rc[:, 1, :], rc[:, 1, :], gates[:, i, 8 + g * 4:8 + g * 4 + 4], ALU.mult, [B("rc", 1), B("gates", i)], [B("rc", 1)])
                    TT("dve", tmp3b[:], osv[:, :, 0:64], rc[:, 1, :].unsqueeze(2).to_broadcast([128, 4, 64]), ALU.mult, [PB[BOS], B("rc", 1)], [B("tmp3b")])
                    TT("dve", v4(otok[:, g * 256:(g + 1) * 256]), accu[:], tmp3b[:], ALU.add, [accB, B("tmp3b")], [B("otok", g)])
                    if g == 1:
                        def trs(h):
                            ins = None
                            for c4 in range(4):
                                ins = h.transpose(out=trb[:, c4 * 128:(c4 + 1) * 128], in_=otok[:, c4 * 128:(c4 + 1) * 128], identity=ident_b)
                            return ins
                        S.op("pe", trs, [B("otok", 0), B("otok", 1), CB], [PB[BTR]])
                        ACT(onst[i % 2][:], v4(trb[:, 0:512]), AF.Copy, [PB[BTR]], [B("onst", i % 2)])
                        DMA("sp", on_d[:, :, i * 128:(i + 1) * 128], onst[i % 2][:], reads=[B("onst", i % 2)], awrites=[B("on_d")])
                        if debug:
                            CP("dve", dbgt[:], otok[:], [B("otok", 0), B("otok", 1)], [B("dbgt")])
                            DMA("sp", dbg["onsa"][i * 128:(i + 1) * 128, :], dbgt[:], reads=[B("dbgt")])

                for u in range(len(units) + 1):
                    if u < len(units):
                        stageA(units[u][0], units[u][1], u)
                    if u >= 1:
                        stageB(units[u - 1][0], units[u - 1][1], u - 1)
                S.flush()

        if upto == 3:
            S.drain_dma()
            S.flush()
            raise _Stop(nc)
        XG, YD, X1F = B("xg_d"), B("y_d"), B("x1f_d")

        def layer_norm(pre, preB, gi, dst, dstB, st6, mv, sfx, lnp, ge="pool"):
            for hf in range(2):
                S.op("dve", lambda h, hf=hf: h.bn_stats(out=st6[:, hf, :], in_=pre[:, hf * 512:(hf + 1) * 512]), [preB], [B("st6" + sfx, hf)])
            S.op("dve", lambda h: h.bn_aggr(out=mv[:, 0:2], in_=st6[:, :, :].rearrange("p a b -> p (a b)")), [B("st6" + sfx, 0), B("st6" + sfx, 1)], [B("mv" + sfx)])
            ACT(mv[:, 2:3], mv[:, 1:2], AF.Sqrt, [B("mv" + sfx)], [B("mv" + sfx)], bias=epsc[:, 0:1])
            RCP(mv[:, 2:3], mv[:, 2:3], [B("mv" + sfx)], [B("mv" + sfx)])
            STT("dve", mv[:, 3:4], mv[:, 0:1], -1.0, mv[:, 2:3], ALU.mult, ALU.mult, [B("mv" + sfx)], [B("mv" + sfx)])
            ACT(pre[:], pre[:], AF.Identity, [preB, B("mv" + sfx)], [preB], scale=mv[:, 2:3], bias=mv[:, 3:4])
            TT(ge, pre[:], pre[:], lnp[:, gi, :], ALU.mult, [preB, B("lnp")], [preB])
            TT(ge, dst[:], pre[:], lnp[:, gi + 1, :], ALU.add, [preB, B("lnp")], [dstB])

        with ExitStack() as p4:
            wmg = sb(p4, "wmg", [128, 8, 2048], BF16)
            wupc = sb(p4, "wupc", [128, 4, 1024], BF16)
            wupn = sb(p4, "wupn", [128, 4, 1024], BF16)
            wo = sb(p4, "wo", [128, 8, 1024], BF16)
            wr = sb(p4, "wr", [128, 8, 32], F32)
            br_ = sb(p4, "br_", [128, 32], F32)
            xTc = [sb(p4, "xTc%d" % i, [128, 8, 512], BF16) for i in range(2)]
            oncs = [sb(p4, "onc%d" % i, [128, 4, 512], BF16) for i in range(2)]
            lnp = sb(p4, "lnp", [128, 2, 1024], F32)
            DMA("sp", lnp[:], I["lnp"][:, 0:2, :], writes=[B("lnp")])
            xt = [sb(p4, "xt%d" % i, [128, 1024], F32) for i in range(2)]
            sga = sb(p4, "sga", [128, 512], F32)
            sgb = sb(p4, "sgb", [128, 512], F32)
            m1 = sb(p4, "m1", [128, 512], F32)
            m2 = sb(p4, "m2", [128, 512], F32)
            mrgT = [sb(p4, "mrgT%d" % i, [128, 8, 512], BF16) for i in range(2)]
            pre = sb(p4, "pre", [128, 1024], F32)
            x1 = [sb(p4, "x1_%d" % i, [128, 1024], F32) for i in range(2)]
            x1b = [sb(p4, "x1b%d" % i, [128, 1024], BF16) for i in range(2)]
            x1T = [sb(p4, "x1T%d" % i, [128, 8, 128], F32) for i in range(2)]
            st6 = sb(p4, "st6", [128, 2, 6], F32)
            mv = sb(p4, "mv", [128, 4], F32)
            lg = sb(p4, "lg", [128, 32], F32)
            mx4 = sb(p4, "mx4", [128, 8], F32)
            nm = sb(p4, "nm", [128, 1], F32)
            msk = sb(p4, "msk", [128, 32], F32)
            mskb = sb(p4, "mskb", [128, 32], BF16)
            ex = sb(p4, "ex", [128, 32], F32)
            ssum = sb(p4, "ssum", [128, 1], F32)
            carry = sb(p4, "carry", [128, 32], F32)
            posf = sb(p4, "posf", [128, 32], F32)
            offv = sb(p4, "offv", [128, 32], F32)
            ovf = sb(p4, "ovf", [128, 32], F32)
            oh = sb(p4, "oh", [128, 32], F32)
            t32 = sb(p4, "t32", [128, 32], F32)
            offk = sb(p4, "offk", [128, 4], F32)
            DMA("pool", wmg[:], I["w_mg"].rearrange("(k p) n -> p k n", p=128), writes=[B("wmg")])
            DMA("pool", wupc[:], I["w_upc"].rearrange("(k p) n -> p k n", p=128), writes=[B("wupc")])
            DMA("pool", wupn[:], I["w_upn"].rearrange("(k p) n -> p k n", p=128), writes=[B("wupn")])
            DMA("pool", wo[:], I["w_o"].rearrange("(k p) n -> p k n", p=128), writes=[B("wo")])
            DMA("sp", wr[:], I["w_r"].rearrange("(k p) n -> p k n", p=128), writes=[B("wr")])
            DMA("sp", br_[:], I["b_r"], writes=[B("br")])
            MS("pool", carry[:], 0.0, [], [B("carry")])
            bi = 0
            bi_ = [0]

            def load_chunk(tc):
                t0 = tc * 512
                DMA("pool", xTc[tc % 2][:], I["xT"][:, t0:t0 + 512].rearrange("(k p) t -> p k t", p=128), writes=[B("xTc", tc % 2)])
                DMA("sp", oncs[tc % 2][:], on_d[:, :, t0:t0 + 512], reads=[B("on_d")], writes=[B("onc", tc % 2)])

            def merge_step(tc, oc):
                t0 = tc * 512
                xc, xcB = xTc[tc % 2], B("xTc", tc % 2)
                onc, oncB = oncs[tc % 2], B("onc", tc % 2)
                cs_ = slice(oc * 128, (oc + 1) * 128)
                b0, b1, b2, b3 = [(bi_[0] + j) % 4 for j in range(4)]
                MM(PS[b0][:, :], [(wmg[:, k, oc * 128:(oc + 1) * 128], xc[:, k, :]) for k in range(8)], [B("wmg"), xcB], [PB[b0]])
                ACT(sga[:], PS[b0][:, :], AF.Sigmoid, [PB[b0]], [B("sga")])
                MM(PS[b1][:, :], [(wmg[:, k, 1024 + oc * 128:1024 + (oc + 1) * 128], xc[:, k, :]) for k in range(8)], [B("wmg"), xcB], [PB[b1]])
                ACT(sgb[:], PS[b1][:, :], AF.Sigmoid, [PB[b1]], [B("sgb")])
                MM(PS[b2][:, :], [(wupc[:, k, cs_], u2T[:, k, t0:t0 + 512]) for k in range(4)], [B("wupc")] + [B("u2T", k, tc) for k in range(4)], [PB[b2]])
                TT("dve", m1[:], PS[b2][:, :], sga[:], ALU.mult, [PB[b2], B("sga")], [B("m1")])
                MM(PS[b3][:, :], [(wupn[:, k, cs_], onc[:, k, :]) for k in range(4)], [B("wupn"), oncB], [PB[b3]])
                TT("dve", m2[:], PS[b3][:, :], sgb[:], ALU.mult, [PB[b3], B("sgb")], [B("m2")])
                TT("pool", mrgT[tc % 2][:, oc, :], m1[:], m2[:], ALU.add, [B("m1"), B("m2")], [B("mrgT", tc % 2, oc)])


            def H1(i):
                tc, tt = i // 4, i % 4
                xti, xtB = xt[i % 2], B("xt", i % 2)
                x1i, x1B = x1[i % 2], B("x1", i % 2)
                x1bi, x1bB = x1b[i % 2], B("x1b", i % 2)
                DMA("sp", xti[:], I["xtok"][i * 128:(i + 1) * 128, :], writes=[xtB])
                for hf in range(2):
                    bz = 4 + hf
                    MM(PS[bz][:, :], [(mrgT[tc % 2][:, k, tt * 128:(tt + 1) * 128], wo[:, k, hf * 512:(hf + 1) * 512]) for k in range(8)], [B("wo")] + [B("mrgT", tc % 2, k) for k in range(8)], [PB[bz]])
                    STT("dve", pre[:, hf * 512:(hf + 1) * 512], xti[:, hf * 512:(hf + 1) * 512], DN_ALPHA, PS[bz][:, :], ALU.mult, ALU.add, [xtB, PB[bz]], [B("pre")] if hf == 0 else [], awrites=[] if hf == 0 else [B("pre")])
                layer_norm(pre, B("pre"), 0, x1i, x1B, st6, mv, "a", lnp)
                DMA("sp", x1f_d[i * 128:(i + 1) * 128, :], x1i[:], reads=[x1B], awrites=[X1F])
                if debug:
                    DMA("sp", dbg["x1"][i * 128:(i + 1) * 128, :], x1i[:], reads=[x1B])
                ACT(x1bi[:], x1i[:], AF.Copy, [x1B], [x1bB])
                for half in range(2):
                    bt = 6 + half

                    def trf(h, half=half, bt=bt, x1i=x1i):
                        ins = None
                        for k4 in range(4):
                            k = half * 4 + k4
                            ins = h.transpose(out=PS[bt][:, k4 * 128:(k4 + 1) * 128], in_=x1i[:, k * 128:(k + 1) * 128], identity=ident_f)
                        return ins
                    S.op("pe", trf, [x1B, CF], [PB[bt]])
                    if half == 0:
                        ACT(x1T[i % 2][:, 0:4, :], v4(PS[bt][:, :]), AF.Copy, [PB[bt]], [B("x1T", i % 2, 0)])
                    else:
                        ACT(x1T[i % 2][:, 4:8, :], v4(PS[bt][:, :]), AF.Copy, [PB[bt]], [B("x1T", i % 2, 1)])


            def H2(i):
                x1bi, x1bB = x1b[i % 2], B("x1b", i % 2)
                bl = bi_[0] % 4
                MM(PS[bl][:, 0:32], [(x1T[i % 2][:, k, :], wr[:, k, :]) for k in range(8)], [B("x1T", i % 2, 0), B("x1T", i % 2, 1), B("wr")], [PB[bl]])
                TT("dve", lg[:], PS[bl][:, 0:32], br_[:], ALU.add, [PB[bl], B("br")], [B("lg")])
                if debug:
                    DMA("sp", dbg["lg"][i * 128:(i + 1) * 128, :], lg[:], reads=[B("lg")])
                S.op("dve", lambda h: h.max(out=mx4[:], in_=lg[:]), [B("lg")], [B("mx4")])
                TS("dve", msk[:], lg[:], mx4[:, 3:4], None, ALU.is_ge, None, [B("lg"), B("mx4")], [B("msk")])
                TS("dve", nm[:], mx4[:, 0:1], -1.0, None, ALU.mult, None, [B("mx4")], [B("nm")])
                ACT(ex[:], lg[:], AF.Exp, [B("lg"), B("nm")], [B("ex")], bias=nm[:, 0:1])
                TT("dve", ex[:], ex[:], msk[:], ALU.mult, [B("ex"), B("msk")], [B("ex")])
                S.op("dve", lambda h: h.reduce_sum(out=ssum[:], in_=ex[:], axis=AX.X), [B("ex")], [B("ssum")])
                RCP(ssum[:], ssum[:], [B("ssum")], [B("ssum")])
                TS("dve", gd[:, i, :], ex[:], ssum[:, 0:1], None, ALU.mult, None, [B("ex"), B("ssum")], [B("gd", i)])
                CP("dve", mskb[:], msk[:], [B("msk")], [B("mskb")])
                bp = (bi_[0] + 1) % 4
                bc_ = (bi_[0] + 2) % 4
                MM(PS[bp][:, 0:32], [(Ust, mskb[:])], [CB, B("mskb")], [PB[bp]])
                MM(PS[bc_][:, 0:32], [(ones_b, mskb[:])], [CB, B("mskb")], [PB[bc_]])
                TT("dve", posf[:], PS[bp][:, 0:32], carry[:], ALU.add, [PB[bp], B("carry")], [B("posf")])
                TT("dve", carry[:], PS[bc_][:, 0:32], carry[:], ALU.add, [PB[bc_], B("carry")], [B("carry")])
                TS("dve", ovf[:], posf[:], float(CAP), 1e6, ALU.is_ge, ALU.mult, [B("posf")], [B("ovf")])
                TT("dve", offv[:], posf[:], eoff, ALU.add, [B("posf"), CF], [B("offv")])
                TT("dve", offv[:], offv[:], ovf[:], ALU.add, [B("offv"), B("ovf")], [B("offv")])
                TS("dve", offv[:], offv[:], float(32 * CAP), None, ALU.min, None, [B("offv")], [B("offv")])
                for k in range(4):
                    TS("dve", oh[:], lg[:], mx4[:, k:k + 1], None, ALU.is_equal, None, [B("lg"), B("mx4")], [B("oh")])
                    TT("dve", t32[:], oh[:], offv[:], ALU.mult, [B("oh"), B("offv")], [B("t32")])
                    S.op("dve", lambda h, k=k: h.reduce_sum(out=offk[:, k:k + 1], in_=t32[:], axis=AX.X), [B("t32")], [B("offk", k)])
                    TT("dve", t32[:], oh[:], gd[:, i, :], ALU.mult, [B("oh"), B("gd", i), B("t32")], [B("t32")])
                    S.op("dve", lambda h, k=k, i=i: h.reduce_sum(out=gk[:, i, k:k + 1], in_=t32[:], axis=AX.X), [B("t32")], [B("gk", i, k)])
                CP("dve", offs[:, i, :], offk[:], [B("offk", k) for k in range(4)], [B("offs", i)])
                for k in range(4):
                    S.dma("pool", lambda h, i=i, k=k, x1bi=x1bi: h.indirect_dma_start(
                        out=xg_d, out_offset=bass.IndirectOffsetOnAxis(ap=offs[:, i, k:k + 1], axis=0), in_=x1bi[:], in_offset=None,
                        ), reads=[x1bB, B("offs", i)], awrites=[XG])
                bi_[0] += 3


            load_chunk(0)
            for oc in range(8):
                merge_step(0, oc)
            pend = None
            for tc in range(NCH):
                if tc + 1 < NCH:
                    load_chunk(tc + 1)
                for tt in range(4):
                    i = tc * 4 + tt
                    H1(i)
                    if tc + 1 < NCH:
                        merge_step(tc + 1, 2 * tt)
                        merge_step(tc + 1, 2 * tt + 1)
                    if pend is not None:
                        H2(pend)
                    pend = i
            H2(pend)
            S.flush()

        if upto == 4:
            S.drain_dma()
            S.flush()
            raise _Stop(nc)
        mid.close()
        with ExitStack() as p5:
            wg = [sb(p5, "wg%d" % i, [128, 8, 1024], BF16) for i in range(2)]
            wl = [sb(p5, "wl%d" % i, [128, 8, 1024], BF16) for i in range(2)]
            wd = [sb(p5, "wd%d" % i, [128, 8, 1024], BF16) for i in range(2)]
            bgu = sb(p5, "bgu", [128, 32, 2, 8], F32)
            xgt = [sb(p5, "xgt%d" % i, [128, NCAPT, 1024], BF16) for i in range(2)]
            xgT = sb(p5, "xgT", [128, 8, CAP], BF16)
            actT = sb(p5, "actT", [128, 8, CAP], BF16)
            NCK = (CAP + 511) // 512
            CW = CAP // NCK
            NBF = 4
            g1 = [sb(p5, "g1_%d" % i, [128, CW], F32) for i in range(NBF)]
            sg = [sb(p5, "sg_%d" % i, [128, CW], F32) for i in range(NBF)]
            l1 = [sb(p5, "l1_%d" % i, [128, CW], F32) for i in range(NBF)]
            ysb = [sb(p5, "ysb%d" % i, [128, 1024], F32) for i in range(2)]
            DMA("sp", bgu[:], I["bgu"].rearrange("p (e a f) -> p e a f", e=32, a=2), writes=[B("bgu")])
            MS("pool", ysb[0][0:1, :], 0.0, [], [B("ysb", 0)])
            DMA("sp", y_d[32 * CAP:32 * CAP + 1, :], ysb[0][0:1, :], reads=[B("ysb", 0)], awrites=[YD])
            nch = [(c * CW, CW) for c in range(NCK)]
            ei = 0
            yi = 0
            bi = 0
            def load_expert(e):
                eb = e % 2
                DMA("sp", xgt[eb][:], xg_d[e * CAP:(e + 1) * CAP, :].rearrange("(s p) d -> p s d", p=128), reads=[XG], writes=[B("xgt", eb)])
                DMA("pool", wg[eb][:], I["w_glu"][e].rearrange("(k p) n -> p k n", p=128), writes=[B("wg", eb)])
                DMA("pool", wl[eb][:], I["w_lin"][e].rearrange("(k p) n -> p k n", p=128), writes=[B("wl", eb)])
                DMA("pool", wd[eb][:], I["w_dn"][e].rearrange("(k p) n -> p k n", p=128), writes=[B("wd", eb)])

            load_expert(0)
            for e in range(N_EXPERTS):
                eb = e % 2
                if e + 1 < N_EXPERTS:
                    load_expert(e + 1)
                trbb = None
                for s in range(NCAPT):
                    bt = 6 + (s % 2)
                    trbb = PS[bt][:, :].bitcast(BF16)

                    def trx(h, s=s, trbb=trbb, eb=eb):
                        ins = None
                        for k in range(8):
                            ins = h.transpose(out=trbb[:, k * 128:(k + 1) * 128], in_=xgt[eb][:, s, k * 128:(k + 1) * 128], identity=ident_b)
                        return ins
                    S.op("pe", trx, [B("xgt", eb), CB], [PB[bt]])
                    if s % 2 == 0:
                        ACT(xgT[:, :, s * 128:(s + 1) * 128], v4(trbb[:, 0:1024], 8), AF.Copy, [PB[bt]], [B("xgT", s)])
                    else:
                        CP("dve", xgT[:, :, s * 128:(s + 1) * 128], v4(trbb[:, 0:1024], 8), [PB[bt]], [B("xgT", s)])
                xgB = [B("xgT", s) for s in range(NCAPT)]
                for f in range(8):
                    fs = slice(f * 128, (f + 1) * 128)
                    for (n0, nn) in nch:
                        bg_, bl_ = bi % 4, (bi + 1) % 4
                        bi += 2
                        gg, ggB = g1[ei % NBF], B("g1", ei % NBF)
                        ss, ssB = sg[ei % NBF], B("sg", ei % NBF)
                        ll, llB = l1[ei % NBF], B("l1", ei % NBF)
                        ei += 1
                        MM(PS[bg_][:, 0:nn], [(wg[eb][:, k, fs], xgT[:, k, n0:n0 + nn]) for k in range(8)], [B("wg", eb)] + xgB, [PB[bg_]])
                        MM(PS[bl_][:, 0:nn], [(wl[eb][:, k, fs], xgT[:, k, n0:n0 + nn]) for k in range(8)], [B("wl", eb)] + xgB, [PB[bl_]])
                        TS("dve", gg[:, 0:nn], PS[bg_][:, 0:nn], bgu[:, e, 0, f:f + 1], 7.0, ALU.add, ALU.min, [PB[bg_], B("bgu")], [ggB])
                        ACT(ss[:, 0:nn], gg[:, 0:nn], AF.Sigmoid, [ggB], [ssB], scale=1.702)
                        TS("dve", ll[:, 0:nn], PS[bl_][:, 0:nn], bgu[:, e, 1, f:f + 1], 7.0, ALU.add, ALU.min, [PB[bl_], B("bgu")], [llB])
                        TS("dve", ll[:, 0:nn], ll[:, 0:nn], -7.0, 1.0, ALU.max, ALU.add, [llB], [llB])
                        TT("pool", gg[:, 0:nn], gg[:, 0:nn], ss[:, 0:nn], ALU.mult, [ggB, ssB], [ggB])
                        TT("pool", actT[:, f, n0:n0 + nn], gg[:, 0:nn], ll[:, 0:nn], ALU.mult, [ggB, llB], [B("actT", f)] if n0 == 0 else [], awrites=[] if n0 == 0 else [B("actT", f)])
                aB = [B("actT", f) for f in range(8)]
                for s in range(NCAPT):
                    yy, yB = ysb[yi % 2], B("ysb", yi % 2)
                    yi += 1
                    for hf in range(2):
                        by = 4 + hf
                        MM(PS[by][:, :], [(actT[:, f, s * 128:(s + 1) * 128], wd[eb][:, f, hf * 512:(hf + 1) * 512]) for f in range(8)], [B("wd", eb)] + aB, [PB[by]])
                        ACT(yy[:, hf * 512:(hf + 1) * 512], PS[by][:, :], AF.Copy, [PB[by]], [yB] if hf == 0 else [], awrites=[] if hf == 0 else [yB])
                    r0 = e * CAP + s * 128
                    DMA("sp", y_d[r0:r0 + 128, :], yy[:], reads=[yB], awrites=[YD])
            S.flush()

        if upto == 5:
            S.drain_dma()
            S.flush()
            raise _Stop(nc)
        with ExitStack() as p6:
            yk = [sb(p6, "yk%d" % i, [128, 4, 1024], F32) for i in range(3)]
            x1r = [sb(p6, "x1r%d" % i, [128, 1024], F32) for i in range(2)]
            accf = sb(p6, "accf", [128, 1024], F32)
            outt = [sb(p6, "outt%d" % i, [128, 1024], F32) for i in range(2)]
            gdT = sb(p6, "gdT", [32, 128], F32)
            bdn = sb(p6, "bdn", [32, 1024], F32)
            st6b = sb(p6, "st6b", [128, 2, 6], F32)
            mvb = sb(p6, "mvb", [128, 4], F32)
            DMA("sp", bdn[:], I["b_dn"], writes=[B("bdn")])
            lnp = sb(p6, "lnp6", [128, 4, 1024], F32)
            DMA("sp", lnp[:], I["lnp"], writes=[B("lnp")])
            for i in range(NT):
                yki, ykB = yk[i % 3], B("yk", i % 3)
                x1i, x1B = x1r[i % 2], B("x1r", i % 2)
                oi, oB = outt[i % 2], B("outt", i % 2)
                MS("pool", yki[:, 0, 0:1], 0.0, [], [ykB])
                for k in range(4):
                    S.dma("pool", lambda h, i=i, k=k, yki=yki: h.indirect_dma_start(
                        out=yki[:, k, :], out_offset=None, in_=y_d, in_offset=bass.IndirectOffsetOnAxis(ap=offs[:, i, k:k + 1], axis=0),
                        ), reads=[YD, B("offs", i)], awrites=[ykB])
                DMA("sp", x1i[:], x1f_d[i * 128:(i + 1) * 128, :], reads=[X1F], writes=[x1B])
                ACT(accf[:], x1i[:], AF.Copy, [x1B], [B("accf")], scale=DN_ALPHA)
                for k in range(4):
                    STT("dve", accf[:], yki[:, k, :], gk[:, i, k:k + 1], accf[:], ALU.mult, ALU.add, [ykB, B("gk", i, k), B("accf")], [B("accf")])
                S.op("pe", lambda h, i=i: h.transpose(out=PS[6][0:32, 0:128], in_=gd[:, i, :], identity=ident_f), [B("gd", i), CF], [PB[6]])
                ACT(gdT[:], PS[6][0:32, 0:128], AF.Copy, [PB[6]], [B("gdT")])
                for hf in range(2):
                    bb_ = 4 + hf
                    MM(PS[bb_][:, :], [(gdT[:], bdn[:, hf * 512:(hf + 1) * 512])], [B("gdT"), B("bdn")], [PB[bb_]])
                    TT("dve", accf[:, hf * 512:(hf + 1) * 512], accf[:, hf * 512:(hf + 1) * 512], PS[bb_][:, :], ALU.add, [B("accf"), PB[bb_]], [B("accf")])
                layer_norm(accf, B("accf"), 2, oi, oB, st6b, mvb, "b", lnp, ge="dve")
                DMA("sp", out[i * 128:(i + 1) * 128, :], oi[:], reads=[oB])
            S.drain_dma()
            S.flush()
    return nc


_CACHE = {}


def core_inputs(inp, sh, b, T, consts):
    m = dict(sh)
    m["xT"] = np.ascontiguousarray(inp["x"][b, :T].T)
    m["xtok"] = np.ascontiguousarray(inp["x"][b, :T])
    m["pos"] = np.ascontiguousarray(inp["positions"][b, :T].reshape(1, T).astype(np.int32))
    m["cf"], m["cb"] = consts
    return m


def kernel(**inputs):
    inp = {k: np.asarray(v) for k, v in inputs.items()}
    Bn, T = inp["x"].shape[0], inp["x"].shape[1]
    sh = prep_shared(inp)
    consts = make_consts(T)
    nc = build(T)
    in_maps = [core_inputs(inp, sh, b, T, consts) for b in range(Bn)]
    res = run_bass_kernel_spmd(nc, in_maps, core_ids=list(range(Bn)))
    out = np.stack([np.asarray(r["out"]) for r in res.results], 0).astype(np.float32)
    return out
```

```python
import os as _os
import numpy as np
from contextlib import ExitStack
import concourse.bass as bass
import concourse.mybir as mybir
from concourse.bass_utils import run_bass_kernel_spmd

F32 = mybir.dt.float32
BF16 = mybir.dt.bfloat16
I32 = mybir.dt.int32
AF = mybir.ActivationFunctionType
ALU = mybir.AluOpType
AX = mybir.AxisListType

D_MODEL = 1024
CONV_CH = 512
N_HEADS = 8
HEAD_DIM = 64
N_EXPERTS = 32
D_FF = 1024
ROPE_THETA = 500000.0
DN_ALPHA = 2.0 ** 0.25
LN_EPS = 1e-5
IN_COLS = 4888
NEGBIG = -30000.0


class Buf:
    __slots__ = ("name", "w", "r")

    def __init__(self, name):
        self.name = name
        self.w = {}
        self.r = {}


class Sched:
    def __init__(self, nc, stack, n_dma_slots=6):
        self.nc = nc
        self.names = ["pe", "dve", "act", "pool", "sp"]
        self.sem = {}
        self.cnt = {}
        self.seen = {}
        self.prog = {}
        for e in self.names:
            self.sem[e] = stack.enter_context(nc.semaphore("sem_" + e))
            self.cnt[e] = 0
            self.seen[e] = {}
            self.prog[e] = []
        self.dq = {}
        self.dqi = {}
        for q in ("sp", "pool", "act"):
            self.dq[q] = [
                {"sem": stack.enter_context(nc.semaphore("dsem_%s%d" % (q, i))), "total": 0, "key": "d%s%d" % (q, i)}
                for i in range(n_dma_slots)
            ]
            self.dqi[q] = 0

    def _waits(self, e, reads, writes, extra=(), awrites=()):
        waits = {}
        seen = self.seen[e]

        def need(key, sem, val):
            if e == "pe" and key == "pe":
                return
            if seen.get(key, 0) >= val:
                return
            if key not in waits or waits[key][1] < val:
                waits[key] = (sem, val)

        for b in reads:
            for key, (sem, val) in b.w.items():
                need(key, sem, val)
        for b in writes:
            for key, (sem, val) in b.w.items():
                need(key, sem, val)
            for key, (sem, val) in b.r.items():
                need(key, sem, val)
        for b in awrites:
            for key, (sem, val) in b.r.items():
                need(key, sem, val)
        for key, sem, val in extra:
            need(key, sem, val)
        for key, (sem, val) in waits.items():
            seen[key] = val
        return list(waits.values())

    def _record(self, key, tok, reads, writes, awrites=()):
        for b in reads:
            b.r[key] = tok
        for b in writes:
            b.w = {key: tok}
            b.r = {}
        for b in awrites:
            b.w[key] = tok

    def op(self, e, fn, reads=(), writes=(), inc=True, awrites=()):
        wl = self._waits(e, reads, writes, (), awrites)
        semE = self.sem[e]
        if inc:
            self.cnt[e] += 1
            tok = (semE, self.cnt[e])
        else:
            tok = (semE, self.cnt[e] + 1)

        def emit(h, wl=wl, fn=fn, inc=inc, semE=semE):
            for sem, val in wl:
                h.wait_ge(sem, val)
            ins = fn(h)
            if inc:
                ins.then_inc(semE, 1)

        self.prog[e].append(emit)
        self._record(e, tok, reads, writes, awrites)

    def dma(self, q, fn, reads=(), writes=(), awrites=()):
        slots = self.dq[q]
        slot = slots[self.dqi[q] % len(slots)]
        self.dqi[q] += 1
        extra = []
        if slot["total"] > 0:
            extra.append((slot["key"], slot["sem"], slot["total"]))
        wl = self._waits(q, reads, writes, extra, awrites)
        slot["total"] += 16
        tok = (slot["sem"], slot["total"])
        sem = slot["sem"]

        def emit(h, wl=wl, fn=fn, sem=sem):
            for s, val in wl:
                h.wait_ge(s, val)
            fn(h).then_inc(sem, 16)

        self.prog[q].append(emit)
        self._record(slot["key"], tok, reads, writes, awrites)

    def drain_dma(self):
        for q in ("sp", "pool", "act"):
            for slot in self.dq[q]:
                if slot["total"] > 0 and self.seen[q].get(slot["key"], 0) < slot["total"]:
                    self.seen[q][slot["key"]] = slot["total"]

                    def emit(h, sem=slot["sem"], val=slot["total"]):
                        h.wait_ge(sem, val)

                    self.prog[q].append(emit)

    def flush(self):
        nc = self.nc
        prog = self.prog
        with nc.Block() as block:

            @block.tensor
            def _(h):
                for f in prog["pe"]:
                    f(h)

            @block.vector
            def _(h):
                for f in prog["dve"]:
                    f(h)

            @block.scalar
            def _(h):
                for f in prog["act"]:
                    f(h)

            @block.gpsimd
            def _(h):
                for f in prog["pool"]:
                    f(h)

            @block.sync
            def _(h):
                for f in prog["sp"]:
                    f(h)

        for e in self.names:
            self.prog[e] = []


def dims(T):
    d = dict(T=T, NT=T // 128, NCH=T // 512, NCMP=T // 16 - 1, NSEL=T // 64, CAP=T // 8 + 128)
    d["NCT"] = (d["NCMP"] + 127) // 128
    d["NCAPT"] = d["CAP"] // 128
    d["WW"] = 4 * d["NT"] - 1
    return d


def make_consts(T):
    import ml_dtypes
    d = dims(T)
    NT, NCT, NCMP, NSEL, CAP, WW = d["NT"], d["NCT"], d["NCMP"], d["NSEL"], d["CAP"], d["WW"]
    p = np.arange(128)
    ident = np.eye(128, dtype=np.float32)
    D1 = (p[None, :] - 16 * p[:, None]).astype(np.float32)
    D2 = (p[None, :] - p[:, None]).astype(np.float32)
    r = p % 64
    invf = np.where(r < 16, ROPE_THETA ** (-(r % 8).astype(np.float32) * (2.0 / 16.0)), 0.0).astype(np.float32)
    sgn = np.where(r < 8, -1.0, 1.0).astype(np.float32)
    m = np.arange(WW)
    dd = m[None, :] - 2 * (NT - 1) - (p[:, None] >= 64)
    Wf = np.where(dd == 0, 2e4, np.where(dd == -1, 1e4, 0.0)).astype(np.float32)
    Wv = np.where(dd > 0, -1e30, 0.0).astype(np.float32)
    eoff = np.broadcast_to((np.arange(32) * CAP).astype(np.float32)[None, :], (128, 32))
    cf = np.concatenate([ident, D1, D2, invf[:, None], sgn[:, None], Wf, Wv, eoff], axis=1).astype(np.float32)
    E = np.zeros((128, NT, 128), np.float32)
    for kt in range(NT):
        for k in range(128):
            E[2 * kt + k // 64, kt, k] = 1.0
            E[64 + 2 * kt + k // 64, kt, k] = 1.0
    ov = np.zeros((128, NCT, 64), np.float32)
    for ct in range(NCT):
        for pp in range(128):
            c = ct * 128 + pp
            if c < NCMP:
                for j in range(min(NSEL, 64)):
                    if 16 * c < 64 * j + 64 and 16 * c + 32 > 64 * j:
                        ov[pp, ct, j] = 1.0
    U = (p[:, None] < p[None, :]).astype(np.float32)
    ones = np.ones((128, 128), np.float32)
    cb = np.concatenate([ident, E.reshape(128, -1), ov.reshape(128, -1), U, ones], axis=1).astype(ml_dtypes.bfloat16)
    return np.ascontiguousarray(cf), np.ascontiguousarray(cb)


def _swap_cols(w64):
    idx = np.arange(64)
    idx[:8] = np.arange(8, 16)
    idx[8:16] = np.arange(0, 8)
    return w64[:, idx]


def prep_shared(inp):
    w_in = inp["w_in"][0]
    c = np.cumsum([0, 512, 512, 512, 512, 128, 128, 128, 128, 128, 128, 24, 1024, 1024])
    xv, bg, cg, q = (w_in[:, c[i]:c[i + 1]] for i in range(4))
    kc, vc, ks, vs, kw, vw = (w_in[:, c[i]:c[i + 1]] for i in range(4, 10))
    ng, mga, mgb = (w_in[:, c[i]:c[i + 1]] for i in range(10, 13))
    chunks = []
    for cc in range(4):
        s = slice(cc * 128, (cc + 1) * 128)
        chunks += [xv[:, s], cg[:, s], bg[:, s]]
    for j in range(4):
        h0 = q[:, j * 64:(j + 1) * 64]
        h1 = q[:, (4 + j) * 64:(5 + j) * 64]
        chunks += [np.concatenate([h0, h1], 1), np.concatenate([_swap_cols(h0), _swap_cols(h1)], 1)]
    for k in (kc, ks, kw):
        chunks += [k, np.concatenate([_swap_cols(k[:, :64]), _swap_cols(k[:, 64:])], 1)]
    chunks += [vc]
    sh = {}
    sh["w_fm"] = np.ascontiguousarray(np.concatenate(chunks, 1))
    sh["w_tm"] = np.ascontiguousarray(np.concatenate([vs, vw, ng], 1))
    sh["w_mg"] = np.ascontiguousarray(np.concatenate([mga, mgb], 1))
    cw = inp["conv_w"][0][:, 0, :]
    sh["convw"] = np.ascontiguousarray(cw.reshape(3, 4, 128).transpose(2, 1, 0).reshape(128, 12))
    for n in ("k", "v"):
        pt = inp["cmp_pos_" + n][0].T
        sh["posT_" + n] = np.ascontiguousarray(np.concatenate([pt, pt], 0))
        sh["w1" + n] = np.ascontiguousarray(inp["cmp_w1_" + n][0])
    w2k = inp["cmp_w2_k"][0]
    sh["w2k"] = np.ascontiguousarray(np.concatenate([w2k, w2k], 1))
    sh["w2v"] = np.ascontiguousarray(inp["cmp_w2_v"][0])
    sh["w_upc"] = np.ascontiguousarray(inp["w_up_conv"][0])
    sh["w_upn"] = np.ascontiguousarray(inp["w_up_nsa"][0])
    sh["w_o"] = np.ascontiguousarray(inp["w_o"][0])
    lnp = np.stack([inp["ln1_g"][0], inp["ln1_b"][0], inp["ln2_g"][0], inp["ln2_b"][0]], 0)
    sh["lnp"] = np.ascontiguousarray(np.broadcast_to(lnp[None], (128, 4, 1024)))
    sh["w_r"] = np.ascontiguousarray(inp["w_router"][0])
    sh["b_r"] = np.ascontiguousarray(np.broadcast_to(inp["b_router"][0][None], (128, 32)))
    wgu = inp["w_gate_up"][0]
    sh["w_glu"] = np.ascontiguousarray(wgu[:, :, 0::2])
    sh["w_lin"] = np.ascontiguousarray(wgu[:, :, 1::2])
    sh["w_dn"] = np.ascontiguousarray(inp["w_down"][0])
    bgu = inp["b_gate_up"][0].reshape(32, 8, 128, 2)
    sh["bgu"] = np.ascontiguousarray(bgu.transpose(2, 0, 3, 1).reshape(128, 32 * 2 * 8))
    sh["b_dn"] = np.ascontiguousarray(inp["b_down"][0])
    return sh


INPUT_SPECS = None


def input_specs(T):
    d = dims(T)
    cf, cb = make_consts(T) if False else (None, None)
    ncf = 128 * 3 + 2 + 2 * d["WW"] + 32
    ncb = 128 + d["NT"] * 128 + d["NCT"] * 64 + 128 + 128
    return [
        ("xT", [1024, T], F32), ("xtok", [T, 1024], F32), ("pos", [1, T], I32),
        ("w_fm", [1024, 27 * 128], F32), ("w_tm", [1024, 280], F32), ("w_mg", [1024, 2048], F32),
        ("convw", [128, 12], F32), ("posT_k", [128, 32], F32), ("posT_v", [128, 32], F32),
        ("w1k", [2048, 256], F32), ("w1v", [2048, 256], F32), ("w2k", [256, 128], F32), ("w2v", [256, 64], F32),
        ("w_upc", [512, 1024], F32), ("w_upn", [512, 1024], F32), ("w_o", [1024, 1024], F32),
        ("lnp", [128, 4, 1024], F32), ("w_r", [1024, 32], F32), ("b_r", [128, 32], F32),
        ("w_glu", [32, 1024, 1024], F32), ("w_lin", [32, 1024, 1024], F32), ("w_dn", [32, 1024, 1024], F32),
        ("bgu", [128, 512], F32), ("b_dn", [32, 1024], F32),
        ("cf", [128, ncf], F32), ("cb", [128, ncb], BF16),
    ]


class _Stop(Exception):
    pass


def build(T, debug=False, upto=9):
    try:
        return _build(T, debug, upto)
    except _Stop as e:
        return e.args[0]


def _build(T, debug, upto):
    d = dims(T)
    NT, NCH, NCMP, NCT, NSEL, CAP, NCAPT, WW = (d[k] for k in ("NT", "NCH", "NCMP", "NCT", "NSEL", "CAP", "NCAPT", "WW"))
    TH = min(T, 1024)
    NH = T // TH
    nc = bass.Bass("TRN2", target_bir_lowering=False)
    I = {}
    for name, shape, dt in input_specs(T):
        I[name] = nc.dram_tensor(name, shape, dt, kind="ExternalInput").ap()
    out = nc.dram_tensor("out", [T, 1024], F32, kind="ExternalOutput").ap()
    x1f_d = nc.dram_tensor("x1f_scr", [T, 1024], F32, kind="Internal").ap()
    xg_d = nc.dram_tensor("xg_scr", [32 * CAP + 128, 1024], BF16, kind="Internal").ap()
    on_d = nc.dram_tensor("on_scr", [128, 4, T], BF16, kind="Internal").ap()
    y_d = nc.dram_tensor("y_scr", [32 * CAP + 128, 1024], F32, kind="Internal").ap()
    dbg = {}
    if debug:
        dbg["x1"] = nc.dram_tensor("dbg_x1", [T, 1024], F32, kind="ExternalOutput").ap()
        dbg["onsa"] = nc.dram_tensor("dbg_onsa", [T, 512], F32, kind="ExternalOutput").ap()
        dbg["lg"] = nc.dram_tensor("dbg_lg", [T, 32], F32, kind="ExternalOutput").ap()
        dbg["nsel"] = nc.dram_tensor("dbg_nsel", [T, 2, NSEL], F32, kind="ExternalOutput").ap()

    bufs = {}

    def B(*key):
        if key not in bufs:
            bufs[key] = Buf(str(key))
        return bufs[key]

    with ExitStack() as top:
        S = Sched(nc, top)

        def TT(e, out, in0, in1, op, reads, writes, **kw):
            S.op(e, lambda h: h.tensor_tensor(out=out, in0=in0, in1=in1, op=op), reads, writes, **kw)

        def TS(e, out, in0, s1, s2, op0, op1, reads, writes, **kw):
            if op1 is None:
                S.op(e, lambda h: h.tensor_scalar(out=out, in0=in0, scalar1=s1, scalar2=None, op0=op0), reads, writes, **kw)
            else:
                S.op(e, lambda h: h.tensor_scalar(out=out, in0=in0, scalar1=s1, scalar2=s2, op0=op0, op1=op1), reads, writes, **kw)

        def STT(e, out, in0, sc, in1, op0, op1, reads, writes, **kw):
            S.op(e, lambda h: h.scalar_tensor_tensor(out=out, in0=in0, scalar=sc, in1=in1, op0=op0, op1=op1), reads, writes, **kw)

        def ACT(out, in_, func, reads, writes, scale=1.0, bias=None, **kw):
            if bias is None:
                S.op("act", lambda h: h.activation(out=out, in_=in_, func=func, scale=scale), reads, writes, **kw)
            else:
                S.op("act", lambda h: h.activation(out=out, in_=in_, func=func, scale=scale, bias=bias), reads, writes, **kw)

        def CP(e, out, in_, reads, writes, **kw):
            S.op(e, lambda h: h.tensor_copy(out=out, in_=in_), reads, writes, **kw)

        def MS(e, ap, val, reads, writes, **kw):
            S.op(e, lambda h: h.memset(ap, val), reads, writes, **kw)

        def RCP(out, in_, reads, writes):
            S.op("dve", lambda h: h.reciprocal(out=out, in_=in_), reads, writes)

        def DMA(q, out, in_, reads=(), writes=(), awrites=()):
            S.dma(q, lambda h: h.dma_start(out=out, in_=in_), reads, writes, awrites)

        def MM(bank_ap, pairs, reads, writes, first=True, last=True):
            n = len(pairs)

            def fn(h):
                ins = None
                for k, (l, r) in enumerate(pairs):
                    ins = h.matmul(bank_ap, lhsT=l, rhs=r, start=(first and k == 0), stop=(last and k == n - 1))
                return ins
            S.op("pe", fn, reads, writes)

        def sb(stack, name, shape, dt):
            return stack.enter_context(nc.sbuf_tensor("s_" + name, shape, dt))

        PS = [top.enter_context(nc.psum_tensor("ps%d" % i, [128, 512], F32)) for i in range(8)]
        PB = [B("ps", i) for i in range(8)]

        NCF = I["cf"].shape[1]
        NCB = I["cb"].shape[1]
        cf = sb(top, "cf", [128, NCF], F32)
        cb = sb(top, "cb", [128, NCB], BF16)
        ident_f = cf[:, 0:128]
        D1 = cf[:, 128:256]
        D2 = cf[:, 256:384]
        invf = cf[:, 384:385]
        sgn = cf[:, 385:386]
        Wf = cf[:, 386:386 + WW]
        Wv = cf[:, 386 + WW:386 + 2 * WW]
        eoff = cf[:, 386 + 2 * WW:386 + 2 * WW + 32]
        ident_b = cb[:, 0:128]
        o_ = 128
        Ecb = cb[:, o_:o_ + NT * 128].rearrange("p (a b) -> p a b", a=NT)
        o_ += NT * 128
        ovc = cb[:, o_:o_ + NCT * 64].rearrange("p (a b) -> p a b", a=NCT)
        o_ += NCT * 64
        Ust = cb[:, o_:o_ + 128]
        o_ += 128
        ones_b = cb[:, o_:o_ + 128]
        CF, CB = B("cf"), B("cb")
        DMA("sp", cf[:], I["cf"], writes=[CF])
        DMA("sp", cb[:], I["cb"], writes=[CB])

        epsc = sb(top, "epsc", [128, 1], F32)
        MS("pool", epsc[:], LN_EPS, [], [B("epsc")])
        offs = sb(top, "offs", [128, NT, 4], I32)
        gk = sb(top, "gk", [128, NT, 4], F32)
        gd = sb(top, "gd", [128, NT, 32], F32)
        mid = ExitStack()
        u2T = sb(mid, "u2T", [128, 4, T], BF16)

        def v4(ap, a=4):
            return ap.rearrange("p (a b) -> p a b", a=a)

        with ExitStack() as att:
            qT = sb(att, "qT", [128, 4, T], BF16)
            kT = [sb(att, "kT0", [128, T], BF16), None, sb(att, "kT2", [128, T], BF16)]
            kA = [sb(att, "kA%d" % i, [128, T], BF16) for i in range(2)]
            DMA("sp", kA[0][64:128, :], I["cb"][64:128, 128:128 + T], awrites=[B("kAE", 0)])
            DMA("sp", kA[1][0:64, :], I["cb"][0:64, 128:128 + T], awrites=[B("kAE", 1)])
            vcT = sb(att, "vcT", [128, T], BF16)
            Vext = [sb(att, "Vext%d" % i, [128, NT, 2, 65], BF16) for i in range(2)]
            gates = sb(att, "gates", [128, NT, 24], F32)
            kcT = sb(att, "kcT", [128, NCT * 128], BF16)
            Vc = sb(att, "Vc", [128, NCT, 2, 65], BF16)
            MS("pool", Vext[0][:, :, :, 64:65], 1.0, [], [B("Vones", 0)])
            MS("pool", Vext[1][:, :, :, 64:65], 1.0, [], [B("Vones", 1)])
            MS("pool", Vc[:, :, :, 64:65], 1.0, [], [B("Vcones")])
            MS("pool", kcT[:], 0.0, [], [B("kcT")])

            with ExitStack() as p1:
                xTb = sb(p1, "xTb", [128, 8, TH], BF16)
                wbuf = [sb(p1, "wbuf%d" % i, [128, 8, 512], BF16) for i in range(2)]
                wtm = sb(p1, "wtm", [128, 8, 280], BF16)
                cosT = sb(p1, "cosT", [128, TH], F32)
                sinT = sb(p1, "sinT", [128, TH], F32)
                posi = sb(p1, "posi", [128, 512], I32)
                ang = sb(p1, "ang", [128, 512], F32)
                angi = sb(p1, "angi", [128, 512], I32)
                angf = sb(p1, "angf", [128, 512], F32)
                ut = [sb(p1, "ut%d" % i, [128, 514], F32) for i in range(2)]
                csb = [sb(p1, "csb%d" % i, [128, 512], F32) for i in range(2)]
                c1 = [sb(p1, "c1_0", [128, 512], F32)] * 2
                hal = sb(p1, "hal", [128, 4, 2], F32)
                cw = sb(p1, "cw", [128, 12], F32)
                rt1 = [sb(p1, "rt1_0", [128, 512], F32)] * 2
                rt2 = [sb(p1, "rt2_0", [128, 512], F32)] * 2
                DMA("sp", cw[:], I["convw"], writes=[B("cw")])
                MS("pool", hal[:], 0.0, [], [B("hal", cc) for cc in range(4)])
                DMA("pool", wtm[:], I["w_tm"].rearrange("(k p) n -> p k n", p=128), writes=[B("wtm")])

                groups = [[0, 1, 2], [3, 4, 5], [6, 7, 8], [9, 10, 11], [12, 13, 14, 15], [16, 17, 18, 19], [20, 21, 22, 23], [24, 25, 26]]
                gl = 0
                bank_rr = [0]

                def take_banks(n):
                    r = [(bank_rr[0] + i) % 6 for i in range(n)]
                    bank_rr[0] = (bank_rr[0] + n) % 6
                    return r

                ui = 0
                for hf in range(NH):
                    h0 = hf * TH
                    DMA("pool", xTb[:, :, :], I["xT"][:, h0:h0 + TH].rearrange("(k p) t -> p k t", p=128), writes=[B("xTb")])
                    for rc_ in range(TH // 512):
                        rs = slice(rc_ * 512, (rc_ + 1) * 512)
                        DMA("sp", posi[:], I["pos"][:, h0 + rc_ * 512:h0 + (rc_ + 1) * 512].to_broadcast([128, 512]), writes=[B("posi")])
                        CP("dve", ang[:], posi[:], [B("posi")], [B("ang")])
                        TS("dve", ang[:], ang[:], invf, float(1.0 / (2 * np.pi)), ALU.mult, ALU.mult, [B("ang"), CF], [B("ang")])
                        for which, tab in ((0, sinT), (1, cosT)):
                            if which == 1:
                                TS("dve", ang[:], ang[:], 0.25, None, ALU.add, None, [B("ang")], [B("ang")])
                            CP("dve", angi[:], ang[:], [B("ang")], [B("angi")])
                            CP("dve", angf[:], angi[:], [B("angi")], [B("angf")])
                            TT("dve", angf[:], ang[:], angf[:], ALU.subtract, [B("ang"), B("angf")], [B("angf")])
                            STT("dve", angf[:], angf[:], 0.5, angf[:], ALU.is_gt, ALU.subtract, [B("angf")], [B("angf")])
                            ACT(tab[:, rs], angf[:], AF.Sin, [B("angf")], [B("tab", which)], scale=float(-2 * np.pi))
                    TS("dve", sinT[:], sinT[:], sgn, None, ALU.mult, None, [B("tab", 0), CF], [B("tab", 0)])
                    if upto == 0.1:
                        S.drain_dma()
                        S.flush()
                        raise _Stop(nc)

                    for grp in groups:
                        wb = wbuf[gl % 2]
                        wB = B("wbuf", gl % 2)
                        gl += 1
                        ncols = len(grp) * 128
                        c0 = grp[0] * 128
                        DMA("pool", wb[:, :, 0:ncols], I["w_fm"][:, c0:c0 + ncols].rearrange("(k p) n -> p k n", p=128), writes=[wB])
                        for tcl in range(TH // 512):
                            t0 = h0 + tcl * 512
                            tl = tcl * 512
                            tc = t0 // 512
                            banks = take_banks(3) if len(grp) == 3 else take_banks(4)
                            for ci, ch in enumerate(grp):
                                bk = banks[ci]
                                MM(PS[bk][:, :], [(wb[:, k, ci * 128:(ci + 1) * 128], xTb[:, k, tl:tl + 512]) for k in range(8)], [wB, B("xTb")], [PB[bk]])
                            if grp[0] < 12:
                                cc = grp[0] // 3
                                bx, bc, bb = banks
                                u, uB = ut[ui % 2], B("ut", ui % 2)
                                cs, csB = csb[ui % 2], B("csb", ui % 2)
                                cc1, c1B = c1[0], B("c1", 0)
                                ui += 1
                                ACT(cs[:], PS[bc][:, :], AF.Copy, [PB[bc]], [csB])
                                CP("pool", u[:, 0:2], hal[:, cc, :], [B("hal", cc)], [uB])
                                TT("dve", u[:, 2:514], PS[bx][:, :], cs[:], ALU.mult, [PB[bx], csB, uB], [uB])
                                CP("pool", hal[:, cc, :], u[:, 512:514], [uB], [B("hal", cc)])
                                TS("dve", cc1[:], u[:, 2:514], cw[:, cc * 3 + 2:cc * 3 + 3], None, ALU.mult, None, [uB, B("cw")], [c1B])
                                STT("dve", cc1[:], u[:, 1:513], cw[:, cc * 3 + 1:cc * 3 + 2], cc1[:], ALU.mult, ALU.add, [uB, c1B], [c1B])
                                STT("dve", cc1[:], u[:, 0:512], cw[:, cc * 3:cc * 3 + 1], cc1[:], ALU.mult, ALU.add, [uB, c1B], [c1B])
                                TT("dve", u2T[:, cc, t0:t0 + 512], PS[bb][:, :], cc1[:], ALU.mult, [PB[bb], c1B], [B("u2T", cc, tc)])
                            else:
                                ci = 0
                                while ci < len(grp):
                                    ch = grp[ci]
                                    if ch == 26:
                                        bk = banks[ci]
                                        ACT(vcT[:, t0:t0 + 512], PS[bk][:, :], AF.Copy, [PB[bk]], [B("vcT", tc)])
                                        ci += 1
                                        continue
                                    bq, bs = banks[ci], banks[ci + 1]
                                    if ch < 20:
                                        j = (ch - 12) // 2
                                        dten, dB = qT[:, j, :], B("qT", j, tc)
                                    else:
                                        br = (ch - 20) // 2
                                        dten, dB = (kT[br][:, :] if br != 1 else None), B("kT", br, tc)
                                    r1, r2, rB = rt1[0], rt2[0], B("rt", 0)
                                    ui += 1
                                    TT("dve", r1[:], PS[bq][:, :], cosT[:, tl:tl + 512], ALU.mult, [PB[bq], B("tab", 1)], [rB])
                                    TT("dve", r2[:], PS[bs][:, :], sinT[:, tl:tl + 512], ALU.mult, [PB[bs], B("tab", 0), rB], [rB])
                                    if dten is None:
                                        TT("pool", kA[0][0:64, t0:t0 + 512], r1[0:64, :], r2[0:64, :], ALU.add, [rB], [dB])
                                        TT("pool", kA[1][64:128, t0:t0 + 512], r1[64:128, :], r2[64:128, :], ALU.add, [rB, dB], [], awrites=[dB])
                                    else:
                                        TT("pool", dten[:, t0:t0 + 512], r1[:], r2[:], ALU.add, [rB], [dB])
                                    ci += 2
                        if upto == 0.2 and grp[0] == 0:
                            S.drain_dma()
                            S.flush()
                            raise _Stop(nc)
                        if upto == 0.25 and grp[0] == 12:
                            S.drain_dma()
                            S.flush()
                            raise _Stop(nc)
                        if upto == 0.3 and grp[0] == 24:
                            S.drain_dma()
                            S.flush()
                            raise _Stop(nc)
                    for il in range(TH // 128):
                        i = h0 // 128 + il
                        bk = 6 + (i % 2)
                        MM(PS[bk][:, 0:280], [(xTb[:, k, il * 128:(il + 1) * 128], wtm[:, k, :]) for k in range(8)], [B("wtm"), B("xTb")], [PB[bk]])
                        for vi in range(2):
                            ACT(Vext[vi][:, i, :, 0:64], v4(PS[bk][:, vi * 128:(vi + 1) * 128], 2), AF.Copy, [PB[bk], B("Vones", vi)], [B("Vext", vi, i)])
                        ACT(gates[:, i, :], PS[bk][:, 256:280], AF.Sigmoid, [PB[bk]], [B("gates", i)])
                S.flush()

            if upto == 1:
                S.drain_dma()
                S.flush()
                raise _Stop(nc)
            with ExitStack() as p2:
                w1 = {n: sb(p2, "w1" + n, [128, 32, 256], BF16) for n in "kv"}
                posT = {n: sb(p2, "posT" + n, [128, 32], BF16) for n in "kv"}
                w2k = sb(p2, "w2k", [128, 2, 128], BF16)
                w2v = sb(p2, "w2v", [128, 2, 64], BF16)
                cst = sb(p2, "cst", [128, 4], F32)
                hsb = sb(p2, "hsb", [128, 256], F32)
                h2 = sb(p2, "h2", [128, 256], F32)
                gT = {(n, g): sb(p2, "gT%s%d" % (n, g), [128, 2, 256], BF16) for n in "kv" for g in range(2)}
                for n in "kv":
                    for half in range(2):
                        DMA("pool", w1[n][half * 64:(half + 1) * 64, :, :], I["w1" + n].rearrange("(l d) h -> d l h", d=64), awrites=[B("w1", n)])
                    DMA("pool", posT[n][:], I["posT_" + n], writes=[B("posT", n)])
                DMA("pool", w2k[:], I["w2k"].rearrange("(c p) n -> p c n", p=128), writes=[B("w2k")])
                DMA("pool", w2v[:], I["w2v"].rearrange("(c p) n -> p c n", p=128), writes=[B("w2v")])
                bi = 0
                for ni, n in enumerate("kv"):
                    for hc in range(2):
                        bk = bi % 6
                        bi += 1
                        MM(PS[bk][:, 0:1], [(w1[n][0:64, l, hc * 128:(hc + 1) * 128], posT[n][0:64, l:l + 1]) for l in range(32)], [B("w1", n), B("posT", n)], [PB[bk]])
                        ACT(cst[:, ni * 2 + hc:ni * 2 + hc + 1], PS[bk][:, 0:1], AF.Copy, [PB[bk]], [B("cst", ni, hc)])
                for ni, n in enumerate("kv"):
                    src = kT[0] if n == "k" else vcT
                    srcB = [B("kT", 0, tc) for tc in range(NCH)] if n == "k" else [B("vcT", tc) for tc in range(NCH)]
                    for g in range(2):
                        for hc in range(2):
                            bk = bi % 6
                            bi += 1
                            MM(PS[bk][:, 0:NCMP], [(w1[n][64 * g:64 * g + 64, l, hc * 128:(hc + 1) * 128], src[64 * g:64 * g + 64, l:l + 16 * (NCMP - 1) + 1:16]) for l in range(32)],
                               [B("w1", n)] + srcB, [PB[bk]])
                            hs, hh2 = hsb[:, 0:NCMP], h2[:, 0:NCMP]
                            ACT(hs, PS[bk][:, 0:NCMP], AF.Identity, [PB[bk], B("cst", ni, hc)], [B("hsb")], bias=cst[:, ni * 2 + hc:ni * 2 + hc + 1])
                            TT("dve", hh2, hs, hs, ALU.mult, [B("hsb")], [B("h2")])
                            TS("dve", hh2, hh2, 0.044715, 1.0, ALU.mult, ALU.add, [B("h2")], [B("h2")])
                            TT("dve", hh2, hh2, hs, ALU.mult, [B("h2"), B("hsb")], [B("h2")])
                            ACT(hh2, hh2, AF.Sigmoid, [B("h2")], [B("h2")], scale=1.5957691216057308)
                            TT("dve", gT[(n, g)][:, hc, 0:NCMP], hh2, hs, ALU.mult, [B("h2"), B("hsb")], [B("gT", n, g, hc)])
                for g in range(2):
                    bk = bi % 6
                    bi += 1
                    MM(PS[bk][:, 0:NCMP], [(w2k[:, hc, :], gT[("k", g)][:, hc, 0:NCMP]) for hc in range(2)], [B("w2k"), B("gT", "k", g, 0), B("gT", "k", g, 1)], [PB[bk]])
                    ACT(kcT[64 * g:64 * g + 64, 0:NCMP], PS[bk][64 * g:64 * g + 64, 0:NCMP], AF.Copy, [PB[bk], B("kcT")], [], awrites=[B("kcT")])
                for g in range(2):
                    for ct in range(NCT):
                        cn = min(128, NCMP - ct * 128)
                        bk = bi % 6
                        bi += 1
                        MM(PS[bk][0:cn, 0:64], [(gT[("v", g)][:, hc, ct * 128:ct * 128 + cn], w2v[:, hc, :]) for hc in range(2)], [B("w2v"), B("gT", "v", g, 0), B("gT", "v", g, 1)], [PB[bk]])
                        ACT(Vc[0:cn, ct, g, 0:64], PS[bk][0:cn, 0:64], AF.Copy, [PB[bk], B("Vcones")], [], awrites=[B("Vc")])
                S.flush()

            if upto == 2:
                S.drain_dma()
                S.flush()
                raise _Stop(nc)
            with ExitStack() as p3:
                NPR = NT + 5 + 2
                Pring = sb(p3, "Pring", [128, NPR, 512], BF16)
                nselT = sb(p3, "nselT", [128, 4, 128], BF16)
                onst = [sb(p3, "onst%d" % i, [128, 4, 128], BF16) for i in range(2)]
                nsel = sb(p3, "nsel", [128, 128], BF16)
                MS("pool", nsel[:], 0.0, [], [B("nsel")])
                otok = sb(p3, "otok", [128, 512], BF16)
                acc = sb(p3, "acc", [128, 4, 64], F32)
                tmp3 = sb(p3, "tmp3", [128, 4, 64], F32)
                rc = sb(p3, "rc", [128, 3, 4], F32)
                impsb = sb(p3, "impsb", [128, 64], F32)
                scr2 = sb(p3, "scr2", [128, 64], F32)
                mx = sb(p3, "mx", [128, 16], F32)
                dbgt = sb(p3, "dbgt", [128, 512], F32) if debug else None
                D1b = D1.unsqueeze(1).to_broadcast([128, 4, 128])
                D2b = D2.unsqueeze(1).to_broadcast([128, 4, 128])
                pti = [0]
                sbank = [0]
                BOC, BIMP, BOS, BOW, BTR = 3, 4, 5, 6, 7
                trb = PS[BTR][:, :].bitcast(BF16)

                def score_tile(i, g, M, lhs_ap, lhsB, mask=None, extra=None, extraB=(), slot=0, rhs=None):
                    bk = sbank[0] % 3
                    sbank[0] += 1
                    P = Pring[:, slot, :]
                    PBf = B("Pt", slot)
                    pairs = [(lhs_ap, qT[64 * g:64 * g + 64, :, i * 128:(i + 1) * 128] if rhs is None else rhs)]
                    if extra is not None:
                        pairs.append(extra)
                    MM(v4(PS[bk][0:M, :]), pairs, list(lhsB) + [B("qT", j, i // 4) for j in range(4)] + list(extraB), [PB[bk]])
                    ACT(P[0:M, :], PS[bk][0:M, :], AF.Exp, [PB[bk]], [PBf], scale=0.125)
                    if mask is not None:
                        Db, cmp_op, thr = mask
                        STT("dve", v4(P[0:M, :]), Db[0:M], float(thr), v4(P[0:M, :]), cmp_op, ALU.mult, [PBf, CF], [PBf])
                    return P, PBf

                def pv(tiles, bank, ncol=65):
                    n = len(tiles)

                    def fn(h):
                        ins = None
                        for hh in range(4):
                            for k, (P, PBf, M, rhs_ap, rhsB) in enumerate(tiles):
                                ins = h.matmul(PS[bank][:, hh * ncol:(hh + 1) * ncol], lhsT=P[0:M, hh * 128:(hh + 1) * 128], rhs=rhs_ap, start=(k == 0), stop=(k == n - 1))
                        return ins
                    rd = []
                    for (P, PBf, M, rhs_ap, rhsB) in tiles:
                        rd.append(PBf)
                        rd += list(rhsB)
                    S.op("pe", fn, rd, [PB[bank]])

                accs = [acc, sb(p3, "acc_b", [128, 4, 64], F32)]
                nselTs = [nselT, sb(p3, "nselT_b", [128, 4, 128], BF16)]
                tmp3b = sb(p3, "tmp3b", [128, 4, 64], F32)
                NI = min(NT, int(_os.environ.get('K_MAXI', '9999')))
                units = [(i, g) for i in range(NI) for g in range(2)]

                def stageA(i, g, u):
                    q0 = i * 128
                    gs = slice(64 * g, 64 * g + 64)
                    accu, accB = accs[u % 2], B("acc", u % 2)
                    nsT, nsTB = nselTs[u % 2], B("nselT", u % 2)
                    cts = []
                    for ct in range(NCT):
                        cn = min(128, NCMP - ct * 128)
                        M = min(cn, 8 * i + 7 - 128 * ct)
                        if M > 0:
                            cts.append((ct, M))
                    tl_c, tl_i = [], []
                    for idx, (ct, M) in enumerate(cts):
                        full = 16 * (ct * 128 + M - 1) + 31 <= q0
                        mask = None if full else (D1b, ALU.is_ge, 31 + 16 * 128 * ct - q0)
                        P, PBf = score_tile(i, g, M, kcT[gs, ct * 128:ct * 128 + M], [B("kcT")], mask, slot=NT + 5 + idx)
                        tl_c.append((P, PBf, M, Vc[0:M, ct, g, :], [B("Vc"), B("Vcones")]))
                        tl_i.append((P, PBf, M, ovc[0:M, ct, 0:NSEL], [CB]))
                    pv(tl_c, BOC)
                    pv(tl_i, BIMP, ncol=NSEL)
                    kts = list(range(max(0, i - 4), i + 1))
                    tl_w = []
                    for wi, kt in enumerate(kts):
                        mask = None
                        if kt == i:
                            mask = (D2b, ALU.is_ge, 0.0)
                        elif kt == i - 4:
                            mask = (D2b, ALU.is_le, -1.0)
                        P, PBf = score_tile(i, g, 128, kT[2][gs, kt * 128:(kt + 1) * 128], [B("kT", 2, kt // 4)], mask, slot=NT + wi)
                        tl_w.append((P, PBf, 128, Vext[1][:, kt, g, :], [B("Vext", 1, kt), B("Vones", 1)]))
                    pv(tl_w, BOW)
                    ocv = v4(PS[BOC][:, 0:260])
                    imp = impsb[:, 0:NSEL]
                    TS("dve", rc[:, 0, :], ocv[:, :, 64], 1e-30, None, ALU.max, None, [PB[BOC]], [B("rc", 0)])
                    RCP(rc[:, 0, :], rc[:, 0, :], [B("rc", 0)], [B("rc", 0)])
                    TS("dve", imp, PS[BIMP][:, 0:NSEL], rc[:, 0, 0:1], None, ALU.mult, None, [PB[BIMP], B("rc", 0)], [B("imp")])
                    for hh in range(1, 4):
                        STT("dve", imp, PS[BIMP][:, hh * NSEL:(hh + 1) * NSEL], rc[:, 0, hh:hh + 1], imp, ALU.mult, ALU.add, [PB[BIMP], B("rc", 0), B("imp")], [B("imp")])
                    w0 = 2 * (NT - 1) - 2 * i
                    TT("dve", imp, imp, Wf[:, w0:w0 + NSEL], ALU.max, [B("imp"), CF], [B("imp")])
                    TT("dve", imp, imp, Wv[:, w0:w0 + NSEL], ALU.add, [B("imp"), CF], [B("imp")])
                    MS("dve", impsb[:, 0:1], 3e4, [B("imp")], [B("imp")])
                    S.op("dve", lambda h: h.max(out=mx[:, 0:8], in_=imp), [B("imp")], [B("mx")])
                    S.op("dve", lambda h: h.match_replace(out=scr2[:, 0:NSEL], in_to_replace=mx[:, 0:8], in_values=imp, imm_value=-3e38), [B("imp"), B("mx")], [B("scr2")])
                    S.op("dve", lambda h: h.max(out=mx[:, 8:16], in_=scr2[:, 0:NSEL]), [B("scr2"), B("mx")], [B("mx")])
                    TS("dve", nsel[:, 0:NSEL], imp, mx[:, 15:16], NEGBIG, ALU.is_lt, ALU.mult, [B("imp"), B("mx")], [B("nsel")])
                    TS("dve", nsel[:, 64:64 + NSEL], imp, mx[:, 15:16], NEGBIG, ALU.is_lt, ALU.mult, [B("imp"), B("mx"), B("nsel")], [], awrites=[B("nsel")])
                    if debug:
                        CP("dve", dbgt[:, 0:NSEL], nsel[:, 0:NSEL], [B("nsel")], [B("dbgt")])
                        DMA("sp", dbg["nsel"][i * 128:(i + 1) * 128, g, :], dbgt[:, 0:NSEL], reads=[B("dbgt")])
                    S.op("pe", lambda h: h.transpose(out=trb[:, 0:128], in_=nsel[:, :], identity=ident_b), [B("nsel"), CB], [PB[BTR]])
                    og = 64 * (1 - g)
                    ACT(nsT[og:og + 64, :, :], trb[og:og + 64, 0:128].unsqueeze(1).to_broadcast([64, 4, 128]), AF.Copy, [PB[BTR]], [nsTB])
                    CP("pool", nsT[gs, :, :], qT[gs, :, i * 128:(i + 1) * 128], [B("qT", j, i // 4) for j in range(4)] + [nsTB], [], awrites=[nsTB])
                    TT("dve", rc[:, 0, :], rc[:, 0, :], gates[:, i, g * 4:g * 4 + 4], ALU.mult, [B("rc", 0), B("gates", i)], [B("rc", 0)])
                    TT("dve", accu[:], ocv[:, :, 0:64], rc[:, 0, :].unsqueeze(2).to_broadcast([128, 4, 64]), ALU.mult, [PB[BOC], B("rc", 0)], [accB])
                    owv = v4(PS[BOW][:, 0:260])
                    TS("dve", rc[:, 2, :], owv[:, :, 64], 1e-30, None, ALU.max, None, [PB[BOW]], [B("rc", 2)])
                    RCP(rc[:, 2, :], rc[:, 2, :], [B("rc", 2)], [B("rc", 2)])
                    TT("dve", rc[:, 2, :], rc[:, 2, :], gates[:, i, 16 + g * 4:16 + g * 4 + 4], ALU.mult, [B("rc", 2), B("gates", i)], [B("rc", 2)])
                    TT("dve", tmp3[:], owv[:, :, 0:64], rc[:, 2, :].unsqueeze(2).to_broadcast([128, 4, 64]), ALU.mult, [PB[BOW], B("rc", 2)], [B("tmp3")])
                    TT("dve", accu[:], accu[:], tmp3[:], ALU.add, [accB, B("tmp3")], [accB])

                def stageB(i, g, u):
                    gs = slice(64 * g, 64 * g + 64)
                    accu, accB = accs[u % 2], B("acc", u % 2)
                    nsT, nsTB = nselTs[u % 2], B("nselT", u % 2)
                    tl_s = []
                    for kt in range(i + 1):
                        mask = (D2b, ALU.is_ge, 0.0) if kt == i else None
                        P, PBf = score_tile(i, g, 128, kA[g][:, kt * 128:(kt + 1) * 128], [B("kT", 1, kt // 4), B("kAE", g)], mask,
                                            extraB=[nsTB], slot=kt, rhs=nsT[:, :, :])
                        tl_s.append((P, PBf, 128, Vext[0][:, kt, g, :], [B("Vext", 0, kt), B("Vones", 0)]))
                    pv(tl_s, BOS)
                    osv = v4(PS[BOS][:, 0:260])
                    TS("dve", rc[:, 1, :], osv[:, :, 64], 1e-30, None, ALU.max, None, [PB[BOS]], [B("rc", 1)])
                    RCP(rc[:, 1, :], rc[:, 1, :], [B("rc", 1)], [B("rc", 1)])
                    TT("dve", rc[:, 1, :], rc[:, 1, :], gates[:, i, 8 + g * 4:8 + g * 4 + 4], ALU.mult, [B("rc", 1), B("gates", i)], [B("rc", 1)])
                    TT("dve", tmp3b[:], osv[:, :, 0:64], rc[:, 1, :].unsqueeze(2).to_broadcast([128, 4, 64]), ALU.mult, [PB[BOS], B("rc", 1)], [B("tmp3b")])
                    TT("dve", v4(otok[:, g * 256:(g + 1) * 256]), accu[:], tmp3b[:], ALU.add, [accB, B("tmp3b")], [B("otok", g)])
                    if g == 1:
                        def trs(h):
                            ins = None
                            for c4 in range(4):
                                ins = h.transpose(out=trb[:, c4 * 128:(c4 + 1) * 128], in_=otok[:, c4 * 128:(c4 + 1) * 128], identity=ident_b)
                            return ins
                        S.op("pe", trs, [B("otok", 0), B("otok", 1), CB], [PB[BTR]])
                        ACT(onst[i % 2][:], v4(trb[:, 0:512]), AF.Copy, [PB[BTR]], [B("onst", i % 2)])
                        DMA("sp", on_d[:, :, i * 128:(i + 1) * 128], onst[i % 2][:], reads=[B("onst", i % 2)], awrites=[B("on_d")])
                        if debug:
                            CP("dve", dbgt[:], otok[:], [B("otok", 0), B("otok", 1)], [B("dbgt")])
                            DMA("sp", dbg["onsa"][i * 128:(i + 1) * 128, :], dbgt[:], reads=[B("dbgt")])

                for u in range(len(units) + 1):
                    if u < len(units):
                        stageA(units[u][0], units[u][1], u)
                    if u >= 1:
                        stageB(units[u - 1][0], units[u - 1][1], u - 1)
                S.flush()

        if upto == 3:
            S.drain_dma()
            S.flush()
            raise _Stop(nc)
        XG, YD, X1F = B("xg_d"), B("y_d"), B("x1f_d")

        def layer_norm(pre, preB, gi, dst, dstB, st6, mv, sfx, lnp, ge="pool"):
            for hf in range(2):
                S.op("dve", lambda h, hf=hf: h.bn_stats(out=st6[:, hf, :], in_=pre[:, hf * 512:(hf + 1) * 512]), [preB], [B("st6" + sfx, hf)])
            S.op("dve", lambda h: h.bn_aggr(out=mv[:, 0:2], in_=st6[:, :, :].rearrange("p a b -> p (a b)")), [B("st6" + sfx, 0), B("st6" + sfx, 1)], [B("mv" + sfx)])
            ACT(mv[:, 2:3], mv[:, 1:2], AF.Sqrt, [B("mv" + sfx)], [B("mv" + sfx)], bias=epsc[:, 0:1])
            RCP(mv[:, 2:3], mv[:, 2:3], [B("mv" + sfx)], [B("mv" + sfx)])
            STT("dve", mv[:, 3:4], mv[:, 0:1], -1.0, mv[:, 2:3], ALU.mult, ALU.mult, [B("mv" + sfx)], [B("mv" + sfx)])
            ACT(pre[:], pre[:], AF.Identity, [preB, B("mv" + sfx)], [preB], scale=mv[:, 2:3], bias=mv[:, 3:4])
            TT(ge, pre[:], pre[:], lnp[:, gi, :], ALU.mult, [preB, B("lnp")], [preB])
            TT(ge, dst[:], pre[:], lnp[:, gi + 1, :], ALU.add, [preB, B("lnp")], [dstB])

        with ExitStack() as p4:
            wmg = sb(p4, "wmg", [128, 8, 2048], BF16)
            wupc = sb(p4, "wupc", [128, 4, 1024], BF16)
            wupn = sb(p4, "wupn", [128, 4, 1024], BF16)
            wo = sb(p4, "wo", [128, 8, 1024], BF16)
            wr = sb(p4, "wr", [128, 8, 32], F32)
            br_ = sb(p4, "br_", [128, 32], F32)
            xTc = [sb(p4, "xTc%d" % i, [128, 8, 512], BF16) for i in range(2)]
            oncs = [sb(p4, "onc%d" % i, [128, 4, 512], BF16) for i in range(2)]
            lnp = sb(p4, "lnp", [128, 2, 1024], F32)
            DMA("sp", lnp[:], I["lnp"][:, 0:2, :], writes=[B("lnp")])
            xt = [sb(p4, "xt%d" % i, [128, 1024], F32) for i in range(2)]
            sga = sb(p4, "sga", [128, 512], F32)
            sgb = sb(p4, "sgb", [128, 512], F32)
            m1 = sb(p4, "m1", [128, 512], F32)
            m2 = sb(p4, "m2", [128, 512], F32)
            mrgT = [sb(p4, "mrgT%d" % i, [128, 8, 512], BF16) for i in range(2)]
            pre = sb(p4, "pre", [128, 1024], F32)
            x1 = [sb(p4, "x1_%d" % i, [128, 1024], F32) for i in range(2)]
            x1b = [sb(p4, "x1b%d" % i, [128, 1024], BF16) for i in range(2)]
            x1T = [sb(p4, "x1T%d" % i, [128, 8, 128], F32) for i in range(2)]
            st6 = sb(p4, "st6", [128, 2, 6], F32)
            mv = sb(p4, "mv", [128, 4], F32)
            lg = sb(p4, "lg", [128, 32], F32)
            mx4 = sb(p4, "mx4", [128, 8], F32)
            nm = sb(p4, "nm", [128, 1], F32)
            msk = sb(p4, "msk", [128, 32], F32)
            mskb = sb(p4, "mskb", [128, 32], BF16)
            ex = sb(p4, "ex", [128, 32], F32)
            ssum = sb(p4, "ssum", [128, 1], F32)
            carry = sb(p4, "carry", [128, 32], F32)
            posf = sb(p4, "posf", [128, 32], F32)
            offv = sb(p4, "offv", [128, 32], F32)
            ovf = sb(p4, "ovf", [128, 32], F32)
            oh = sb(p4, "oh", [128, 32], F32)
            t32 = sb(p4, "t32", [128, 32], F32)
            offk = sb(p4, "offk", [128, 4], F32)
            DMA("pool", wmg[:], I["w_mg"].rearrange("(k p) n -> p k n", p=128), writes=[B("wmg")])
            DMA("pool", wupc[:], I["w_upc"].rearrange("(k p) n -> p k n", p=128), writes=[B("wupc")])
            DMA("pool", wupn[:], I["w_upn"].rearrange("(k p) n -> p k n", p=128), writes=[B("wupn")])
            DMA("pool", wo[:], I["w_o"].rearrange("(k p) n -> p k n", p=128), writes=[B("wo")])
            DMA("sp", wr[:], I["w_r"].rearrange("(k p) n -> p k n", p=128), writes=[B("wr")])
            DMA("sp", br_[:], I["b_r"], writes=[B("br")])
            MS("pool", carry[:], 0.0, [], [B("carry")])
            bi = 0
            bi_ = [0]

            def load_chunk(tc):
                t0 = tc * 512
                DMA("pool", xTc[tc % 2][:], I["xT"][:, t0:t0 + 512].rearrange("(k p) t -> p k t", p=128), writes=[B("xTc", tc % 2)])
                DMA("sp", oncs[tc % 2][:], on_d[:, :, t0:t0 + 512], reads=[B("on_d")], writes=[B("onc", tc % 2)])

            def merge_step(tc, oc):
                t0 = tc * 512
                xc, xcB = xTc[tc % 2], B("xTc", tc % 2)
                onc, oncB = oncs[tc % 2], B("onc", tc % 2)
                cs_ = slice(oc * 128, (oc + 1) * 128)
                b0, b1, b2, b3 = [(bi_[0] + j) % 4 for j in range(4)]
                MM(PS[b0][:, :], [(wmg[:, k, oc * 128:(oc + 1) * 128], xc[:, k, :]) for k in range(8)], [B("wmg"), xcB], [PB[b0]])
                ACT(sga[:], PS[b0][:, :], AF.Sigmoid, [PB[b0]], [B("sga")])
                MM(PS[b1][:, :], [(wmg[:, k, 1024 + oc * 128:1024 + (oc + 1) * 128], xc[:, k, :]) for k in range(8)], [B("wmg"), xcB], [PB[b1]])
                ACT(sgb[:], PS[b1][:, :], AF.Sigmoid, [PB[b1]], [B("sgb")])
                MM(PS[b2][:, :], [(wupc[:, k, cs_], u2T[:, k, t0:t0 + 512]) for k in range(4)], [B("wupc")] + [B("u2T", k, tc) for k in range(4)], [PB[b2]])
                TT("dve", m1[:], PS[b2][:, :], sga[:], ALU.mult, [PB[b2], B("sga")], [B("m1")])
                MM(PS[b3][:, :], [(wupn[:, k, cs_], onc[:, k, :]) for k in range(4)], [B("wupn"), oncB], [PB[b3]])
                TT("dve", m2[:], PS[b3][:, :], sgb[:], ALU.mult, [PB[b3], B("sgb")], [B("m2")])
                TT("pool", mrgT[tc % 2][:, oc, :], m1[:], m2[:], ALU.add, [B("m1"), B("m2")], [B("mrgT", tc % 2, oc)])


            def H1a(i):
                tc, tt = i // 4, i % 4
                xti, xtB = xt[i % 2], B("xt", i % 2)
                x1i, x1B = x1[i % 2], B("x1", i % 2)
                x1bi, x1bB = x1b[i % 2], B("x1b", i % 2)
                DMA("sp", xti[:], I["xtok"][i * 128:(i + 1) * 128, :], writes=[xtB])
                for hf in range(2):
                    bz = 4 + hf
                    MM(PS[bz][:, :], [(mrgT[tc % 2][:, k, tt * 128:(tt + 1) * 128], wo[:, k, hf * 512:(hf + 1) * 512]) for k in range(8)], [B("wo")] + [B("mrgT", tc % 2, k) for k in range(8)], [PB[bz]])
                    STT("dve", pre[:, hf * 512:(hf + 1) * 512], xti[:, hf * 512:(hf + 1) * 512], DN_ALPHA, PS[bz][:, :], ALU.mult, ALU.add, [xtB, PB[bz]], [B("pre")] if hf == 0 else [], awrites=[] if hf == 0 else [B("pre")])
                layer_norm(pre, B("pre"), 0, x1i, x1B, st6, mv, "a", lnp)
                DMA("sp", x1f_d[i * 128:(i + 1) * 128, :], x1i[:], reads=[x1B], awrites=[X1F])
                if debug:
                    DMA("sp", dbg["x1"][i * 128:(i + 1) * 128, :], x1i[:], reads=[x1B])
                ACT(x1bi[:], x1i[:], AF.Copy, [x1B], [x1bB])

            def H1b(i):
                x1i, x1B = x1[i % 2], B("x1", i % 2)
                for half in range(2):
                    bt = 6 + half

                    def trf(h, half=half, bt=bt, x1i=x1i):
                        ins = None
                        for k4 in range(4):
                            k = half * 4 + k4
                            ins = h.transpose(out=PS[bt][:, k4 * 128:(k4 + 1) * 128], in_=x1i[:, k * 128:(k + 1) * 128], identity=ident_f)
                        return ins
                    S.op("pe", trf, [x1B, CF], [PB[bt]])
                    if half == 0:
                        ACT(x1T[i % 2][:, 0:4, :], v4(PS[bt][:, :]), AF.Copy, [PB[bt]], [B("x1T", i % 2, 0)])
                    else:
                        ACT(x1T[i % 2][:, 4:8, :], v4(PS[bt][:, :]), AF.Copy, [PB[bt]], [B("x1T", i % 2, 1)])


            def H2(i):
                x1bi, x1bB = x1b[i % 2], B("x1b", i % 2)
                bl = bi_[0] % 4
                MM(PS[bl][:, 0:32], [(x1T[i % 2][:, k, :], wr[:, k, :]) for k in range(8)], [B("x1T", i % 2, 0), B("x1T", i % 2, 1), B("wr")], [PB[bl]])
                TT("dve", lg[:], PS[bl][:, 0:32], br_[:], ALU.add, [PB[bl], B("br")], [B("lg")])
                if debug:
                    DMA("sp", dbg["lg"][i * 128:(i + 1) * 128, :], lg[:], reads=[B("lg")])
                S.op("dve", lambda h: h.max(out=mx4[:], in_=lg[:]), [B("lg")], [B("mx4")])
                TS("dve", msk[:], lg[:], mx4[:, 3:4], None, ALU.is_ge, None, [B("lg"), B("mx4")], [B("msk")])
                TS("dve", nm[:], mx4[:, 0:1], -1.0, None, ALU.mult, None, [B("mx4")], [B("nm")])
                ACT(ex[:], lg[:], AF.Exp, [B("lg"), B("nm")], [B("ex")], bias=nm[:, 0:1])
                TT("dve", ex[:], ex[:], msk[:], ALU.mult, [B("ex"), B("msk")], [B("ex")])
                S.op("dve", lambda h: h.reduce_sum(out=ssum[:], in_=ex[:], axis=AX.X), [B("ex")], [B("ssum")])
                RCP(ssum[:], ssum[:], [B("ssum")], [B("ssum")])
                TS("dve", gd[:, i, :], ex[:], ssum[:, 0:1], None, ALU.mult, None, [B("ex"), B("ssum")], [B("gd", i)])
                CP("dve", mskb[:], msk[:], [B("msk")], [B("mskb")])
                bp = (bi_[0] + 1) % 4
                bc_ = (bi_[0] + 2) % 4
                MM(PS[bp][:, 0:32], [(Ust, mskb[:])], [CB, B("mskb")], [PB[bp]])
                MM(PS[bc_][:, 0:32], [(ones_b, mskb[:])], [CB, B("mskb")], [PB[bc_]])
                TT("dve", posf[:], PS[bp][:, 0:32], carry[:], ALU.add, [PB[bp], B("carry")], [B("posf")])
                TT("dve", carry[:], PS[bc_][:, 0:32], carry[:], ALU.add, [PB[bc_], B("carry")], [B("carry")])
                TS("dve", ovf[:], posf[:], float(CAP), 1e6, ALU.is_ge, ALU.mult, [B("posf")], [B("ovf")])
                TT("dve", offv[:], posf[:], eoff, ALU.add, [B("posf"), CF], [B("offv")])
                TT("dve", offv[:], offv[:], ovf[:], ALU.add, [B("offv"), B("ovf")], [B("offv")])
                TS("dve", offv[:], offv[:], float(32 * CAP), None, ALU.min, None, [B("offv")], [B("offv")])
                for k in range(4):
                    TS("dve", oh[:], lg[:], mx4[:, k:k + 1], None, ALU.is_equal, None, [B("lg"), B("mx4")], [B("oh")])
                    TT("dve", t32[:], oh[:], offv[:], ALU.mult, [B("oh"), B("offv")], [B("t32")])
                    S.op("dve", lambda h, k=k: h.reduce_sum(out=offk[:, k:k + 1], in_=t32[:], axis=AX.X), [B("t32")], [B("offk", k)])
                    TT("dve", t32[:], oh[:], gd[:, i, :], ALU.mult, [B("oh"), B("gd", i), B("t32")], [B("t32")])
                    S.op("dve", lambda h, k=k, i=i: h.reduce_sum(out=gk[:, i, k:k + 1], in_=t32[:], axis=AX.X), [B("t32")], [B("gk", i, k)])
                CP("dve", offs[:, i, :], offk[:], [B("offk", k) for k in range(4)], [B("offs", i)])
                for k in range(4):
                    S.dma("pool", lambda h, i=i, k=k, x1bi=x1bi: h.indirect_dma_start(
                        out=xg_d, out_offset=bass.IndirectOffsetOnAxis(ap=offs[:, i, k:k + 1], axis=0), in_=x1bi[:], in_offset=None,
                        ), reads=[x1bB, B("offs", i)], awrites=[XG])
                bi_[0] += 3


            load_chunk(0)
            for oc in range(8):
                merge_step(0, oc)
            pend = None
            for tc in range(NCH):
                if tc + 1 < NCH:
                    load_chunk(tc + 1)
                for tt in range(4):
                    i = tc * 4 + tt
                    H1a(i)
                    if tc + 1 < NCH:
                        merge_step(tc + 1, 2 * tt)
                        merge_step(tc + 1, 2 * tt + 1)
                    H1b(i)
                    if pend is not None:
                        H2(pend)
                    pend = i
            H2(pend)
            S.flush()

        if upto == 4:
            S.drain_dma()
            S.flush()
            raise _Stop(nc)
        mid.close()
        with ExitStack() as p5:
            wg = [sb(p5, "wg%d" % i, [128, 8, 1024], BF16) for i in range(2)]
            wl = [sb(p5, "wl%d" % i, [128, 8, 1024], BF16) for i in range(2)]
            wd = [sb(p5, "wd%d" % i, [128, 8, 1024], BF16) for i in range(2)]
            bgu = sb(p5, "bgu", [128, 32, 2, 8], F32)
            xgt = [sb(p5, "xgt%d" % i, [128, NCAPT, 1024], BF16) for i in range(2)]
            xgT = sb(p5, "xgT", [128, 8, CAP], BF16)
            actT = sb(p5, "actT", [128, 8, CAP], BF16)
            NCK = (CAP + 511) // 512
            CW = CAP // NCK
            NBF = 4
            g1 = [sb(p5, "g1_%d" % i, [128, CW], F32) for i in range(NBF)]
            sg = [sb(p5, "sg_%d" % i, [128, CW], F32) for i in range(NBF)]
            l1 = [sb(p5, "l1_%d" % i, [128, CW], F32) for i in range(NBF)]
            ysb = [sb(p5, "ysb%d" % i, [128, 1024], F32) for i in range(2)]
            DMA("sp", bgu[:], I["bgu"].rearrange("p (e a f) -> p e a f", e=32, a=2), writes=[B("bgu")])
            MS("pool", ysb[0][0:1, :], 0.0, [], [B("ysb", 0)])
            DMA("sp", y_d[32 * CAP:32 * CAP + 1, :], ysb[0][0:1, :], reads=[B("ysb", 0)], awrites=[YD])
            nch = [(c * CW, CW) for c in range(NCK)]
            ei = 0
            yi = 0
            bi = 0
            def load_expert(e):
                eb = e % 2
                DMA("sp", xgt[eb][:], xg_d[e * CAP:(e + 1) * CAP, :].rearrange("(s p) d -> p s d", p=128), reads=[XG], writes=[B("xgt", eb)])
                DMA("pool", wg[eb][:], I["w_glu"][e].rearrange("(k p) n -> p k n", p=128), writes=[B("wg", eb)])
                DMA("pool", wl[eb][:], I["w_lin"][e].rearrange("(k p) n -> p k n", p=128), writes=[B("wl", eb)])
                DMA("pool", wd[eb][:], I["w_dn"][e].rearrange("(k p) n -> p k n", p=128), writes=[B("wd", eb)])

            load_expert(0)
            for e in range(N_EXPERTS):
                eb = e % 2
                if e + 1 < N_EXPERTS:
                    load_expert(e + 1)
                trbb = None
                for s in range(NCAPT):
                    bt = 6 + (s % 2)
                    trbb = PS[bt][:, :].bitcast(BF16)

                    def trx(h, s=s, trbb=trbb, eb=eb):
                        ins = None
                        for k in range(8):
                            ins = h.transpose(out=trbb[:, k * 128:(k + 1) * 128], in_=xgt[eb][:, s, k * 128:(k + 1) * 128], identity=ident_b)
                        return ins
                    S.op("pe", trx, [B("xgt", eb), CB], [PB[bt]])
                    if s % 2 == 0:
                        ACT(xgT[:, :, s * 128:(s + 1) * 128], v4(trbb[:, 0:1024], 8), AF.Copy, [PB[bt]], [B("xgT", s)])
                    else:
                        CP("dve", xgT[:, :, s * 128:(s + 1) * 128], v4(trbb[:, 0:1024], 8), [PB[bt]], [B("xgT", s)])
                xgB = [B("xgT", s) for s in range(NCAPT)]
                for f in range(8):
                    fs = slice(f * 128, (f + 1) * 128)
                    for (n0, nn) in nch:
                        bg_, bl_ = bi % 4, (bi + 1) % 4
                        bi += 2
                        gg, ggB = g1[ei % NBF], B("g1", ei % NBF)
                        ss, ssB = sg[ei % NBF], B("sg", ei % NBF)
                        ll, llB = l1[ei % NBF], B("l1", ei % NBF)
                        ei += 1
                        MM(PS[bg_][:, 0:nn], [(wg[eb][:, k, fs], xgT[:, k, n0:n0 + nn]) for k in range(8)], [B("wg", eb)] + xgB, [PB[bg_]])
                        MM(PS[bl_][:, 0:nn], [(wl[eb][:, k, fs], xgT[:, k, n0:n0 + nn]) for k in range(8)], [B("wl", eb)] + xgB, [PB[bl_]])
                        TS("dve", gg[:, 0:nn], PS[bg_][:, 0:nn], bgu[:, e, 0, f:f + 1], 7.0, ALU.add, ALU.min, [PB[bg_], B("bgu")], [ggB])
                        ACT(ss[:, 0:nn], gg[:, 0:nn], AF.Sigmoid, [ggB], [ssB], scale=1.702)
                        TS("dve", ll[:, 0:nn], PS[bl_][:, 0:nn], bgu[:, e, 1, f:f + 1], 7.0, ALU.add, ALU.min, [PB[bl_], B("bgu")], [llB])
                        TS("dve", ll[:, 0:nn], ll[:, 0:nn], -7.0, 1.0, ALU.max, ALU.add, [llB], [llB])
                        TT("pool", gg[:, 0:nn], gg[:, 0:nn], ss[:, 0:nn], ALU.mult, [ggB, ssB], [ggB])
                        TT("pool", actT[:, f, n0:n0 + nn], gg[:, 0:nn], ll[:, 0:nn], ALU.mult, [ggB, llB], [B("actT", f)] if n0 == 0 else [], awrites=[] if n0 == 0 else [B("actT", f)])
                aB = [B("actT", f) for f in range(8)]
                for s in range(NCAPT):
                    yy, yB = ysb[yi % 2], B("ysb", yi % 2)
                    yi += 1
                    for hf in range(2):
                        by = 4 + hf
                        MM(PS[by][:, :], [(actT[:, f, s * 128:(s + 1) * 128], wd[eb][:, f, hf * 512:(hf + 1) * 512]) for f in range(8)], [B("wd", eb)] + aB, [PB[by]])
                        ACT(yy[:, hf * 512:(hf + 1) * 512], PS[by][:, :], AF.Copy, [PB[by]], [yB] if hf == 0 else [], awrites=[] if hf == 0 else [yB])
                    r0 = e * CAP + s * 128
                    DMA("sp", y_d[r0:r0 + 128, :], yy[:], reads=[yB], awrites=[YD])
            S.flush()

        if upto == 5:
            S.drain_dma()
            S.flush()
            raise _Stop(nc)
        with ExitStack() as p6:
            yk = [sb(p6, "yk%d" % i, [128, 4, 1024], F32) for i in range(3)]
            x1r = [sb(p6, "x1r%d" % i, [128, 1024], F32) for i in range(2)]
            accfs = [sb(p6, "accf%d" % i, [128, 1024], F32) for i in range(2)]
            outt = [sb(p6, "outt%d" % i, [128, 1024], F32) for i in range(2)]
            gdT = sb(p6, "gdT", [32, 128], F32)
            bdn = sb(p6, "bdn", [32, 1024], F32)
            st6b = sb(p6, "st6b", [128, 2, 6], F32)
            mvb = sb(p6, "mvb", [128, 4], F32)
            DMA("sp", bdn[:], I["b_dn"], writes=[B("bdn")])
            lnp = sb(p6, "lnp6", [128, 4, 1024], F32)
            DMA("sp", lnp[:], I["lnp"], writes=[B("lnp")])
            for i in range(NT):
                yki, ykB = yk[i % 3], B("yk", i % 3)
                x1i, x1B = x1r[i % 2], B("x1r", i % 2)
                oi, oB = outt[i % 2], B("outt", i % 2)
                accf, accB6 = accfs[i % 2], B("accf", i % 2)
                MS("pool", yki[:, 0, 0:1], 0.0, [], [ykB])
                for k in range(4):
                    S.dma("pool", lambda h, i=i, k=k, yki=yki: h.indirect_dma_start(
                        out=yki[:, k, :], out_offset=None, in_=y_d, in_offset=bass.IndirectOffsetOnAxis(ap=offs[:, i, k:k + 1], axis=0),
                        ), reads=[YD, B("offs", i)], awrites=[ykB])
                DMA("sp", x1i[:], x1f_d[i * 128:(i + 1) * 128, :], reads=[X1F], writes=[x1B])
                ACT(accf[:], x1i[:], AF.Copy, [x1B], [accB6], scale=DN_ALPHA)
                for k in range(4):
                    STT("dve", accf[:], yki[:, k, :], gk[:, i, k:k + 1], accf[:], ALU.mult, ALU.add, [ykB, B("gk", i, k), accB6], [accB6])
                S.op("pe", lambda h, i=i: h.transpose(out=PS[6][0:32, 0:128], in_=gd[:, i, :], identity=ident_f), [B("gd", i), CF], [PB[6]])
                ACT(gdT[:], PS[6][0:32, 0:128], AF.Copy, [PB[6]], [B("gdT")])
                for hf in range(2):
                    bb_ = 4 + hf
                    MM(PS[bb_][:, :], [(gdT[:], bdn[:, hf * 512:(hf + 1) * 512])], [B("gdT"), B("bdn")], [PB[bb_]])
                    TT("dve", accf[:, hf * 512:(hf + 1) * 512], accf[:, hf * 512:(hf + 1) * 512], PS[bb_][:, :], ALU.add, [accB6, PB[bb_]], [accB6])
                layer_norm(accf, accB6, 2, oi, oB, st6b, mvb, "b", lnp, ge="dve")
                DMA("sp", out[i * 128:(i + 1) * 128, :], oi[:], reads=[oB])
            S.drain_dma()
            S.flush()
    return nc


_CACHE = {}


def core_inputs(inp, sh, b, T, consts):
    m = dict(sh)
    m["xT"] = np.ascontiguousarray(inp["x"][b, :T].T)
    m["xtok"] = np.ascontiguousarray(inp["x"][b, :T])
    m["pos"] = np.ascontiguousarray(inp["positions"][b, :T].reshape(1, T).astype(np.int32))
    m["cf"], m["cb"] = consts
    return m


def kernel(**inputs):
    inp = {k: np.asarray(v) for k, v in inputs.items()}
    Bn, T = inp["x"].shape[0], inp["x"].shape[1]
    sh = prep_shared(inp)
    consts = make_consts(T)
    nc = build(T)
    in_maps = [core_inputs(inp, sh, b, T, consts) for b in range(Bn)]
    res = run_bass_kernel_spmd(nc, in_maps, core_ids=list(range(Bn)))
    out = np.stack([np.asarray(r["out"]) for r in res.results], 0).astype(np.float32)
    return out
```
